# Optimizing a Trainium2 kernel written in Bass

```python
import math
import jax, jax.numpy as jnp
from jax import lax
import numpy as np

D_MODEL = 1024
BATCH = 8
SEQ = 2048
DEPTH = 4

RMS_EPS = 1e-6
NEG_INF = -1e30

MLA_HEADS = 16
MLA_Q_LORA = 512
MLA_KV_LORA = 256
MLA_NOPE = 64
MLA_ROPE = 32
MLA_V = 64
MLA_ROPE_THETA = 10000.0
ATTN_BLOCK = 128

DIL_PATTERN = ((128, 1), (512, 4), (2048, 16))
DIL_GROUPS = 3
DIL_HEADS = 4
DIL_HEAD_DIM = 64
ROPE_THETA = 500000.0
PARTIAL_ROT = DIL_HEAD_DIM // 4

MLA_COLS = MLA_Q_LORA + MLA_KV_LORA + MLA_ROPE
DIL_COLS = DIL_GROUPS * 3 * DIL_HEADS * DIL_HEAD_DIM
GATE_COLS = 2 * D_MODEL
IN_COLS = MLA_COLS + DIL_COLS + GATE_COLS
MLA_OUT = MLA_HEADS * MLA_V
DIL_OUT = DIL_HEADS * DIL_HEAD_DIM

N_GROUPS = 8
EXPERTS_PER_GROUP = 8
N_EXPERTS = N_GROUPS * EXPERTS_PER_GROUP
TOP_K = 2
EXPERT_FF = 256
EXPERT_BLOCK = 128

PLE_DIM = 256

kernel_name = 'hybrid_mla_dilated_hmoe_block'


def rms_norm(x, g):
    xf = x.astype(jnp.float32)
    y = xf * lax.rsqrt(jnp.mean(xf * xf, axis=-1, keepdims=True) + RMS_EPS)
    return (y * g.astype(jnp.float32)).astype(x.dtype)


def apply_rope(x, pos, theta, rot_dim):
    half = rot_dim // 2
    inv = jnp.float32(theta) ** (-jnp.arange(half, dtype=jnp.float32) * 2.0 / rot_dim)
    ang = pos.astype(jnp.float32)[..., None] * inv
    cos = jnp.cos(ang)[:, :, None, :].astype(x.dtype)
    sin = jnp.sin(ang)[:, :, None, :].astype(x.dtype)
    x1 = x[..., :half]
    x2 = x[..., half:rot_dim]
    return jnp.concatenate([x1 * cos - x2 * sin, x1 * sin + x2 * cos, x[..., rot_dim:]], axis=-1)


def causal_block_attention(q, k, v, scale):
    B, S, H, Dq = q.shape
    Dv = v.shape[-1]
    nb = S // ATTN_BLOCK
    qb = q.reshape(B, nb, ATTN_BLOCK, H, Dq).transpose(1, 0, 2, 3, 4)
    kpos = jnp.arange(S)

    def one_block(args):
        qi, b = args
        s = jnp.einsum('bqhd,bkhd->bhqk', qi, k).astype(jnp.float32) * scale
        qpos = b * ATTN_BLOCK + jnp.arange(ATTN_BLOCK)
        s = jnp.where(kpos[None, :] <= qpos[:, None], s, NEG_INF)
        pr = jax.nn.softmax(s, axis=-1).astype(v.dtype)
        return jnp.einsum('bhqk,bkhd->bqhd', pr, v)

    out = lax.map(one_block, (qb, jnp.arange(nb)))
    return out.transpose(1, 0, 2, 3, 4).reshape(B, S, H, Dv)


def dilated_window_attention(q, k, v, dilation, window):
    B, S, H, Dh = q.shape
    nback = window // dilation
    L = S // dilation
    nb = -(-L // nback)
    Lp = nb * nback

    def to_blocks(t):
        t = t.reshape(B, L, dilation, H, Dh).transpose(0, 2, 1, 3, 4)
        t = jnp.pad(t, ((0, 0), (0, 0), (0, Lp - L), (0, 0), (0, 0)))
        return t.reshape(B, dilation, nb, nback, H, Dh)

    def with_prev(t):
        prev = jnp.pad(t[:, :, :-1], ((0, 0), (0, 0), (1, 0), (0, 0), (0, 0), (0, 0)))
        return jnp.concatenate([prev, t], axis=3)

    qb = to_blocks(q)
    kk = with_prev(to_blocks(k))
    vv = with_prev(to_blocks(v))
    s = jnp.einsum('brnqhd,brnkhd->brnhqk', qb, kk).astype(jnp.float32) * (Dh ** -0.5)
    qi = jnp.arange(nback)[:, None]
    kj = jnp.arange(2 * nback)[None, :]
    band = (kj >= qi) & (kj <= qi + nback)
    has_prev = jnp.arange(nb)[:, None, None] > 0
    mask = band[None] & (has_prev | (kj >= nback)[None])
    s = jnp.where(mask[None, None, :, None], s, NEG_INF)
    m = jnp.max(s, axis=-1, keepdims=True)
    e = jnp.exp(s - m)
    den = jnp.sum(e, axis=-1, keepdims=True)
    o = jnp.einsum('brnhqk,brnkhd->brnqhd', (e / den).astype(v.dtype), vv)
    lse = (m + jnp.log(den))[..., 0]
    o = o.reshape(B, dilation, Lp, H, Dh)[:, :, :L].transpose(0, 2, 1, 3, 4).reshape(B, S, H, Dh)
    lse = lse.transpose(0, 1, 2, 4, 3).reshape(B, dilation, Lp, H)[:, :, :L]
    lse = lse.transpose(0, 2, 1, 3).reshape(B, S, H)
    return o, lse


def routed_expert_ffn(hf, expert_idx, gate_w, w1, w3, w2):
    T, D = hf.shape
    A = T * TOP_K
    flat_e = expert_idx.reshape(A)
    flat_tok = jnp.arange(A, dtype=jnp.int32) // TOP_K
    flat_w = gate_w.reshape(A)
    order = jnp.argsort(flat_e)
    e_s = flat_e[order]
    tok_s = flat_tok[order]
    w_s = flat_w[order]
    counts = jnp.bincount(flat_e, length=N_EXPERTS)
    starts = jnp.cumsum(counts) - counts
    pcounts = (counts + EXPERT_BLOCK - 1) // EXPERT_BLOCK * EXPERT_BLOCK
    pends = jnp.cumsum(pcounts)
    pstarts = pends - pcounts
    dest = pstarts[e_s] + (jnp.arange(A, dtype=jnp.int32) - starts[e_s])
    n_blocks = -(-A // EXPERT_BLOCK) + N_EXPERTS
    n_rows = n_blocks * EXPERT_BLOCK
    row_tok = jnp.zeros((n_rows,), jnp.int32).at[dest].set(tok_s)
    xs = hf[row_tok].reshape(n_blocks, EXPERT_BLOCK, D)
    block_e = jnp.minimum(jnp.searchsorted(pends, jnp.arange(n_blocks) * EXPERT_BLOCK, side='right'), N_EXPERTS - 1)

    def one_block(args):
        xb, e = args
        return (jax.nn.silu(xb @ w1[e]) * (xb @ w3[e])) @ w2[e]

    ys = lax.map(one_block, (xs, block_e)).reshape(n_rows, D)
    contrib = ys[dest] * w_s[:, None].astype(hf.dtype)
    return jnp.zeros_like(hf).at[tok_s].add(contrib)


def hier_moe(h, w_grp, b_grp, w_exp, b_exp, w1, w3, w2):
    B, S, D = h.shape
    T = B * S
    hf = h.reshape(T, D)
    g_logits = (hf @ w_grp).astype(jnp.float32) + b_grp.astype(jnp.float32)
    g_prob = jax.nn.softmax(g_logits, axis=-1)
    g_sel = jnp.argmax(g_logits, axis=-1).astype(jnp.int32)
    e_logits = (hf @ w_exp).astype(jnp.float32).reshape(T, N_GROUPS, EXPERTS_PER_GROUP) + b_exp.astype(jnp.float32)
    idx = jnp.broadcast_to(g_sel[:, None, None], (T, 1, EXPERTS_PER_GROUP))
    e_in = jnp.take_along_axis(e_logits, idx, axis=1)[:, 0]
    top_v, top_i = lax.top_k(e_in, TOP_K)
    p_g = jnp.take_along_axis(g_prob, g_sel[:, None], axis=1)
    gate_w = jax.nn.softmax(top_v, axis=-1) * p_g
    expert_idx = g_sel[:, None] * EXPERTS_PER_GROUP + top_i.astype(jnp.int32)
    return routed_expert_ffn(hf, expert_idx, gate_w, w1, w3, w2).reshape(B, S, D)


def setup_inputs(seed: int = 0) -> dict:
    key = jax.random.key(seed)
    ks = jax.random.split(key, 32)
    f32 = jnp.float32

    def w(k, shape, fan_in):
        return jax.random.normal(k, shape, f32) * (fan_in ** -0.5)

    def gain(k, shape):
        return 1.0 + 0.02 * jax.random.normal(k, shape, f32)

    return {
        'x': jax.random.normal(ks[0], (BATCH, SEQ, D_MODEL), f32),
        'p': jax.random.normal(ks[1], (DEPTH, BATCH, SEQ, PLE_DIM), f32),
        'positions': jnp.broadcast_to(jnp.arange(SEQ, dtype=jnp.int32), (BATCH, SEQ)),
        'g_mix': gain(ks[2], (DEPTH, D_MODEL)),
        'w_in': w(ks[3], (DEPTH, D_MODEL, IN_COLS), D_MODEL),
        'g_q_lat': gain(ks[4], (DEPTH, MLA_Q_LORA)),
        'w_q_up': w(ks[5], (DEPTH, MLA_Q_LORA, MLA_HEADS * (MLA_NOPE + MLA_ROPE)), MLA_Q_LORA),
        'g_kv_lat': gain(ks[6], (DEPTH, MLA_KV_LORA)),
        'w_kv_up': w(ks[7], (DEPTH, MLA_KV_LORA, MLA_HEADS * (MLA_NOPE + MLA_V)), MLA_KV_LORA),
        'w_branch_a': w(ks[8], (DEPTH, MLA_OUT, D_MODEL), MLA_OUT),
        'w_branch_b': w(ks[9], (DEPTH, DIL_OUT, D_MODEL), DIL_OUT),
        'w_out': w(ks[10], (DEPTH, D_MODEL, D_MODEL), D_MODEL),
        'g_ffn': gain(ks[11], (DEPTH, D_MODEL)),
        'w_router_grp': w(ks[12], (DEPTH, D_MODEL, N_GROUPS), D_MODEL),
        'b_router_grp': 0.01 * jax.random.normal(ks[13], (DEPTH, N_GROUPS), f32),
        'w_router_exp': w(ks[14], (DEPTH, D_MODEL, N_EXPERTS), D_MODEL),
        'b_router_exp': 0.01 * jax.random.normal(ks[15], (DEPTH, N_GROUPS, EXPERTS_PER_GROUP), f32),
        'w_exp_gate': w(ks[16], (DEPTH, N_EXPERTS, D_MODEL, EXPERT_FF), D_MODEL),
        'w_exp_up': w(ks[17], (DEPTH, N_EXPERTS, D_MODEL, EXPERT_FF), D_MODEL),
        'w_exp_down': w(ks[18], (DEPTH, N_EXPERTS, EXPERT_FF, D_MODEL), EXPERT_FF),
        'g_ple': gain(ks[19], (DEPTH, D_MODEL)),
        'w_ple_gate': w(ks[20], (DEPTH, D_MODEL, D_MODEL), D_MODEL),
        'w_ple_proj': w(ks[21], (DEPTH, PLE_DIM, D_MODEL), PLE_DIM),
        'g_final': gain(ks[22], (D_MODEL,)),
    }


def reference(x, p, positions, g_mix, w_in, g_q_lat, w_q_up, g_kv_lat, w_kv_up, w_branch_a, w_branch_b, w_out, g_ffn, w_router_grp, b_router_grp, w_router_exp, b_router_exp, w_exp_gate, w_exp_up, w_exp_down, g_ple, w_ple_gate, w_ple_proj, g_final):
    B, S, D = x.shape
    splits = [MLA_Q_LORA, MLA_Q_LORA + MLA_KV_LORA, MLA_COLS, MLA_COLS + DIL_COLS]
    for i in range(DEPTH):
        h = rms_norm(x, g_mix[i])
        proj = h @ w_in[i]
        c_q, c_kv, k_pe, dil, gates = jnp.split(proj, splits, axis=-1)

        q = (rms_norm(c_q, g_q_lat[i]) @ w_q_up[i]).reshape(B, S, MLA_HEADS, MLA_NOPE + MLA_ROPE)
        q = jnp.concatenate([q[..., :MLA_NOPE], apply_rope(q[..., MLA_NOPE:], positions, MLA_ROPE_THETA, MLA_ROPE)], axis=-1)
        kv = (rms_norm(c_kv, g_kv_lat[i]) @ w_kv_up[i]).reshape(B, S, MLA_HEADS, MLA_NOPE + MLA_V)
        k_rot = apply_rope(k_pe[:, :, None, :], positions, MLA_ROPE_THETA, MLA_ROPE)
        k = jnp.concatenate([kv[..., :MLA_NOPE], jnp.broadcast_to(k_rot, (B, S, MLA_HEADS, MLA_ROPE))], axis=-1)
        v = kv[..., MLA_NOPE:]
        o_a = causal_block_attention(q, k, v, (MLA_NOPE + MLA_ROPE) ** -0.5).reshape(B, S, MLA_OUT)

        dil = dil.reshape(B, S, DIL_GROUPS, 3, DIL_HEADS, DIL_HEAD_DIM)
        outs = []
        lses = []
        for gi, (win, rate) in enumerate(DIL_PATTERN):
            qg = apply_rope(dil[:, :, gi, 0], positions, ROPE_THETA, PARTIAL_ROT)
            kg = apply_rope(dil[:, :, gi, 1], positions, ROPE_THETA, PARTIAL_ROT)
            o_g, lse_g = dilated_window_attention(qg, kg, dil[:, :, gi, 2], rate, win)
            outs.append(o_g)
            lses.append(lse_g)
        wts = jax.nn.softmax(jnp.stack(lses, axis=0), axis=0)
        o_b = jnp.sum(wts[..., None].astype(x.dtype) * jnp.stack(outs, axis=0), axis=0).reshape(B, S, DIL_OUT)

        g_a, g_b = jnp.split(gates, 2, axis=-1)
        merged = jax.nn.sigmoid(g_a) * (o_a @ w_branch_a[i]) + jax.nn.sigmoid(g_b) * (o_b @ w_branch_b[i])
        x = x + merged @ w_out[i]

        h2 = rms_norm(x, g_ffn[i])
        x = x + hier_moe(h2, w_router_grp[i], b_router_grp[i], w_router_exp[i], b_router_exp[i], w_exp_gate[i], w_exp_up[i], w_exp_down[i])

        e = p[i] @ w_ple_proj[i]
        x = x + jax.nn.sigmoid(rms_norm(x, g_ple[i]) @ w_ple_gate[i]) * e
    return rms_norm(x, g_final)
```

```python
import math
import bisect
from contextlib import ExitStack

import numpy as np
import concourse.bass as bass
import concourse.mybir as mybir
from concourse.bass_utils import run_bass_kernel_spmd

F32 = mybir.dt.float32
BF16 = mybir.dt.bfloat16
I32 = mybir.dt.int32
AF = mybir.ActivationFunctionType
ALU = mybir.AluOpType
AX = mybir.AxisListType

S = 2048
D = 1024
NT = 16
DEPTH = 4
NCORES = 8
EPS = 1e-6
CAP = 128
NEXP = 64
XS_ROWS = NEXP * CAP
PI = math.pi

EPOCH = 30000


class Prog:
    ENGS = ("pe", "act", "dve", "pool", "sp")

    def __init__(self):
        self.ops = []
        self.lastw = {}
        self.rd = {}

    def add(self, eng, fn, r=(), w=(), dma=None, noinc=False):
        i = len(self.ops)
        raw = set()
        oth = set()
        psr = [k for k in r if isinstance(k, tuple) and k and k[0] == "ps"]
        if psr:
            r = [k for k in r if not (isinstance(k, tuple) and k and k[0] == "ps")]
            w = list(w) + [k for k in psr if k not in w]
        for k in r:
            lw = self.lastw.get(k)
            if lw is not None:
                raw.add(lw)
        for k in w:
            lw = self.lastw.get(k)
            if lw is not None:
                oth.add(lw)
            oth.update(self.rd.get(k, ()))
        raw.discard(i)
        oth.discard(i)
        for k in r:
            self.rd.setdefault(k, []).append(i)
        for k in w:
            self.lastw[k] = i
            self.rd[k] = []
        self.ops.append(dict(eng=eng, fn=fn, raw=raw, oth=oth - raw, dma=dma, noinc=noinc))
        return i

    def alias(self, new_keys, old_keys):
        s = set()
        for k in old_keys:
            lw = self.lastw.get(k)
            if lw is not None:
                s.add(lw)
            s.update(self.rd.get(k, ()))
        for k in new_keys:
            cur = self.rd.get(k, [])
            self.rd[k] = list(set(cur) | s)

    def emit(self, nc, stack, trunc=None):
        ops = self.ops
        cnt = {}
        dma_hist = {}
        for i, op in enumerate(ops):
            op["skip"] = trunc is not None and trunc[0] <= i < trunc[1]
            if op["skip"]:
                op["stream"] = None
                continue
            if op["noinc"]:
                op["stream"] = None
                continue
            if op["dma"] is not None:
                base = "d_" + op["dma"]
                c = cnt.get(base, 0)
                cnt[base] = c + 1
                per = EPOCH // 16
                op["stream"] = f"{base}_{c // per}"
                op["val"] = (c % per + 1) * 16
                dma_hist.setdefault(op["stream"], []).append((i, op["val"]))
            else:
                base = "c_" + op["eng"]
                c = cnt.get(base, 0)
                cnt[base] = c + 1
                op["stream"] = f"{base}_{c // EPOCH}"
                op["val"] = c % EPOCH + 1
        streams = sorted({op["stream"] for op in ops if op["stream"] is not None})
        sems = {s: stack.enter_context(nc.semaphore(s)) for s in streams}
        self.n_sems = len(sems)
        queues = {E: [] for E in self.ENGS}
        for i, op in enumerate(ops):
            queues[op["eng"]].append(i)
        waited = {E: {} for E in self.ENGS}
        stats = {"waits": 0}

        def run(e, E):
            for i in queues[E]:
                op = ops[i]
                if op["skip"]:
                    continue
                need = {}
                is_c = op["dma"] is None
                implied = set()
                for kind in ("raw", "oth"):
                    for d in op[kind]:
                        implied |= ops[d]["raw"]
                        implied |= ops[d]["oth"]
                for kind in ("raw", "oth"):
                    for d in op[kind]:
                        if d in implied:
                            continue
                        dop = ops[d]
                        if dop["skip"]:
                            continue
                        if is_c and dop["dma"] is None and dop["eng"] == E:
                            if E == "pe" or kind == "oth":
                                continue
                        s, v = dop["stream"], dop["val"]
                        if need.get(s, 0) < v:
                            need[s] = v
                for s, v in need.items():
                    if waited[E].get(s, 0) >= v:
                        continue
                    if s.startswith("d_"):
                        hist = dma_hist[s]
                        j = bisect.bisect_left(hist, (i, 0)) - 1
                        assert j >= 0 and hist[j][1] == v, f"partial DMA wait on {s}: want {v} issued {hist[j][1]} op {i}"
                    e.wait_ge(sems[s], v)
                    waited[E][s] = v
                    stats["waits"] += 1
                ins = op["fn"](e)
                if op["stream"] is not None:
                    ins.then_inc(sems[op["stream"]], 16 if op["dma"] is not None else 1)

        with nc.Block() as block:
            @block.tensor
            def _(e):
                run(e, "pe")

            @block.scalar
            def _(e):
                run(e, "act")

            @block.vector
            def _(e):
                run(e, "dve")

            @block.gpsimd
            def _(e):
                run(e, "pool")

            @block.sync
            def _(e):
                run(e, "sp")
        self.stats = stats


class Buf:
    def __init__(self, prog, name, gen, ap, start, end, old_keys):
        self.p = prog
        self.name = name
        self.gen = gen
        self.ap = ap
        self.start = start
        self.end = end
        self.old_keys = old_keys
        self.keys = {}

    def k(self, i=0):
        key = self.keys.get(i)
        if key is None:
            key = (self.name, self.gen, i)
            self.keys[i] = key
            if self.old_keys:
                self.p.alias([key], self.old_keys)
        return key

    def ks(self, it):
        return [self.k(i) for i in it]


class Arena:
    def __init__(self, prog, big, nwords):
        self.p = prog
        self.big = big
        self.n = nwords
        self.top = 0
        self.hi = nwords
        self.dead = []
        self.live = []
        self.live_hi = []
        self.gen = 0
        self.peak = 0

    def mark(self):
        return (self.top, len(self.live))

    def release(self, m):
        top, nl = m
        while len(self.live) > nl:
            self.dead.append(self.live.pop())
        self.top = top

    def mark_hi(self):
        return (self.hi, len(self.live_hi))

    def release_hi(self, m):
        hi, nl = m
        while len(self.live_hi) > nl:
            self.dead.append(self.live_hi.pop())
        self.hi = hi

    def alloc(self, name, shape, dtype, top=False):
        nel = 1
        for s_ in shape:
            nel *= s_
        esz = 4 if dtype in (F32, I32) else 2
        nw = (nel * esz + 3) // 4
        nw = (nw + 7) // 8 * 8
        if top:
            end = self.hi
            start = end - nw
            assert start >= self.top, f"SBUF arena overflow allocating {name} (top)"
            self.hi = start
        else:
            start = self.top
            end = start + nw
            assert end <= self.hi, f"SBUF arena overflow allocating {name}: {end} > {self.hi}"
            self.top = end
        self.peak = max(self.peak, self.top + (self.n - self.hi))
        old_keys = []
        keep = []
        for b in self.dead:
            if b.start < end and start < b.end:
                old_keys.extend(b.keys.values())
                old_keys.extend(b.old_keys)
                if not (start <= b.start and b.end <= end):
                    keep.append(b)
            else:
                keep.append(b)
        self.dead = keep
        self.gen += 1
        ap = self.big[:, start:end]
        if esz == 2:
            ap = ap.bitcast(dtype)[:, 0:nel]
        elif dtype == I32:
            ap = ap.bitcast(I32)[:, 0:nel]
        else:
            ap = ap[:, 0:nel]
        if len(shape) == 2:
            ap = ap.rearrange("p (a b) -> p a b", a=shape[0])
        elif len(shape) == 3:
            ap = ap.rearrange("p (a b c) -> p a b c", a=shape[0], b=shape[1])
        b = Buf(self.p, name, self.gen, ap, start, end, list(dict.fromkeys(old_keys)))
        (self.live_hi if top else self.live).append(b)
        return b


class Builder:
    def __init__(self, n_layers, first_layer=0, final_norm=True, debug=None):
        self.n_layers = n_layers
        self.first_layer = first_layer
        self.final_norm = final_norm
        self.debug = debug or {}
        self.dbg_outs = []

    def mm(self, out, lhsT, rhs, start, stop, r, w):
        self.p.add("pe", lambda e: e.matmul(out, lhsT=lhsT, rhs=rhs, start=start, stop=stop), r=r, w=w)

    def tr(self, out, in_, ident, r, w):
        self.p.add("pe", lambda e: e.transpose(out, in_, ident), r=r, w=w)

    def act(self, out, in_, func, r, w, bias=None, scale=None, accum_out=None, eng="act"):
        kw = {}
        if bias is not None:
            kw["bias"] = bias
        if scale is not None:
            kw["scale"] = scale
        if accum_out is not None:
            kw["accum_out"] = accum_out
        self.p.add(eng, lambda e: e.activation(out=out, in_=in_, func=func, **kw), r=r, w=w)

    def tt(self, eng, out, in0, in1, op, r, w):
        if eng == "pool" and "nopool" in self.debug.get("skip", ""):
            eng = "dve"
        self.p.add(eng, lambda e: e.tensor_tensor(out=out, in0=in0, in1=in1, op=op), r=r, w=w)

    def ts(self, eng, out, in0, s1, s2, op0, op1, r, w):
        if op1 is None:
            self.p.add(eng, lambda e: e.tensor_scalar(out=out, in0=in0, scalar1=s1, scalar2=None, op0=op0), r=r, w=w)
        else:
            self.p.add(eng, lambda e: e.tensor_scalar(out=out, in0=in0, scalar1=s1, scalar2=s2, op0=op0, op1=op1), r=r, w=w)

    def stt(self, out, in0, scalar, in1, op0, op1, r, w):
        self.p.add("dve", lambda e: e.scalar_tensor_tensor(out=out, in0=in0, scalar=scalar, in1=in1, op0=op0, op1=op1), r=r, w=w)

    def cp(self, eng, out, in_, r, w):
        if eng == "act":
            self.p.add("act", lambda e: e.activation(out=out, in_=in_, func=AF.Copy), r=r, w=w)
        else:
            self.p.add(eng, lambda e: e.tensor_copy(out=out, in_=in_), r=r, w=w)

    def memset(self, eng, ap, val, w):
        if eng == "pool" and "nopoolms" in self.debug.get("skip", ""):
            eng = "dve"
        self.p.add(eng, lambda e: e.memset(ap, val), w=w)

    def dma(self, eng, out, in_, r, w, sem, **kw):
        self.p.add(eng, lambda e: e.dma_start(out=out, in_=in_, **kw), r=r, w=w, dma=sem)

    def bc_reg(self, e):
        if getattr(self, "_bc_reg", None) is None:
            self._bc_reg = e.to_reg(XS_ROWS - 1)
        return self._bc_reg

    def dbg(self, name, ap, shape, dtype, r):
        t = self.nc.dram_tensor("dbg_" + name, list(shape), dtype, kind="ExternalOutput").ap()
        self.dma("sp", t, ap, r=r, w=[("dbgout", name)], sem="dbg_" + name)
        self.dbg_outs.append(name)
        self.out_keys.append(("dbgout", name))

    def build(self):
        nc = bass.Bass("TRN2", target_bir_lowering=False)
        self.nc = nc
        p = Prog()
        self.p = p
        nl = self.n_layers
        self.out_keys = []

        def din(name, shape, dt=F32):
            return nc.dram_tensor(name, list(shape), dt, kind="ExternalInput").ap()

        self.d_x = din("x", [S, D])
        self.d_p = din("p", [nl, S, 256])
        self.d_pos = din("positions", [S], I32)
        self.d_g_mix = din("g_mix", [nl, D])
        self.d_w_in = din("w_in", [nl, D, 5152])
        self.d_g_q = din("g_q_lat", [nl, 512])
        self.d_w_q = din("w_q_up", [nl, 512, 1536])
        self.d_g_kv = din("g_kv_lat", [nl, 256])
        self.d_w_kv = din("w_kv_up", [nl, 256, 2048])
        self.d_w_ba = din("w_branch_a", [nl, D, D])
        self.d_w_bb = din("w_branch_b", [nl, 256, D])
        self.d_w_out = din("w_out", [nl, D, D])
        self.d_g_ffn = din("g_ffn", [nl, D])
        self.d_w_rg = din("w_router_grp", [nl, D, 8])
        self.d_b_rg = din("b_router_grp", [nl, 8])
        self.d_w_re = din("w_router_exp", [nl, D, 64])
        self.d_b_re = din("b_router_exp", [nl, 64])
        self.d_w1 = din("w_exp_gate", [nl, NEXP, D, 256])
        self.d_w3 = din("w_exp_up", [nl, NEXP, D, 256])
        self.d_w2 = din("w_exp_down", [nl, NEXP, 256, D])
        self.d_g_ple = din("g_ple", [nl, D])
        self.d_w_pg = din("w_ple_gate", [nl, D, D])
        self.d_w_pp = din("w_ple_proj", [nl, 256, D])
        self.d_g_fin = din("g_final", [D])
        self.d_out = nc.dram_tensor("out", [S, D], F32, kind="ExternalOutput").ap()
        self.d_xs = [nc.dram_tensor(f"xs{l}", [XS_ROWS, D], BF16, kind="Internal").ap() for l in range(nl)]
        self.d_ys = [nc.dram_tensor(f"ys{l}", [XS_ROWS, D], F32, kind="Internal").ap() for l in range(nl)]

        with ExitStack() as stack:
            XW = NT * D
            CW = 2560
            total_words = 212800 // 4
            AW = total_words - XW - CW
            big = stack.enter_context(nc.sbuf_tensor("big", [128, total_words], F32))
            self.ps = stack.enter_context(nc.psum_tensor("ps", [128, 4096], F32))
            self.x_tok = big[:, 0:XW].rearrange("p (t d) -> p t d", t=NT)
            self.carena = Arena(p, big[:, XW:XW + CW], CW)
            self.arena = Arena(p, big[:, XW + CW:XW + CW + AW], AW)
            self.setup_consts()
            xv = self.d_x.rearrange("(t p) d -> p t d", p=128)
            for t in range(NT):
                self.dma("sp", self.x_tok[:, t, :], xv[:, t, :], r=[], w=[("x", t)], sem=f"xload{t}")
            for li in range(nl):
                if self.debug.get("stop") == "consts":
                    break
                self.layer(li)
            n_body = len(p.ops)
            if self.final_norm:
                self.final()
            else:
                ov = self.d_out.rearrange("(t p) d -> p t d", p=128)
                for t in range(NT):
                    self.dma("sp", ov[:, t, :], self.x_tok[:, t, :], r=[("x", t)], w=[("out", t)], sem=f"ost{t % 4}")
                    self.out_keys.append(("out", t))
            p.add("sp", lambda e: e.nop(), r=list(self.out_keys), w=[], noinc=True)
            mo = self.debug.get("maxops")
            p.emit(nc, stack, trunc=(int(mo), n_body) if mo else None)
        self.arena_peak = self.arena.peak
        return nc

    def bank(self, b, n=1):
        return self.ps[:, b * 512:(b + n) * 512]

    def bank_bf(self, b):
        return self.ps[:, b * 512:(b + 1) * 512].bitcast(BF16)

    def setup_consts(self):
        p = self.p
        ca = self.carena
        self.ident_bf = ca.alloc("ident_bf", [128], BF16)
        self.ident_f = ca.alloc("ident_f", [128], F32)
        self.tri2 = ca.alloc("tri2", [256], BF16)
        self.ones_f = ca.alloc("ones_f", [128], F32)
        self.ones_bf = ca.alloc("ones_bf", [128], BF16)
        self.ustrict = ca.alloc("ustrict", [128], BF16)
        self.pos_i = ca.alloc("pos_i", [4, NT], I32)
        self.pos_f = ca.alloc("pos_f", [4, NT], F32)
        self.inv_m = ca.alloc("inv_m", [16], F32)
        self.inv_d = ca.alloc("inv_d", [8], F32)
        self.cos_m = ca.alloc("cos_m", [NT, 16], F32)
        self.sin_m = ca.alloc("sin_m", [NT, 16], F32)
        self.cos_d = ca.alloc("cos_d", [3, NT, 8], F32)
        self.sin_d = ca.alloc("sin_d", [3, NT, 8], F32)
        self.neghalf = ca.alloc("neghalf", [NT], F32)
        self.ebase = ca.alloc("ebase", [NEXP], F32)
        self.negpi = ca.alloc("negpi", [1], F32)
        CK = ("consts",)

        SK = self.debug.get("skip", "")
        for _i in range(int(self.debug.get("pad") or 0)):
            self.memset("dve", self.ones_bf.ap, 1.0, w=[("c_onesbf",)])
        def iden(buf, key):
            self.memset("pool", buf.ap, 1.0, w=[key])
            if "sel" in SK:
                return
            p.add("pool", lambda e: e.affine_select(out=buf.ap, in_=buf.ap, pattern=[[1, 128]], compare_op=ALU.is_equal,
                                                     fill=0.0, base=0, channel_multiplier=-1), r=[key], w=[key])
        iden(self.ident_bf, ("c_identbf",))
        iden(self.ident_f, ("c_identf",))
        t2 = self.tri2.ap
        self.memset("pool", t2, 1.0, w=[("c_tri",)])
        if "sel" not in SK:
          p.add("pool", lambda e: e.affine_select(out=t2[:, 0:128], in_=t2[:, 0:128], pattern=[[1, 128]], compare_op=ALU.is_ge,
                                                 fill=0.0, base=0, channel_multiplier=-1), r=[("c_tri",)], w=[("c_tri",)])
        if "sel" not in SK:
          p.add("pool", lambda e: e.affine_select(out=t2[:, 128:256], in_=t2[:, 128:256], pattern=[[-1, 128]], compare_op=ALU.is_ge,
                                                 fill=0.0, base=0, channel_multiplier=1), r=[("c_tri",)], w=[("c_tri",)])
        us = self.ustrict.ap
        self.memset("pool", us, 1.0, w=[("c_us",)])
        if "sel" not in SK:
          p.add("pool", lambda e: e.affine_select(out=us, in_=us, pattern=[[1, 128]], compare_op=ALU.is_gt,
                                                 fill=0.0, base=0, channel_multiplier=-1), r=[("c_us",)], w=[("c_us",)])
        self.memset("pool", self.ones_f.ap, 1.0, w=[("c_ones",)])
        self.memset("pool", self.ones_bf.ap, 1.0, w=[("c_onesbf",)])
        self.memset("pool", self.neghalf.ap, -0.5, w=[("c_nh",)])
        self.memset("pool", self.negpi.ap, -PI, w=[("c_negpi",)])
        eb = self.ebase.ap
        if "iota" not in SK:
          p.add("pool", lambda e: e.iota(eb, pattern=[[CAP, NEXP]], base=0, channel_multiplier=0,
                                        allow_small_or_imprecise_dtypes=True), w=[("c_ebase",)])
        for j in range(16):
            self.memset("pool", self.inv_m.ap[:, j:j + 1], float(np.float32(10000.0) ** np.float32(-j * 2.0 / 32)), w=[("c_invm",)])
        for j in range(8):
            self.memset("pool", self.inv_d.ap[:, j:j + 1], float(np.float32(500000.0) ** np.float32(-j * 2.0 / 16)), w=[("c_invd",)])
        pv = [self.d_pos.rearrange("(t p) -> p t", p=128)]
        for d in (1, 4, 16):
            nb = NT // d
            pv.append(self.d_pos.rearrange("(j p r) -> p r j", p=128, r=d, j=nb))
        for gi in range(4):
            if "pos" in SK:
                break
            if gi == 0:
                dst = self.pos_i.ap[:, gi, :]
            else:
                d = (1, 4, 16)[gi - 1]
                dst = self.pos_i.ap[:, gi, :].rearrange("p (r j) -> p r j", r=d)
            self.dma("sp", dst, pv[gi], r=[], w=[("c_posi", gi)], sem="posload", allow_slow_non_contiguous=True)
        self.cp("dve", self.pos_f.ap, self.pos_i.ap, r=[("c_posi", g) for g in range(4)], w=[("c_posf",)])

        a = self.arena
        msc = a.mark()
        angb = a.alloc("ang", [NT * 16], F32)
        ab = a.alloc("ang_a", [NT * 16], F32)
        kib = a.alloc("ang_ki", [NT * 16], I32)
        kfb = a.alloc("ang_kf", [NT * 16], F32)
        mb = a.alloc("ang_m", [NT * 16], F32)

        def table(dst_cos, dst_sin, posf, inv, nf):
            n = NT * nf
            v3 = lambda b_: b_.ap[:, 0:n].rearrange("p (t j) -> p t j", t=NT)
            ang, aa, ki, kf, mm_ = v3(angb), v3(ab), v3(kib), v3(kfb), v3(mb)
            a0 = posf.unsqueeze(2).to_broadcast([128, NT, nf])
            a1 = inv.unsqueeze(1).to_broadcast([128, NT, nf])
            self.tt("dve", ang, a0, a1, ALU.mult, r=[("c_posf",), ("c_invm",), ("c_invd",)], w=[angb.k()])
            for dst, shift in ((dst_sin, 0.0), (dst_cos, 0.5 * PI)):
                self.ts("dve", aa, ang, shift, None, ALU.add, None, r=[angb.k()], w=[ab.k()])
                self.ts("dve", kf, aa, 1.0 / (2 * PI), None, ALU.mult, None, r=[ab.k()], w=[kfb.k()])
                self.cp("dve", ki, kf, r=[kfb.k()], w=[kib.k()])
                self.cp("dve", kf, ki, r=[kib.k()], w=[kfb.k()])
                self.stt(aa, kf, -2 * PI, aa, ALU.mult, ALU.add, r=[kfb.k(), ab.k()], w=[ab.k()])
                self.ts("dve", mm_, aa, PI, None, ALU.is_gt, None, r=[ab.k()], w=[mb.k()])
                self.stt(aa, mm_, -2 * PI, aa, ALU.mult, ALU.add, r=[mb.k(), ab.k()], w=[ab.k()])
                self.ts("dve", mm_, aa, -PI, None, ALU.is_lt, None, r=[ab.k()], w=[mb.k()])
                self.stt(aa, mm_, 2 * PI, aa, ALU.mult, ALU.add, r=[mb.k(), ab.k()], w=[ab.k()])
                self.ts("dve", aa, aa, 3.14159, -3.14159, ALU.min, ALU.max, r=[ab.k()], w=[ab.k()])
                self.act(dst, aa, AF.Copy if "nosin" in SK else AF.Sin, r=[ab.k()], w=[("c_tab",)])
        if "tab" not in SK:
            table(self.cos_m.ap, self.sin_m.ap, self.pos_f.ap[:, 0, :], self.inv_m.ap, 16)
        for g in range(3):
            if "tab" in SK:
                break
            table(self.cos_d.ap[:, g], self.sin_d.ap[:, g], self.pos_f.ap[:, g + 1, :], self.inv_d.ap, 8)
        a.release(msc)
        if "consts" in self.debug:
            self.dbg("cos_m", self.cos_m.ap, [128, NT, 16], F32, r=[("c_tab",)])
            self.dbg("sin_d", self.sin_d.ap, [128, 3, NT, 8], F32, r=[("c_tab",)])
            self.dbg("tri2", self.tri2.ap, [128, 256], BF16, r=[("c_tri",)])
            self.dbg("ident", self.ident_f.ap, [128, 128], F32, r=[("c_identf",)])
            self.dbg("ebase", self.ebase.ap, [128, 64], F32, r=[("c_ebase",)])
            self.dbg("ustrict", self.ustrict.ap, [128, 128], BF16, r=[("c_us",)])
        self.CONST_KEYS = [("c_identbf",), ("c_identf",), ("c_tri",), ("c_us",), ("c_ones",), ("c_onesbf",), ("c_nh",),
                           ("c_ebase",), ("c_tab",)]

    def x_rstd(self, tag):
        a = self.arena
        ss = a.alloc("ss" + tag, [NT], F32)
        rstd = a.alloc("rstd" + tag, [NT], F32)
        mj = a.mark()
        junk = a.alloc("junk" + tag, [D], BF16)
        for t in range(NT):
            self.act(junk.ap, self.x_tok[:, t, :], AF.Square, r=[("x", t)], w=[ss.k(t), junk.k()], accum_out=ss.ap[:, t:t + 1])
        a.release(mj)
        self.ts("dve", ss.ap, ss.ap, 1.0 / D, EPS, ALU.mult, ALU.add, r=ss.ks(range(NT)), w=[ss.k("ms")])
        self.act(ss.ap, ss.ap, AF.Sqrt, r=[ss.k("ms")], w=[ss.k("ms")])
        p_ = self.p
        p_.add("dve", lambda e: e.reciprocal(out=rstd.ap, in_=ss.ap), r=[ss.k("ms")], w=[rstd.k()])
        return rstd

    def gain_T(self, tag, d_g, li, nchunk, mode="nat"):
        gT = self.arena.alloc("gT" + tag, [nchunk], F32)
        if mode == "nat":
            src = d_g[li].rearrange("(c p) -> p c", p=128)
        else:
            src = d_g[li].rearrange("(p c) -> p c", p=128)
        self.dma("sp", gT.ap, src, r=[], w=[gT.k()], sem="gT" + tag, allow_slow_non_contiguous=True)
        return gT

    def norm_T_tile(self, t, rstd, gT, htok, dstT, dst_cols, psb, dst_keys):
        self.act(htok.ap, self.x_tok[:, t, :], AF.Copy, r=[("x", t), rstd.k()], w=[htok.k()], scale=rstd.ap[:, t:t + 1])
        pb = self.bank_bf(psb)
        for c in range(8):
            self.tr(pb[:, c * 128:(c + 1) * 128], htok.ap[:, c * 128:(c + 1) * 128], self.ident_bf.ap,
                    r=[htok.k(), ("c_identbf",)], w=[("ps", psb)])
        self.tt("dve", dstT[:, :, dst_cols], pb.rearrange("p (c t) -> p c t", c=8),
                gT.ap.unsqueeze(2).to_broadcast([128, 8, 128]), ALU.mult, r=[("ps", psb), gT.k()], w=dst_keys)

    def layer(self, li):
        L = self.first_layer + li
        a = self.arena
        m_layer = a.mark()
        self.oaT = a.alloc("oaT", [4 * S], F32)
        self.oaT_ap = self.oaT.ap.bitcast(BF16).rearrange("p (c s) -> p c s", c=8)
        self.acc_ap = self.oaT.ap.rearrange("p (h s) -> p h s", h=4)
        self.obT = a.alloc("obT", [2, S], BF16)
        self.mixer(li)
        a.release(m_layer)
        if self.debug.get("stop") in ("p1", "dil", "lat", "mla", "mixer"):
            return
        m = a.mark()
        self.moe(li)
        a.release(m)
        if self.debug.get("stop") == "moe":
            return
        m = a.mark()
        self.ple(li)
        a.release(m)

    def mixer(self, li):
        p = self.p
        a = self.arena
        rstd = self.x_rstd("mix")
        gT = self.gain_T("mix", self.d_g_mix, li, 8)
        self.rstd_mix = rstd
        self.gT_mix = gT
        m_lat = a.mark_hi()
        m1 = a.mark()
        hT = a.alloc("hT", [8, S], BF16)
        htok = [a.alloc(f"htok{i}", [D], BF16) for i in range(2)]
        for t in range(NT):
            self.norm_T_tile(t, rstd, gT, htok[t % 2], hT.ap, slice(t * 128, (t + 1) * 128), 6 + t % 2, [hT.k(t)])
        if "hT" in self.debug:
            self.dbg("hT", hT.ap, [128, 8, S], BF16, r=hT.ks(range(NT)))
        if self.debug.get("stop") == "p1":
            a.release(m1)
            return
        self.dilated(li, hT)
        if self.debug.get("stop") == "dil":
            a.release(m1)
            return
        hqT = a.alloc("hqT", [4, S], BF16, top=True)
        hkvT = a.alloc("hkvT", [2, S], BF16, top=True)
        krot = a.alloc("krot", [NT, 32], BF16, top=True)
        self.latents(li, hT, hqT, hkvT, krot)
        a.release(m1)
        if self.debug.get("stop") == "lat":
            a.release_hi(m_lat)
            return
        self.p.alias(self.oaT.ks([(c, q, i) for c in range(8) for q in range(4) for i in range(2)]), self.oaT.ks([("acc", hh) for hh in range(4)]))
        self.mla(li, hqT, hkvT, krot)
        a.release_hi(m_lat)
        if self.debug.get("stop") == "mla":
            return
        self.merge(li)

    def dilated(self, li, hT):
        p = self.p
        a = self.arena
        m0 = a.mark()
        acc_ap = self.acc_ap
        akey = lambda hh: self.oaT.k(("acc", hh))
        wd = a.alloc("wdil", [8, 768], BF16)
        qkT = a.alloc("dqkT", [4, S], BF16)
        dv = a.alloc("dv", [NT, 4, 66], BF16)
        qk_tok = [a.alloc(f"dqk_tok{i}", [8, 64], BF16) for i in range(2)]
        tmp = [a.alloc(f"dtmp{i}", [8, 8], F32) for i in range(4)]
        pT = [a.alloc(f"dpT{i}", [256], BF16) for i in range(3)]
        rden = a.alloc("drden", [512], F32)
        w_in = self.d_w_in[li]
        self.memset("pool", dv.ap[:, :, :, 64:66], 1.0, w=[dv.k("ones")])
        for g, d in enumerate((1, 4, 16)):
            if "dil_g1" in self.debug.get("skip", "") and g > 0:
                break
            nb = NT // d
            c0 = 800 + g * 768
            self.dma("pool", wd.ap, w_in[:, c0:c0 + 768].rearrange("(c p) w -> p c w", p=128), r=[], w=[wd.k()], sem="wdil")
            cosg = self.cos_d.ap[:, g]
            sing = self.sin_d.ap[:, g]
            for u in range(NT):
                r_, j_ = divmod(u, nb)
                st = r_ + d * 128 * j_
                tok = slice(st, st + d * 127 + 1, d)
                pa, pb_, pt = 0 + (u % 2) * 3, 1 + (u % 2) * 3, 2 + (u % 2) * 3
                for c in range(8):
                    self.mm(self.bank(pa), hT.ap[:, c, tok], wd.ap[:, c, 0:512], c == 0, c == 7,
                            r=hT.ks(range(NT)) + [wd.k()], w=[("ps", pa)])
                for c in range(8):
                    self.mm(self.bank(pb_)[:, 0:256], hT.ap[:, c, tok], wd.ap[:, c, 512:768], c == 0, c == 7,
                            r=hT.ks(range(NT)) + [wd.k()], w=[("ps", pb_)])
                qt = qk_tok[u % 2]
                pav = self.bank(pa).rearrange("p (h e) -> p h e", h=8)
                self.cp("act", qt.ap[:, :, 16:64], pav[:, :, 16:64], r=[("ps", pa)], w=[qt.k("rest")])
                cb = cosg[:, u, :].unsqueeze(1).to_broadcast([128, 8, 8])
                sb = sing[:, u, :].unsqueeze(1).to_broadcast([128, 8, 8])
                x1 = pav[:, :, 0:8]
                x2 = pav[:, :, 8:16]
                t0, t1, t2, t3 = [x.ap for x in tmp]
                ck = [("c_tab",)]
                self.tt("dve", t0, x1, cb, ALU.mult, r=[("ps", pa)] + ck, w=[tmp[0].k()])
                self.tt("dve", t1, x2, sb, ALU.mult, r=[("ps", pa)] + ck, w=[tmp[1].k()])
                self.tt("dve", t2, x1, sb, ALU.mult, r=[("ps", pa)] + ck, w=[tmp[2].k()])
                self.tt("dve", t3, x2, cb, ALU.mult, r=[("ps", pa)] + ck, w=[tmp[3].k()])
                self.tt("pool", qt.ap[:, :, 0:8], t0, t1, ALU.subtract, r=[tmp[0].k(), tmp[1].k()], w=[qt.k("r1")])
                self.tt("pool", qt.ap[:, :, 8:16], t2, t3, ALU.add, r=[tmp[2].k(), tmp[3].k()], w=[qt.k("r2")])
                self.cp("act", dv.ap[:, u, :, 0:64], self.bank(pb_)[:, 0:256].rearrange("p (h e) -> p h e", h=4),
                        r=[("ps", pb_)], w=[dv.k(u)])
                ptb = self.bank_bf(pt)
                qflat = qt.ap.rearrange("p h e -> p (h e)")
                for i4 in range(4):
                    self.tr(ptb[:, i4 * 128:(i4 + 1) * 128], qflat[:, i4 * 128:(i4 + 1) * 128], self.ident_bf.ap,
                            r=[qt.k("rest"), qt.k("r1"), qt.k("r2"), ("c_identbf",)], w=[("ps", pt)])
                self.cp("dve", qkT.ap[:, :, u * 128:(u + 1) * 128], ptb[:, 0:512].rearrange("p (c t) -> p c t", c=4),
                        r=[("ps", pt)], w=[qkT.k(u)])
            for hh in range(4):
                if "dil_noattn" in self.debug.get("skip", ""):
                    break
                pp, pb0 = hh // 2, (hh % 2) * 64
                for r_ in range(d):
                    prevP = None
                    for j_ in range(nb):
                        u = r_ * nb + j_
                        ncols = 256 if j_ + 1 < nb else 128
                        sb_ = 6 + (u % 2)
                        self.mm(self.bank(sb_)[:, 0:ncols], qkT.ap[pb0:pb0 + 64, 2 + pp, u * 128:(u + 1) * 128],
                                qkT.ap[pb0:pb0 + 64, pp, u * 128:u * 128 + ncols], True, True,
                                r=[qkT.k(u), qkT.k(u + 1)] if ncols == 256 else [qkT.k(u)], w=[("ps", sb_)])
                        P = pT[u % 3]
                        self.act(P.ap[:, 0:ncols], self.bank(sb_)[:, 0:ncols], AF.Exp, r=[("ps", sb_)], w=[P.k()], scale=0.125)
                        self.tt("pool", P.ap[:, 0:ncols], P.ap[:, 0:ncols], self.tri2.ap[:, 0:ncols], ALU.mult,
                                r=[P.k(), ("c_tri",)], w=[P.k()])
                        ob = 0 + (u // 4) % 2
                        oc = (u % 4) * 128
                        oap = self.bank(ob)[0:65, oc:oc + 128]
                        if prevP is not None:
                            self.mm(oap, dv.ap[:, u - 1, hh, 0:65], prevP.ap[:, 128:256], True, False,
                                    r=[dv.k(u - 1), dv.k("ones"), prevP.k()], w=[("ps", ob)])
                        self.mm(oap, dv.ap[:, u, hh, 0:65], P.ap[:, 0:128], prevP is None, True,
                                r=[dv.k(u), dv.k("ones"), P.k()], w=[("ps", ob)])
                        prevP = P
                        st = r_ + d * 128 * j_
                        dst = acc_ap[0:65, hh, st:st + d * 127 + 1:d]
                        if g == 0:
                            self.cp("dve", dst, oap, r=[("ps", ob)], w=[akey(hh)])
                        else:
                            self.tt("dve", dst, oap, dst, ALU.add, r=[("ps", ob), akey(hh)], w=[akey(hh)])
        for hh in range(4):
            if "dil_noattn" in self.debug.get("skip", ""):
                break
            pp, pb0 = hh // 2, (hh % 2) * 64
            akeys = [akey(hh)]
            for tc in range(4):
                cs = slice(tc * 512, (tc + 1) * 512)
                p.add("dve", (lambda e, cs=cs, hh=hh: e.reciprocal(out=rden.ap[64:65, :], in_=acc_ap[64:65, hh, cs])), r=akeys, w=[rden.k()])
                bb = 2 + (tc % 2)
                self.mm(self.bank(bb), self.ones_f.ap[64:65, :], rden.ap[64:65, :], True, True, r=[rden.k(), ("c_ones",)], w=[("ps", bb)])
                self.tt("dve", self.obT.ap[pb0:pb0 + 64, pp, cs], acc_ap[0:64, hh, cs], self.bank(bb)[0:64, :], ALU.mult,
                        r=akeys + [("ps", bb)], w=[self.obT.k((pp, tc))])
        if "obT" in self.debug:
            self.dbg("obT", self.obT.ap, [128, 2, S], BF16, r=self.obT.ks([(pp, tc) for pp in range(2) for tc in range(4)]))
        a.release(m0)

    def latents(self, li, hT, hqT, hkvT, krot):
        p = self.p
        a = self.arena
        m0 = a.mark()
        wl = a.alloc("wlat", [8, 800], BF16)
        gq = self.gain_T("q", self.d_g_q, li, 4)
        gkv = self.gain_T("kv", self.d_g_kv, li, 2)
        st = a.alloc("lstat", [NT, 4], F32)
        junk = a.alloc("ljunk", [512], BF16)
        hq_tok = [a.alloc(f"hq_tok{i}", [768], BF16) for i in range(2)]
        tmp = [a.alloc(f"ltmp{i}", [16], F32) for i in range(4)]
        self.dma("pool", wl.ap, self.d_w_in[li][:, 0:800].rearrange("(c p) w -> p c w", p=128), r=[], w=[wl.k()], sem="wlat")
        for t in range(NT):
            pa, pb_, pt = 0 + (t % 2) * 3, 1 + (t % 2) * 3, 2 + (t % 2) * 3
            tok = slice(t * 128, (t + 1) * 128)
            for c in range(8):
                self.mm(self.bank(pa), hT.ap[:, c, tok], wl.ap[:, c, 0:512], c == 0, c == 7, r=[hT.k(t), wl.k()], w=[("ps", pa)])
            for c in range(8):
                self.mm(self.bank(pb_)[:, 0:288], hT.ap[:, c, tok], wl.ap[:, c, 512:800], c == 0, c == 7, r=[hT.k(t), wl.k()], w=[("ps", pb_)])
            self.act(junk.ap, self.bank(pa), AF.Square, r=[("ps", pa)], w=[st.k((t, 0))], accum_out=st.ap[:, t, 0:1])
            self.act(junk.ap[:, 0:256], self.bank(pb_)[:, 0:256], AF.Square, r=[("ps", pb_)], w=[st.k((t, 1))], accum_out=st.ap[:, t, 1:2])
            self.ts("dve", st.ap[:, t, 0:1], st.ap[:, t, 0:1], 1.0 / 512, EPS, ALU.mult, ALU.add, r=[st.k((t, 0))], w=[st.k((t, 0))])
            self.ts("dve", st.ap[:, t, 1:2], st.ap[:, t, 1:2], 1.0 / 256, EPS, ALU.mult, ALU.add, r=[st.k((t, 1))], w=[st.k((t, 1))])
            self.act(st.ap[:, t, 0:2], st.ap[:, t, 0:2], AF.Sqrt, r=[st.k((t, 0)), st.k((t, 1))], w=[st.k((t, 0)), st.k((t, 1))])
            p.add("dve", (lambda e, t=t: e.reciprocal(out=st.ap[:, t, 2:4], in_=st.ap[:, t, 0:2])), r=[st.k((t, 0)), st.k((t, 1))], w=[st.k((t, 2))])
            hq = hq_tok[t % 2]
            self.ts("dve", hq.ap[:, 0:512], self.bank(pa), st.ap[:, t, 2:3], None, ALU.mult, None, r=[("ps", pa), st.k((t, 2))], w=[hq.k(0)])
            self.ts("dve", hq.ap[:, 512:768], self.bank(pb_)[:, 0:256], st.ap[:, t, 3:4], None, ALU.mult, None, r=[("ps", pb_), st.k((t, 2))], w=[hq.k(1)])
            x1 = self.bank(pb_)[:, 256:272]
            x2 = self.bank(pb_)[:, 272:288]
            cb = self.cos_m.ap[:, t, :]
            sb = self.sin_m.ap[:, t, :]
            ck = [("c_tab",), ("ps", pb_)]
            self.tt("dve", tmp[0].ap, x1, cb, ALU.mult, r=ck, w=[tmp[0].k()])
            self.tt("dve", tmp[1].ap, x2, sb, ALU.mult, r=ck, w=[tmp[1].k()])
            self.tt("dve", tmp[2].ap, x1, sb, ALU.mult, r=ck, w=[tmp[2].k()])
            self.tt("dve", tmp[3].ap, x2, cb, ALU.mult, r=ck, w=[tmp[3].k()])
            self.tt("pool", krot.ap[:, t, 0:16], tmp[0].ap, tmp[1].ap, ALU.subtract, r=[tmp[0].k(), tmp[1].k()], w=[krot.k((t, 0))])
            self.tt("pool", krot.ap[:, t, 16:32], tmp[2].ap, tmp[3].ap, ALU.add, r=[tmp[2].k(), tmp[3].k()], w=[krot.k((t, 1))])
            ptb = self.bank_bf(pt)
            for c in range(6):
                self.tr(ptb[:, c * 128:(c + 1) * 128], hq.ap[:, c * 128:(c + 1) * 128], self.ident_bf.ap,
                        r=[hq.k(0), hq.k(1), ("c_identbf",)], w=[("ps", pt)])
            self.tt("dve", hqT.ap[:, :, tok], ptb[:, 0:512].rearrange("p (c t) -> p c t", c=4),
                    gq.ap.unsqueeze(2).to_broadcast([128, 4, 128]), ALU.mult, r=[("ps", pt), gq.k()], w=[hqT.k(t)])
            self.tt("dve", hkvT.ap[:, :, tok], ptb[:, 512:768].rearrange("p (c t) -> p c t", c=2),
                    gkv.ap.unsqueeze(2).to_broadcast([128, 2, 128]), ALU.mult, r=[("ps", pt), gkv.k()], w=[hkvT.k(t)])
        if "hqT" in self.debug:
            self.dbg("hqT", hqT.ap, [128, 4, S], BF16, r=hqT.ks(range(NT)))
            self.dbg("hkvT", hkvT.ap, [128, 2, S], BF16, r=hkvT.ks(range(NT)))
            self.dbg("krot", krot.ap, [128, NT, 32], BF16, r=krot.ks([(t, i) for t in range(NT) for i in range(2)]))
        a.release(m0)

    def mla(self, li, hqT, hkvT, krot):
        p = self.p
        a = self.arena
        m0 = a.mark()
        G = 4
        qkT = a.alloc("qkT", [2 * G, S], BF16)
        v = a.alloc("v", [NT, G, 96], BF16)
        wq = a.alloc("wq", [4, G * 96], BF16)
        wkv = a.alloc("wkv", [2, G * 128], BF16)
        q_tok = [a.alloc(f"q_tok{i}", [2 * G, 96], BF16) for i in range(2)]
        tmp = [a.alloc(f"mtmp{i}", [G, 16], F32) for i in range(4)]
        pT = [a.alloc(f"pT{i}", [1024], BF16) for i in range(2)]
        rden = [a.alloc(f"rden{i}", [512], F32) for i in range(2)]
        scale = 96.0 ** -0.5
        krot_keys = krot.ks([(t, i) for t in range(NT) for i in range(2)])
        cnt = 0
        for gg in range(16 // G):
            self.dma("pool", wq.ap, self.d_w_q[li][:, gg * G * 96:(gg + 1) * G * 96].rearrange("(c p) w -> p c w", p=128), r=[], w=[wq.k()], sem="wq")
            self.dma("pool", wkv.ap, self.d_w_kv[li][:, gg * G * 128:(gg + 1) * G * 128].rearrange("(c p) w -> p c w", p=128), r=[], w=[wkv.k()], sem="wkv")
            self.memset("pool", v.ap[:, :, :, 64:96], 1.0, w=[v.k("ones")])
            for t in range(NT):
                pa, pb_, pt = 0 + (t % 2) * 3, 1 + (t % 2) * 3, 2 + (t % 2) * 3
                tok = slice(t * 128, (t + 1) * 128)
                for c in range(4):
                    self.mm(self.bank(pa)[:, 0:G * 96], hqT.ap[:, c, tok], wq.ap[:, c, :], c == 0, c == 3, r=[hqT.k(t), wq.k()], w=[("ps", pa)])
                for c in range(2):
                    self.mm(self.bank(pb_), hkvT.ap[:, c, tok], wkv.ap[:, c, :], c == 0, c == 1, r=[hkvT.k(t), wkv.k()], w=[("ps", pb_)])
                qt = q_tok[t % 2]
                qv = self.bank(pa)[:, 0:G * 96].rearrange("p (h e) -> p h e", h=G)
                kvv = self.bank(pb_).rearrange("p (h e) -> p h e", h=G)
                self.cp("act", qt.ap[:, 0:G, 0:64], qv[:, :, 0:64], r=[("ps", pa)], w=[qt.k("qn")])
                self.cp("act", qt.ap[:, G:2 * G, 0:64], kvv[:, :, 0:64], r=[("ps", pb_)], w=[qt.k("kn")])
                self.cp("dve", v.ap[:, t, :, 0:64], kvv[:, :, 64:128], r=[("ps", pb_)], w=[v.k(t)])
                self.cp("pool", qt.ap[:, G:2 * G, 64:96], krot.ap[:, t, :].unsqueeze(1).to_broadcast([128, G, 32]), r=krot_keys, w=[qt.k("kr")])
                cb = self.cos_m.ap[:, t, :].unsqueeze(1).to_broadcast([128, G, 16])
                sb = self.sin_m.ap[:, t, :].unsqueeze(1).to_broadcast([128, G, 16])
                x1 = qv[:, :, 64:80]
                x2 = qv[:, :, 80:96]
                ck = [("c_tab",), ("ps", pa)]
                self.tt("dve", tmp[0].ap, x1, cb, ALU.mult, r=ck, w=[tmp[0].k()])
                self.tt("dve", tmp[1].ap, x2, sb, ALU.mult, r=ck, w=[tmp[1].k()])
                self.tt("dve", tmp[2].ap, x1, sb, ALU.mult, r=ck, w=[tmp[2].k()])
                self.tt("dve", tmp[3].ap, x2, cb, ALU.mult, r=ck, w=[tmp[3].k()])
                self.tt("pool", qt.ap[:, 0:G, 64:80], tmp[0].ap, tmp[1].ap, ALU.subtract, r=[tmp[0].k(), tmp[1].k()], w=[qt.k("r1")])
                self.tt("pool", qt.ap[:, 0:G, 80:96], tmp[2].ap, tmp[3].ap, ALU.add, r=[tmp[2].k(), tmp[3].k()], w=[qt.k("r2")])
                ptb = self.bank_bf(pt)
                for hx in range(2 * G):
                    self.tr(ptb[0:96, hx * 128:(hx + 1) * 128], qt.ap[:, hx, :], self.ident_bf.ap,
                            r=[qt.k("qn"), qt.k("kn"), qt.k("kr"), qt.k("r1"), qt.k("r2"), ("c_identbf",)], w=[("ps", pt)])
                self.cp("dve", qkT.ap[0:96, :, tok], ptb[0:96, :].rearrange("p (c t) -> p c t", c=2 * G), r=[("ps", pt)], w=[qkT.k(t)])
            if "dqkT" in self.debug:
                pass
            if "qkT" in self.debug and gg == 0:
                self.dbg("qkT", qkT.ap, [128, 2 * G, S], BF16, r=qkT.ks(range(NT)))
                self.dbg("v", v.ap, [128, NT, G, 96], BF16, r=v.ks(list(range(NT)) + ["ones"]))
            steps = [(hl, qc, kp) for hl in range(G) for qc in range(4) for kp in range((4 * qc + 4) // 2)]

            def qk(i):
                hl, qc, kp = steps[i]
                sbk = 2 * (i % 2)
                qkeys = qkT.ks(range(4 * qc, 4 * qc + 4))
                for half in range(2):
                    kt = 2 * kp + half
                    r_ = max(0, kt - 4 * qc)
                    self.mm(self.bank(sbk + half)[:, 128 * r_:512], qkT.ap[0:96, G + hl, kt * 128:(kt + 1) * 128],
                            qkT.ap[0:96, hl, qc * 512 + 128 * r_:(qc + 1) * 512], True, True,
                            r=qkeys + [qkT.k(kt)], w=[("ps", sbk + half)])

            def expm(i):
                hl, qc, kp = steps[i]
                sbk = 2 * (i % 2)
                P = pT[i % 2]
                self.act(P.ap, self.bank(sbk, 2), AF.Exp, r=[("ps", sbk), ("ps", sbk + 1)], w=[P.k()], scale=scale)
                for half in range(2):
                    kt = 2 * kp + half
                    r_ = kt - 4 * qc
                    if r_ >= 0:
                        dg = P.ap[:, half * 512 + 128 * r_: half * 512 + 128 * r_ + 128]
                        self.tt("pool", dg, dg, self.tri2.ap[:, 0:128], ALU.mult, r=[P.k(), ("c_tri",)], w=[P.k()])

            def pv(i):
                hl, qc, kp = steps[i]
                h = gg * G + hl
                cc, pb0 = h // 2, (h % 2) * 64
                nk = 4 * qc + 4
                hq = hl * 4 + qc
                ob = 4 + hq % 2
                o_ps = self.bank(ob)
                P = pT[i % 2]
                for half in range(2):
                    kt = 2 * kp + half
                    r_ = max(0, kt - 4 * qc)
                    self.mm(o_ps[0:96, 128 * r_:512], v.ap[:, kt, hl, 0:96], P.ap[:, half * 512 + 128 * r_:(half + 1) * 512],
                            kt == 0, kt == nk - 1, r=[v.k(kt), v.k("ones"), P.k()], w=[("ps", ob)])
                if kp == nk // 2 - 1:
                    rd = rden[hq % 2]
                    qcols = slice(qc * 512, (qc + 1) * 512)
                    p.add("dve", (lambda e, rd=rd, o_ps=o_ps: e.reciprocal(out=rd.ap[0:32, :], in_=o_ps[64:96, :])), r=[("ps", ob)], w=[rd.k(0)])
                    p.add("dve", (lambda e, rd=rd, o_ps=o_ps: e.reciprocal(out=rd.ap[32:64, :], in_=o_ps[64:96, :])), r=[("ps", ob)], w=[rd.k(1)])
                    self.tt("dve", self.oaT_ap[pb0:pb0 + 64, cc, qcols], o_ps[0:64, :], rd.ap[0:64, :], ALU.mult,
                            r=[("ps", ob), rd.k(0), rd.k(1)], w=[self.oaT.k((cc, qc, h % 2))])

            for i in range(len(steps)):
                qk(i)
                if i >= 1:
                    pv(i - 1)
                expm(i)
            pv(len(steps) - 1)
        if "oaT" in self.debug:
            self.dbg("oaT", self.oaT_ap, [128, 8, S], BF16, r=self.oaT.ks([(c, q, i) for c in range(8) for q in range(4) for i in range(2)]))
        a.release(m0)

    def merge(self, li):
        p = self.p
        a = self.arena
        m0 = a.mark()
        wba = a.alloc("wba", [8, D], BF16)
        wbb = a.alloc("wbb", [2, D], BF16)
        wg = a.alloc("wg", [8, 2 * D], BF16)
        wo = a.alloc("wo", [8, D], BF16)
        hTc = a.alloc("hTc", [8, 512], BF16)
        mT = a.alloc("mT", [8, 512], BF16)
        htok = [a.alloc("mhtok", [D], BF16)] * 2
        sa = a.alloc("sa", [512], BF16)
        sb = a.alloc("sb", [512], BF16)
        m1 = a.alloc("m1", [512], F32)
        m2 = a.alloc("m2", [512], F32)
        self.dma("pool", wba.ap, self.d_w_ba[li].rearrange("(c p) w -> p c w", p=128), r=[], w=[wba.k()], sem="wba")
        self.dma("pool", wbb.ap, self.d_w_bb[li].rearrange("(c p) w -> p c w", p=128), r=[], w=[wbb.k()], sem="wbb")
        for hf in range(2):
            self.dma("pool", wg.ap[:, :, hf * D:(hf + 1) * D], self.d_w_in[li][:, 3104 + hf * D:3104 + (hf + 1) * D].rearrange("(c p) w -> p c w", p=128),
                     r=[], w=[wg.k(hf)], sem=f"wg{hf}")
        self.dma("pool", wo.ap, self.d_w_out[li].rearrange("(c p) w -> p c w", p=128), r=[], w=[wo.k()], sem="wo")
        oa_keys = lambda tc: self.oaT.ks([(c, tc, i) for c in range(8) for i in range(2)])
        ob_keys = lambda tc: self.obT.ks([(pp, tc) for pp in range(2)])
        for tc in range(4):
            cs = slice(tc * 512, (tc + 1) * 512)
            for tl in range(4):
                t = tc * 4 + tl
                self.norm_T_tile(t, self.rstd_mix, self.gT_mix, htok[t % 2], hTc.ap, slice(tl * 128, (tl + 1) * 128), 7, [hTc.k(tl)])
            for f in range(8):
                fs = slice(f * 128, (f + 1) * 128)
                for c in range(8):
                    self.mm(self.bank(0), wba.ap[:, c, fs], self.oaT_ap[:, c, cs], c == 0, c == 7, r=[wba.k()] + oa_keys(tc), w=[("ps", 0)])
                for c in range(2):
                    self.mm(self.bank(1), wbb.ap[:, c, fs], self.obT.ap[:, c, cs], c == 0, c == 1, r=[wbb.k()] + ob_keys(tc), w=[("ps", 1)])
                for c in range(8):
                    self.mm(self.bank(2), wg.ap[:, c, fs], hTc.ap[:, c, :], c == 0, c == 7, r=[wg.k(0)] + hTc.ks(range(4)), w=[("ps", 2)])
                for c in range(8):
                    self.mm(self.bank(3), wg.ap[:, c, D + f * 128:D + (f + 1) * 128], hTc.ap[:, c, :], c == 0, c == 7, r=[wg.k(1)] + hTc.ks(range(4)), w=[("ps", 3)])
                self.act(sa.ap, self.bank(2), AF.Sigmoid, r=[("ps", 2)], w=[sa.k()])
                self.act(sb.ap, self.bank(3), AF.Sigmoid, r=[("ps", 3)], w=[sb.k()])
                self.tt("dve", m1.ap, self.bank(0), sa.ap, ALU.mult, r=[("ps", 0), sa.k()], w=[m1.k()])
                self.tt("dve", m2.ap, self.bank(1), sb.ap, ALU.mult, r=[("ps", 1), sb.k()], w=[m2.k()])
                self.tt("pool", mT.ap[:, f, :], m1.ap, m2.ap, ALU.add, r=[m1.k(), m2.k()], w=[mT.k(f)])
            if "mT" in self.debug and tc == 0:
                self.dbg("mT", mT.ap, [128, 8, 512], BF16, r=mT.ks(range(8)))
            for tl in range(4):
                t = tc * 4 + tl
                for hf in range(2):
                    ob = 4 + (2 * tl + hf) % 2
                    for f in range(8):
                        self.mm(self.bank(ob), mT.ap[:, f, tl * 128:(tl + 1) * 128], wo.ap[:, f, hf * 512:(hf + 1) * 512], f == 0, f == 7,
                                r=mT.ks(range(8)) + [wo.k()], w=[("ps", ob)])
                    xs_ = self.x_tok[:, t, hf * 512:(hf + 1) * 512]
                    self.tt("dve", xs_, xs_, self.bank(ob), ALU.add, r=[("x", t), ("ps", ob)], w=[("x", t)])
        a.release(m0)

    def moe(self, li):
        p = self.p
        a = self.arena
        m0 = a.mark()
        rstd = self.x_rstd("ffn")
        gT = self.gain_T("ffn", self.d_g_ffn, li, 8)
        gT8 = self.gain_T("ffn8", self.d_g_ffn, li, 8, mode="p8")
        xh = a.alloc("xh", [NT, D], BF16)
        gw1 = a.alloc("gw1", [NT], F32)
        gw2 = a.alloc("gw2", [NT], F32)
        di1 = a.alloc("di1", [NT], I32)
        di2 = a.alloc("di2", [NT], I32)
        m_route = a.mark()
        wr = a.alloc("wr", [8, 72], F32)
        brb = a.alloc("brb", [72], F32)
        lt = a.alloc("lt", [NT, 72], F32)
        x32 = [a.alloc(f"x32_{i}", [D], F32) for i in range(2)]
        h2T = [a.alloc(f"h2T{i}", [8, 128], F32) for i in range(2)]
        self.dma("sp", wr.ap[:, :, 0:8], self.d_w_rg[li].rearrange("(c p) w -> p c w", p=128), r=[], w=[wr.k(0)], sem="wr0", allow_slow_non_contiguous=True)
        self.dma("sp", wr.ap[:, :, 8:72], self.d_w_re[li].rearrange("(c p) w -> p c w", p=128), r=[], w=[wr.k(1)], sem="wr1", allow_slow_non_contiguous=True)
        self.dma("sp", brb.ap[:, 0:8], self.d_b_rg[li:li + 1, :].to_broadcast([128, 8]), r=[], w=[brb.k(0)], sem="brb0", allow_slow_non_contiguous=True)
        self.dma("sp", brb.ap[:, 8:72], self.d_b_re[li:li + 1, :].to_broadcast([128, 64]), r=[], w=[brb.k(1)], sem="brb1", allow_slow_non_contiguous=True)
        for t in range(NT):
            self.act(xh.ap[:, t, :], self.x_tok[:, t, :], AF.Copy, r=[("x", t), rstd.k()], w=[xh.k(t)], scale=rstd.ap[:, t:t + 1])
            x3 = x32[t % 2]
            self.ts("dve", x3.ap, self.x_tok[:, t, :], rstd.ap[:, t:t + 1], None, ALU.mult, None, r=[("x", t), rstd.k()], w=[x3.k()])
            pa = 0 + 2 * (t % 2)
            for c in range(8):
                self.tr(self.bank(pa, 2)[:, c * 128:(c + 1) * 128], x3.ap[:, c * 128:(c + 1) * 128], self.ident_f.ap,
                        r=[x3.k(), ("c_identf",)], w=[("ps", pa), ("ps", pa + 1)])
            hT_ = h2T[t % 2]
            self.tt("dve", hT_.ap, self.bank(pa, 2).rearrange("p (c t) -> p c t", c=8), gT.ap.unsqueeze(2).to_broadcast([128, 8, 128]), ALU.mult,
                    r=[("ps", pa), ("ps", pa + 1), gT.k()], w=[hT_.k()])
            pr = 4 + (t % 2)
            for c in range(8):
                self.mm(self.bank(pr)[:, 0:72], hT_.ap[:, c, :], wr.ap[:, c, :], c == 0, c == 7, r=[hT_.k(), wr.k(0), wr.k(1)], w=[("ps", pr)])
            self.tt("dve", lt.ap[:, t, :], self.bank(pr)[:, 0:72], brb.ap, ALU.add, r=[("ps", pr), brb.k(0), brb.k(1)], w=[lt.k(t)])
        ltk = lt.ks(range(NT))
        al = lambda name, shape, dt=F32: a.alloc(name, shape, dt)
        ngmax = al("ngmax", [NT])
        oh_g = al("oh_g", [NT, 8])
        eg = al("eg", [NT, 8])
        sume = al("sume", [NT])
        pg = al("pg", [NT])
        tmp64 = al("tmp64", [NT, 8, 8])
        e_in = al("e_in", [NT, 8])
        mx1 = al("mx1", [NT])
        mk1 = al("mk1", [NT, 8])
        e2 = al("e2", [NT, 8])
        mx2 = al("mx2", [NT])
        mk2 = al("mk2", [NT, 8])
        dd = al("dd", [NT])
        sel1 = al("sel1", [NT, 8, 8])
        sel2 = al("sel2", [NT, 8, 8])
        selb = al("selb", [NT, 64], BF16)
        rank = al("rank", [NT, 64])
        rs1 = al("rs1", [NT])
        rs2 = al("rs2", [NT])
        d1 = al("d1", [NT])
        d2 = al("d2", [NT])
        lg = lt.ap[:, :, 0:8]
        le = lt.ap[:, :, 8:72].rearrange("p t (g e) -> p t g e", g=8)
        B3 = lambda ap_: ap_.unsqueeze(2).to_broadcast([128, NT, 8])
        V = "dve"
        p.add(V, lambda e: e.tensor_reduce(out=ngmax.ap, in_=lg, axis=AX.X, op=ALU.max, negate=True), r=ltk, w=[ngmax.k()])
        self.tt(V, eg.ap, lg, B3(ngmax.ap), ALU.add, r=ltk + [ngmax.k()], w=[eg.k()])
        self.ts(V, oh_g.ap, eg.ap, 0.0, None, ALU.is_equal, None, r=[eg.k()], w=[oh_g.k()])
        self.act(eg.ap, eg.ap, AF.Exp, r=[eg.k()], w=[eg.k()])
        p.add(V, lambda e: e.tensor_reduce(out=sume.ap, in_=eg.ap, axis=AX.X, op=ALU.add), r=[eg.k()], w=[sume.k()])
        p.add(V, lambda e: e.reciprocal(out=pg.ap, in_=sume.ap), r=[sume.k()], w=[pg.k()])
        self.tt(V, tmp64.ap, le, oh_g.ap.unsqueeze(3).to_broadcast([128, NT, 8, 8]), ALU.mult, r=ltk + [oh_g.k()], w=[tmp64.k()])
        p.add(V, lambda e: e.tensor_reduce(out=e_in.ap, in_=tmp64.ap.rearrange("p t g e -> p t e g"), axis=AX.X, op=ALU.add), r=[tmp64.k()], w=[e_in.k()])
        p.add(V, lambda e: e.tensor_reduce(out=mx1.ap, in_=e_in.ap, axis=AX.X, op=ALU.max), r=[e_in.k()], w=[mx1.k()])
        self.tt(V, mk1.ap, e_in.ap, B3(mx1.ap), ALU.is_equal, r=[e_in.k(), mx1.k()], w=[mk1.k()])
        self.stt(e2.ap, mk1.ap, -1e30, e_in.ap, ALU.mult, ALU.add, r=[mk1.k(), e_in.k()], w=[e2.k()])
        p.add(V, lambda e: e.tensor_reduce(out=mx2.ap, in_=e2.ap, axis=AX.X, op=ALU.max), r=[e2.k()], w=[mx2.k()])
        self.tt(V, mk2.ap, e2.ap, B3(mx2.ap), ALU.is_equal, r=[e2.k(), mx2.k()], w=[mk2.k()])
        self.tt(V, dd.ap, mx2.ap, mx1.ap, ALU.subtract, r=[mx1.k(), mx2.k()], w=[dd.k()])
        self.act(dd.ap, dd.ap, AF.Exp, r=[dd.k()], w=[dd.k()])
        self.ts(V, gw1.ap, dd.ap, 1.0, None, ALU.add, None, r=[dd.k()], w=[gw1.k()])
        p.add(V, lambda e: e.reciprocal(out=gw1.ap, in_=gw1.ap), r=[gw1.k()], w=[gw1.k()])
        self.tt(V, gw1.ap, gw1.ap, pg.ap, ALU.mult, r=[gw1.k(), pg.k()], w=[gw1.k()])
        self.tt(V, gw2.ap, gw1.ap, dd.ap, ALU.mult, r=[gw1.k(), dd.k()], w=[gw2.k()])
        ohb = oh_g.ap.unsqueeze(3).to_broadcast([128, NT, 8, 8])
        self.tt(V, sel1.ap, ohb, mk1.ap.unsqueeze(2).to_broadcast([128, NT, 8, 8]), ALU.mult, r=[oh_g.k(), mk1.k()], w=[sel1.k()])
        self.tt(V, sel2.ap, ohb, mk2.ap.unsqueeze(2).to_broadcast([128, NT, 8, 8]), ALU.mult, r=[oh_g.k(), mk2.k()], w=[sel2.k()])
        s1f = sel1.ap.rearrange("p t g e -> p t (g e)")
        s2f = sel2.ap.rearrange("p t g e -> p t (g e)")
        self.tt(V, selb.ap, s1f, s2f, ALU.add, r=[sel1.k(), sel2.k()], w=[selb.k()])
        for t in range(NT):
            rb = 6 + t // 8
            out = self.bank(rb)[:, (t % 8) * 64:(t % 8) * 64 + 64]
            self.mm(out, self.ustrict.ap, selb.ap[:, t, :], True, t == 0, r=[selb.k(), ("c_us",)], w=[("ps", rb)])
            for t2 in range(t):
                self.mm(out, self.ones_bf.ap, selb.ap[:, t2, :], False, t2 == t - 1, r=[selb.k(), ("c_onesbf",)], w=[("ps", rb)])
        self.cp(V, rank.ap, self.bank(6, 2).rearrange("p (t e) -> p t e", t=NT), r=[("ps", 6), ("ps", 7)], w=[rank.k()])
        ebb = self.ebase.ap.unsqueeze(1).to_broadcast([128, NT, 64])
        tf = tmp64.ap.rearrange("p t g e -> p t (g e)")
        for (sf, sk, rs, dst, dsti, gw) in ((s1f, sel1, rs1, d1, di1, gw1), (s2f, sel2, rs2, d2, di2, gw2)):
            self.tt(V, tf, sf, rank.ap, ALU.mult, r=[sk.k(), rank.k()], w=[tmp64.k()])
            p.add(V, (lambda e, rs=rs: e.tensor_reduce(out=rs.ap, in_=tf, axis=AX.X, op=ALU.add)), r=[tmp64.k()], w=[rs.k()])
            self.tt(V, tf, sf, ebb, ALU.mult, r=[sk.k(), ("c_ebase",)], w=[tmp64.k()])
            p.add(V, (lambda e, dst=dst: e.tensor_reduce(out=dst.ap, in_=tf, axis=AX.X, op=ALU.add)), r=[tmp64.k()], w=[dst.k()])
            self.tt(V, dst.ap, dst.ap, rs.ap, ALU.add, r=[dst.k(), rs.k()], w=[dst.k()])
            self.ts(V, rs.ap, rs.ap, float(CAP), None, ALU.is_ge, None, r=[rs.k()], w=[rs.k()])
            self.stt(dst.ap, rs.ap, 1.0e6, dst.ap, ALU.mult, ALU.add, r=[rs.k(), dst.k()], w=[dst.k()])
            self.cp(V, dsti.ap, dst.ap, r=[dst.k()], w=[dsti.k()])
        if "route" in self.debug:
            self.dbg("lt", lt.ap, [128, NT, 72], F32, r=ltk)
            self.dbg("d1", d1.ap, [128, NT], F32, r=[d1.k()])
            self.dbg("d2", d2.ap, [128, NT], F32, r=[d2.k()])
            self.dbg("gw1", gw1.ap, [128, NT], F32, r=[gw1.k()])
            self.dbg("gw2", gw2.ap, [128, NT], F32, r=[gw2.k()])
        xs_d = self.d_xs[li]
        ys_d = self.d_ys[li]
        for t in range(NT):
            for (dsti, nm) in ((di1, "a"), (di2, "b")):
                p.add("pool", (lambda e, t=t, dsti=dsti: e.indirect_dma_start(
                    out=xs_d, out_offset=bass.IndirectOffsetOnAxis(ap=dsti.ap[:, t:t + 1], axis=0),
                    in_=xh.ap[:, t, :], in_offset=None, bounds_check=self.bc_reg(e), oob_is_err=False)),
                    r=[xh.k(t), dsti.k()], w=[("xs", li, t, nm)], dma="scat")
        xs_keys = [("xs", li, t, nm) for t in range(NT) for nm in "ab"]
        a.release(m_route)
        m_exp = a.mark()
        NW, NW2, NX = 4, 6, 3
        w1 = [a.alloc(f"w1_{i}", [8, 256], BF16) for i in range(NW)]
        w3 = [a.alloc(f"w3_{i}", [8, 256], BF16) for i in range(NW)]
        w2 = [a.alloc(f"w2_{i}", [2, D], BF16) for i in range(NW2)]
        xse = [a.alloc(f"xse{i}", [D], BF16) for i in range(NX)]
        xsT = [a.alloc(f"xsT{i}", [8, 128], BF16) for i in range(2)]
        sl = [a.alloc(f"sl{i}", [256], F32) for i in range(2)]
        gg_ = [a.alloc(f"g{i}", [256], BF16) for i in range(2)]
        gT_ = [a.alloc(f"gT{i}", [2, 128], BF16) for i in range(2)]
        ye = [a.alloc(f"ye{i}", [D], F32) for i in range(2)]

        def st_load(ex):
            self.dma("pool", w1[ex % NW].ap, self.d_w1[li, ex].rearrange("(p c) f -> p c f", p=128), r=[], w=[w1[ex % NW].k()], sem=f"w1_{ex % NW}")
            self.dma("pool", w3[ex % NW].ap, self.d_w3[li, ex].rearrange("(p c) f -> p c f", p=128), r=[], w=[w3[ex % NW].k()], sem=f"w3_{ex % NW}")
            self.dma("pool", w2[ex % NW2].ap, self.d_w2[li, ex].rearrange("(p c) f -> p c f", p=128), r=[], w=[w2[ex % NW2].k()], sem=f"w2_{ex % NW2}")
            self.dma("sp", xse[ex % NX].ap, xs_d[ex * CAP:(ex + 1) * CAP, :], r=xs_keys, w=[xse[ex % NX].k()], sem=f"xse{ex % NX}")

        def st_a(ex):
            pt = ex % 2
            ptb = self.bank_bf(pt)
            xb = xse[ex % NX]
            for c in range(8):
                self.tr(ptb[:, c * 128:(c + 1) * 128], xb.ap[:, c:D:8], self.ident_bf.ap, r=[xb.k(), ("c_identbf",)], w=[("ps", pt)])
            self.tt("dve", xsT[ex % 2].ap, ptb.rearrange("p (c t) -> p c t", c=8), gT8.ap.unsqueeze(2).to_broadcast([128, 8, 128]), ALU.mult,
                    r=[("ps", pt), gT8.k()], w=[xsT[ex % 2].k()])

        def st_b(ex):
            s2 = ex % 2
            ph = 2 + s2
            for c in range(8):
                self.mm(self.bank(ph)[:, 0:256], xsT[s2].ap[:, c, :], w1[ex % NW].ap[:, c, :], c == 0, c == 7, r=[xsT[s2].k(), w1[ex % NW].k()], w=[("ps", ph)])
            for c in range(8):
                self.mm(self.bank(ph)[:, 256:512], xsT[s2].ap[:, c, :], w3[ex % NW].ap[:, c, :], c == 0, c == 7, r=[xsT[s2].k(), w3[ex % NW].k()], w=[("ps", ph)])
            self.act(sl[s2].ap, self.bank(ph)[:, 0:256], AF.Silu, r=[("ps", ph)], w=[sl[s2].k()])
            self.tt("dve", gg_[s2].ap, sl[s2].ap, self.bank(ph)[:, 256:512], ALU.mult, r=[sl[s2].k(), ("ps", ph)], w=[gg_[s2].k()])

        def st_c(ex):
            s2 = ex % 2
            pt = (ex + 1) % 2
            ptb = self.bank_bf(pt)
            for c in range(2):
                self.tr(ptb[:, c * 128:(c + 1) * 128], gg_[s2].ap[:, c:256:2], self.ident_bf.ap, r=[gg_[s2].k(), ("c_identbf",)], w=[("ps", pt)])
            self.cp("act", gT_[s2].ap, ptb[:, 0:256].rearrange("p (c t) -> p c t", c=2), r=[("ps", pt)], w=[gT_[s2].k()])

        def st_d(ex):
            s2 = ex % 2
            py = 4 + 2 * s2
            for hf in range(2):
                for c in range(2):
                    self.mm(self.bank(py + hf), gT_[s2].ap[:, c, :], w2[ex % NW2].ap[:, c, hf * 512:(hf + 1) * 512], c == 0, c == 1,
                            r=[gT_[s2].k(), w2[ex % NW2].k()], w=[("ps", py + hf)])
            self.cp("act", ye[s2].ap[:, 0:512], self.bank(py), r=[("ps", py)], w=[ye[s2].k(0)])
            self.cp("dve", ye[s2].ap[:, 512:1024], self.bank(py + 1), r=[("ps", py + 1)], w=[ye[s2].k(1)])
            self.dma("sp", ys_d[ex * CAP:(ex + 1) * CAP, :], ye[s2].ap, r=[ye[s2].k(0), ye[s2].k(1)], w=[("ys", li, ex)], sem=f"yst{s2}")

        for ex in range(min(NX - 1, NEXP)):
            st_load(ex)
        for i in range(NEXP + 4):
            if 0 <= i - 1 < NEXP:
                st_a(i - 1)
            if 0 <= i - 2 < NEXP:
                st_b(i - 2)
            if 0 <= i - 3 < NEXP:
                st_c(i - 3)
            if 0 <= i - 4 < NEXP:
                st_d(i - 4)
            if i + NX - 1 < NEXP:
                st_load(i + NX - 1)
        ys_keys = [("ys", li, ex) for ex in range(NEXP)]
        a.release(m_exp)
        y1 = [a.alloc(f"y1_{i}", [D], F32) for i in range(2)]
        y2 = [a.alloc(f"y2_{i}", [D], F32) for i in range(2)]
        for t in range(NT):
            for (yb, dsti, gw, nm) in ((y1[t % 2], di1, gw1, "a"), (y2[t % 2], di2, gw2, "b")):
                self.memset("pool", yb.ap, 0.0, w=[yb.k()])
                p.add("pool", (lambda e, t=t, yb=yb, dsti=dsti: e.indirect_dma_start(
                    out=yb.ap, out_offset=None, in_=ys_d, in_offset=bass.IndirectOffsetOnAxis(ap=dsti.ap[:, t:t + 1], axis=0),
                    bounds_check=self.bc_reg(e), oob_is_err=False)), r=ys_keys + [dsti.k(), yb.k()], w=[yb.k()], dma=f"gath{nm}{t % 2}")
                self.stt(self.x_tok[:, t, :], yb.ap, gw.ap[:, t:t + 1], self.x_tok[:, t, :], ALU.mult, ALU.add,
                         r=[yb.k(), gw.k(), ("x", t)], w=[("x", t)])
        a.release(m0)

    def ple(self, li):
        p = self.p
        a = self.arena
        m0 = a.mark()
        rstd = self.x_rstd("ple")
        gT = self.gain_T("ple", self.d_g_ple, li, 8)
        wpg = a.alloc("wpg", [8, D], BF16)
        wpp = a.alloc("wpp", [2, D], BF16)
        ptok = [a.alloc(f"ptok{i}", [256], BF16) for i in range(2)]
        pTt = [a.alloc(f"pT{i}", [2, 128], BF16) for i in range(2)]
        htok = [a.alloc(f"phtok{i}", [D], BF16) for i in range(2)]
        h3T = [a.alloc(f"h3T{i}", [8, 128], BF16) for i in range(2)]
        sg = [a.alloc(f"sg{i}", [512], F32) for i in range(2)]
        ge = [a.alloc(f"ge{i}", [512], F32) for i in range(2)]
        self.dma("pool", wpg.ap, self.d_w_pg[li].rearrange("(c p) w -> p c w", p=128), r=[], w=[wpg.k()], sem="wpg")
        self.dma("pool", wpp.ap, self.d_w_pp[li].rearrange("(c p) w -> p c w", p=128), r=[], w=[wpp.k()], sem="wpp")
        pv = self.d_p[li].rearrange("(t p) d -> p t d", p=128)
        for t in range(NT):
            s2 = t % 2
            self.dma("pool", ptok[s2].ap, pv[:, t, :], r=[], w=[ptok[s2].k()], sem=f"ptok{s2}")
            self.norm_T_tile(t, rstd, gT, htok[s2], h3T[s2].ap, slice(0, 128), 6, [h3T[s2].k()])
            ptb = self.bank_bf(7)
            for c in range(2):
                self.tr(ptb[:, c * 128:(c + 1) * 128], ptok[s2].ap[:, c * 128:(c + 1) * 128], self.ident_bf.ap, r=[ptok[s2].k(), ("c_identbf",)], w=[("ps", 7)])
            self.cp("act", pTt[s2].ap, ptb[:, 0:256].rearrange("p (c t) -> p c t", c=2), r=[("ps", 7)], w=[pTt[s2].k()])
            for hf in range(2):
                pg_, pe_ = 0 + 2 * hf, 1 + 2 * hf
                cs = slice(hf * 512, (hf + 1) * 512)
                for c in range(8):
                    self.mm(self.bank(pg_), h3T[s2].ap[:, c, :], wpg.ap[:, c, cs], c == 0, c == 7, r=[h3T[s2].k(), wpg.k()], w=[("ps", pg_)])
                for c in range(2):
                    self.mm(self.bank(pe_), pTt[s2].ap[:, c, :], wpp.ap[:, c, cs], c == 0, c == 1, r=[pTt[s2].k(), wpp.k()], w=[("ps", pe_)])
                self.act(sg[hf].ap, self.bank(pg_), AF.Sigmoid, r=[("ps", pg_)], w=[sg[hf].k()])
                self.tt("dve", ge[hf].ap, sg[hf].ap, self.bank(pe_), ALU.mult, r=[sg[hf].k(), ("ps", pe_)], w=[ge[hf].k()])
                xs_ = self.x_tok[:, t, cs]
                self.tt("pool", xs_, xs_, ge[hf].ap, ALU.add, r=[("x", t), ge[hf].k()], w=[("x", t)])
        a.release(m0)

    def final(self):
        p = self.p
        a = self.arena
        m0 = a.mark()
        rstd = self.x_rstd("fin")
        gb = a.alloc("gfin", [D], F32)
        yb = [a.alloc(f"fy{i}", [D], F32) for i in range(2)]
        self.dma("sp", gb.ap, self.d_g_fin.rearrange("(o d) -> o d", o=1).to_broadcast([128, D]), r=[], w=[gb.k()], sem="gfin", allow_slow_non_contiguous=True)
        ov = self.d_out.rearrange("(t p) d -> p t d", p=128)
        for t in range(NT):
            y = yb[t % 2]
            self.stt(y.ap, self.x_tok[:, t, :], rstd.ap[:, t:t + 1], gb.ap, ALU.mult, ALU.mult, r=[("x", t), rstd.k(), gb.k()], w=[y.k()])
            self.dma("sp", ov[:, t, :], y.ap, r=[y.k()], w=[("out", t)], sem=f"ost{t % 2}")
            self.out_keys.append(("out", t))
        a.release(m0)


_W_NAMES = ["g_mix", "w_in", "g_q_lat", "w_q_up", "g_kv_lat", "w_kv_up", "w_branch_a", "w_branch_b", "w_out", "g_ffn",
            "w_router_grp", "b_router_grp", "w_router_exp", "b_router_exp", "w_exp_gate", "w_exp_up", "w_exp_down",
            "g_ple", "w_ple_gate", "w_ple_proj"]

LAYERS_PER_LAUNCH = 4


def _run(x, inputs, layer_lo, layer_hi, final_norm):
    nl = layer_hi - layer_lo
    b = Builder(nl, first_layer=layer_lo, final_norm=final_norm)
    nc = b.build()
    shared = {}
    for k in _W_NAMES:
        shared[k] = np.ascontiguousarray(np.asarray(inputs[k])[layer_lo:layer_hi])
    shared["b_router_exp"] = shared["b_router_exp"].reshape(nl, 64)
    shared["g_final"] = np.ascontiguousarray(np.asarray(inputs["g_final"]))
    p_all = np.asarray(inputs["p"])
    pos = np.asarray(inputs["positions"]).astype(np.int32)
    in_maps = []
    for c in range(NCORES):
        m = dict(shared)
        m["x"] = np.ascontiguousarray(x[c])
        m["p"] = np.ascontiguousarray(p_all[layer_lo:layer_hi, c])
        m["positions"] = np.ascontiguousarray(pos[c])
        in_maps.append(m)
    res = run_bass_kernel_spmd(nc, in_maps, core_ids=list(range(NCORES)))
    return np.stack([np.asarray(r["out"]) for r in res.results], axis=0)


def kernel(**inputs):
    x = np.asarray(inputs["x"]).astype(np.float32, copy=False)
    lo = 0
    while lo < DEPTH:
        hi = min(DEPTH, lo + LAYERS_PER_LAUNCH)
        x = _run(x, inputs, lo, hi, final_norm=(hi == DEPTH))
        lo = hi
    return x.astype(np.float32, copy=False)
```

```python
import math
import bisect
from contextlib import ExitStack

import numpy as np
import concourse.bass as bass
import concourse.mybir as mybir
from concourse.bass_utils import run_bass_kernel_spmd

F32 = mybir.dt.float32
BF16 = mybir.dt.bfloat16
I32 = mybir.dt.int32
AF = mybir.ActivationFunctionType
ALU = mybir.AluOpType
AX = mybir.AxisListType

S = 2048
D = 1024
NT = 16
DEPTH = 4
NCORES = 8
EPS = 1e-6
CAP = 128
NEXP = 64
XS_ROWS = NEXP * CAP
PI = math.pi

EPOCH = 30000


class Prog:
    ENGS = ("pe", "act", "dve", "pool", "sp")

    def __init__(self):
        self.ops = []
        self.lastw = {}
        self.rd = {}

    def add(self, eng, fn, r=(), w=(), dma=None, noinc=False):
        i = len(self.ops)
        raw = set()
        oth = set()
        psr = [k for k in r if isinstance(k, tuple) and k and k[0] == "ps"]
        if psr:
            r = [k for k in r if not (isinstance(k, tuple) and k and k[0] == "ps")]
            w = list(w) + [k for k in psr if k not in w]
        for k in r:
            lw = self.lastw.get(k)
            if lw is not None:
                raw.add(lw)
        for k in w:
            lw = self.lastw.get(k)
            if lw is not None:
                oth.add(lw)
            oth.update(self.rd.get(k, ()))
        raw.discard(i)
        oth.discard(i)
        for k in r:
            self.rd.setdefault(k, []).append(i)
        for k in w:
            self.lastw[k] = i
            self.rd[k] = []
        self.ops.append(dict(eng=eng, fn=fn, raw=raw, oth=oth - raw, dma=dma, noinc=noinc))
        return i

    def alias(self, new_keys, old_keys):
        s = set()
        for k in old_keys:
            lw = self.lastw.get(k)
            if lw is not None:
                s.add(lw)
            s.update(self.rd.get(k, ()))
        for k in new_keys:
            cur = self.rd.get(k, [])
            self.rd[k] = list(set(cur) | s)

    def emit(self, nc, stack, trunc=None):
        ops = self.ops
        cnt = {}
        dma_hist = {}
        for i, op in enumerate(ops):
            op["skip"] = trunc is not None and trunc[0] <= i < trunc[1]
            if op["skip"]:
                op["stream"] = None
                continue
            if op["noinc"]:
                op["stream"] = None
                continue
            if op["dma"] is not None:
                base = "d_" + op["dma"]
                c = cnt.get(base, 0)
                cnt[base] = c + 1
                per = EPOCH // 16
                op["stream"] = f"{base}_{c // per}"
                op["val"] = (c % per + 1) * 16
                dma_hist.setdefault(op["stream"], []).append((i, op["val"]))
            else:
                base = "c_" + op["eng"]
                c = cnt.get(base, 0)
                cnt[base] = c + 1
                op["stream"] = f"{base}_{c // EPOCH}"
                op["val"] = c % EPOCH + 1
        streams = sorted({op["stream"] for op in ops if op["stream"] is not None})
        sems = {s: stack.enter_context(nc.semaphore(s)) for s in streams}
        self.n_sems = len(sems)
        queues = {E: [] for E in self.ENGS}
        for i, op in enumerate(ops):
            queues[op["eng"]].append(i)
        waited = {E: {} for E in self.ENGS}
        stats = {"waits": 0}

        def run(e, E):
            for i in queues[E]:
                op = ops[i]
                if op["skip"]:
                    continue
                need = {}
                is_c = op["dma"] is None
                implied = set()
                for kind in ("raw", "oth"):
                    for d in op[kind]:
                        implied |= ops[d]["raw"]
                        implied |= ops[d]["oth"]
                for kind in ("raw", "oth"):
                    for d in op[kind]:
                        if d in implied:
                            continue
                        dop = ops[d]
                        if dop["skip"]:
                            continue
                        if is_c and dop["dma"] is None and dop["eng"] == E:
                            if E == "pe" or kind == "oth":
                                continue
                        s, v = dop["stream"], dop["val"]
                        if need.get(s, 0) < v:
                            need[s] = v
                for s, v in need.items():
                    if waited[E].get(s, 0) >= v:
                        continue
                    if s.startswith("d_"):
                        hist = dma_hist[s]
                        j = bisect.bisect_left(hist, (i, 0)) - 1
                        assert j >= 0 and hist[j][1] == v, f"partial DMA wait on {s}: want {v} issued {hist[j][1]} op {i}"
                    e.wait_ge(sems[s], v)
                    waited[E][s] = v
                    stats["waits"] += 1
                ins = op["fn"](e)
                if op["stream"] is not None:
                    ins.then_inc(sems[op["stream"]], 16 if op["dma"] is not None else 1)

        with nc.Block() as block:
            @block.tensor
            def _(e):
                run(e, "pe")

            @block.scalar
            def _(e):
                run(e, "act")

            @block.vector
            def _(e):
                run(e, "dve")

            @block.gpsimd
            def _(e):
                run(e, "pool")

            @block.sync
            def _(e):
                run(e, "sp")
        self.stats = stats


class Buf:
    def __init__(self, prog, name, gen, ap, start, end, old_keys):
        self.p = prog
        self.name = name
        self.gen = gen
        self.ap = ap
        self.start = start
        self.end = end
        self.old_keys = old_keys
        self.keys = {}

    def k(self, i=0):
        key = self.keys.get(i)
        if key is None:
            key = (self.name, self.gen, i)
            self.keys[i] = key
            if self.old_keys:
                self.p.alias([key], self.old_keys)
        return key

    def ks(self, it):
        return [self.k(i) for i in it]


class Arena:
    def __init__(self, prog, big, nwords):
        self.p = prog
        self.big = big
        self.n = nwords
        self.top = 0
        self.hi = nwords
        self.dead = []
        self.live = []
        self.live_hi = []
        self.gen = 0
        self.peak = 0

    def mark(self):
        return (self.top, len(self.live))

    def release(self, m):
        top, nl = m
        while len(self.live) > nl:
            self.dead.append(self.live.pop())
        self.top = top

    def mark_hi(self):
        return (self.hi, len(self.live_hi))

    def release_hi(self, m):
        hi, nl = m
        while len(self.live_hi) > nl:
            self.dead.append(self.live_hi.pop())
        self.hi = hi

    def alloc(self, name, shape, dtype, top=False):
        nel = 1
        for s_ in shape:
            nel *= s_
        esz = 4 if dtype in (F32, I32) else 2
        nw = (nel * esz + 3) // 4
        nw = (nw + 7) // 8 * 8
        if top:
            end = self.hi
            start = end - nw
            assert start >= self.top, f"SBUF arena overflow allocating {name} (top)"
            self.hi = start
        else:
            start = self.top
            end = start + nw
            assert end <= self.hi, f"SBUF arena overflow allocating {name}: {end} > {self.hi}"
            self.top = end
        self.peak = max(self.peak, self.top + (self.n - self.hi))
        old_keys = []
        keep = []
        for b in self.dead:
            if b.start < end and start < b.end:
                old_keys.extend(b.keys.values())
                old_keys.extend(b.old_keys)
                if not (start <= b.start and b.end <= end):
                    keep.append(b)
            else:
                keep.append(b)
        self.dead = keep
        self.gen += 1
        ap = self.big[:, start:end]
        if esz == 2:
            ap = ap.bitcast(dtype)[:, 0:nel]
        elif dtype == I32:
            ap = ap.bitcast(I32)[:, 0:nel]
        else:
            ap = ap[:, 0:nel]
        if len(shape) == 2:
            ap = ap.rearrange("p (a b) -> p a b", a=shape[0])
        elif len(shape) == 3:
            ap = ap.rearrange("p (a b c) -> p a b c", a=shape[0], b=shape[1])
        b = Buf(self.p, name, self.gen, ap, start, end, list(dict.fromkeys(old_keys)))
        (self.live_hi if top else self.live).append(b)
        return b


class Builder:
    def __init__(self, n_layers, first_layer=0, final_norm=True, debug=None):
        self.n_layers = n_layers
        self.first_layer = first_layer
        self.final_norm = final_norm
        self.debug = debug or {}
        self.dbg_outs = []

    def mm(self, out, lhsT, rhs, start, stop, r, w):
        self.p.add("pe", lambda e: e.matmul(out, lhsT=lhsT, rhs=rhs, start=start, stop=stop), r=r, w=w)

    def tr(self, out, in_, ident, r, w):
        self.p.add("pe", lambda e: e.transpose(out, in_, ident), r=r, w=w)

    def act(self, out, in_, func, r, w, bias=None, scale=None, accum_out=None, eng="act"):
        kw = {}
        if bias is not None:
            kw["bias"] = bias
        if scale is not None:
            kw["scale"] = scale
        if accum_out is not None:
            kw["accum_out"] = accum_out
        self.p.add(eng, lambda e: e.activation(out=out, in_=in_, func=func, **kw), r=r, w=w)

    def tt(self, eng, out, in0, in1, op, r, w):
        if eng == "pool" and "nopool" in self.debug.get("skip", ""):
            eng = "dve"
        self.p.add(eng, lambda e: e.tensor_tensor(out=out, in0=in0, in1=in1, op=op), r=r, w=w)

    def ts(self, eng, out, in0, s1, s2, op0, op1, r, w):
        if op1 is None:
            self.p.add(eng, lambda e: e.tensor_scalar(out=out, in0=in0, scalar1=s1, scalar2=None, op0=op0), r=r, w=w)
        else:
            self.p.add(eng, lambda e: e.tensor_scalar(out=out, in0=in0, scalar1=s1, scalar2=s2, op0=op0, op1=op1), r=r, w=w)

    def stt(self, out, in0, scalar, in1, op0, op1, r, w):
        self.p.add("dve", lambda e: e.scalar_tensor_tensor(out=out, in0=in0, scalar=scalar, in1=in1, op0=op0, op1=op1), r=r, w=w)

    def cp(self, eng, out, in_, r, w):
        if eng == "act":
            self.p.add("act", lambda e: e.activation(out=out, in_=in_, func=AF.Copy), r=r, w=w)
        else:
            self.p.add(eng, lambda e: e.tensor_copy(out=out, in_=in_), r=r, w=w)

    def memset(self, eng, ap, val, w):
        if eng == "pool" and "nopoolms" in self.debug.get("skip", ""):
            eng = "dve"
        self.p.add(eng, lambda e: e.memset(ap, val), w=w)

    def dma(self, eng, out, in_, r, w, sem, **kw):
        self.p.add(eng, lambda e: e.dma_start(out=out, in_=in_, **kw), r=r, w=w, dma=sem)

    def bc_reg(self, e):
        if getattr(self, "_bc_reg", None) is None:
            self._bc_reg = e.to_reg(XS_ROWS - 1)
        return self._bc_reg

    def dbg(self, name, ap, shape, dtype, r):
        t = self.nc.dram_tensor("dbg_" + name, list(shape), dtype, kind="ExternalOutput").ap()
        self.dma("sp", t, ap, r=r, w=[("dbgout", name)], sem="dbg_" + name)
        self.dbg_outs.append(name)
        self.out_keys.append(("dbgout", name))

    def build(self):
        nc = bass.Bass("TRN2", target_bir_lowering=False)
        self.nc = nc
        p = Prog()
        self.p = p
        nl = self.n_layers
        self.out_keys = []

        def din(name, shape, dt=F32):
            return nc.dram_tensor(name, list(shape), dt, kind="ExternalInput").ap()

        self.d_x = din("x", [S, D])
        self.d_p = din("p", [nl, S, 256])
        self.d_pos = din("positions", [S], I32)
        self.d_g_mix = din("g_mix", [nl, D])
        self.d_w_in = din("w_in", [nl, D, 5152])
        self.d_g_q = din("g_q_lat", [nl, 512])
        self.d_w_q = din("w_q_up", [nl, 512, 1536])
        self.d_g_kv = din("g_kv_lat", [nl, 256])
        self.d_w_kv = din("w_kv_up", [nl, 256, 2048])
        self.d_w_ba = din("w_branch_a", [nl, D, D])
        self.d_w_bb = din("w_branch_b", [nl, 256, D])
        self.d_w_out = din("w_out", [nl, D, D])
        self.d_g_ffn = din("g_ffn", [nl, D])
        self.d_w_rg = din("w_router_grp", [nl, D, 8])
        self.d_b_rg = din("b_router_grp", [nl, 8])
        self.d_w_re = din("w_router_exp", [nl, D, 64])
        self.d_b_re = din("b_router_exp", [nl, 64])
        self.d_w1 = din("w_exp_gate", [nl, NEXP, D, 256])
        self.d_w3 = din("w_exp_up", [nl, NEXP, D, 256])
        self.d_w2 = din("w_exp_down", [nl, NEXP, 256, D])
        self.d_g_ple = din("g_ple", [nl, D])
        self.d_w_pg = din("w_ple_gate", [nl, D, D])
        self.d_w_pp = din("w_ple_proj", [nl, 256, D])
        self.d_g_fin = din("g_final", [D])
        self.d_out = nc.dram_tensor("out", [S, D], F32, kind="ExternalOutput").ap()
        self.d_xs = [nc.dram_tensor(f"xs{l}", [XS_ROWS, D], BF16, kind="Internal").ap() for l in range(nl)]
        self.d_ys = [nc.dram_tensor(f"ys{l}", [XS_ROWS, D], F32, kind="Internal").ap() for l in range(nl)]

        with ExitStack() as stack:
            XW = NT * D
            CW = 2560
            total_words = 212800 // 4
            AW = total_words - XW - CW
            big = stack.enter_context(nc.sbuf_tensor("big", [128, total_words], F32))
            self.ps = stack.enter_context(nc.psum_tensor("ps", [128, 4096], F32))
            self.x_tok = big[:, 0:XW].rearrange("p (t d) -> p t d", t=NT)
            self.carena = Arena(p, big[:, XW:XW + CW], CW)
            self.arena = Arena(p, big[:, XW + CW:XW + CW + AW], AW)
            self.setup_consts()
            xv = self.d_x.rearrange("(t p) d -> p t d", p=128)
            for t in range(NT):
                self.dma("sp", self.x_tok[:, t, :], xv[:, t, :], r=[], w=[("x", t)], sem=f"xload{t}")
            for li in range(nl):
                if self.debug.get("stop") == "consts":
                    break
                self.layer(li)
            n_body = len(p.ops)
            if self.final_norm:
                self.final()
            else:
                ov = self.d_out.rearrange("(t p) d -> p t d", p=128)
                for t in range(NT):
                    self.dma("sp", ov[:, t, :], self.x_tok[:, t, :], r=[("x", t)], w=[("out", t)], sem=f"ost{t % 4}")
                    self.out_keys.append(("out", t))
            p.add("sp", lambda e: e.nop(), r=list(self.out_keys), w=[], noinc=True)
            mo = self.debug.get("maxops")
            p.emit(nc, stack, trunc=(int(mo), n_body) if mo else None)
        self.arena_peak = self.arena.peak
        return nc

    def bank(self, b, n=1):
        return self.ps[:, b * 512:(b + n) * 512]

    def bank_bf(self, b):
        return self.ps[:, b * 512:(b + 1) * 512].bitcast(BF16)

    def setup_consts(self):
        p = self.p
        ca = self.carena
        self.ident_bf = ca.alloc("ident_bf", [128], BF16)
        self.ident_f = ca.alloc("ident_f", [128], F32)
        self.tri2 = ca.alloc("tri2", [256], BF16)
        self.ones_f = ca.alloc("ones_f", [128], F32)
        self.ones_bf = ca.alloc("ones_bf", [128], BF16)
        self.ustrict = ca.alloc("ustrict", [128], BF16)
        self.pos_i = ca.alloc("pos_i", [4, NT], I32)
        self.pos_f = ca.alloc("pos_f", [4, NT], F32)
        self.inv_m = ca.alloc("inv_m", [16], F32)
        self.inv_d = ca.alloc("inv_d", [8], F32)
        self.cos_m = ca.alloc("cos_m", [NT, 16], F32)
        self.sin_m = ca.alloc("sin_m", [NT, 16], F32)
        self.cos_d = ca.alloc("cos_d", [3, NT, 8], F32)
        self.sin_d = ca.alloc("sin_d", [3, NT, 8], F32)
        self.neghalf = ca.alloc("neghalf", [NT], F32)
        self.ebase = ca.alloc("ebase", [NEXP], F32)
        self.negpi = ca.alloc("negpi", [1], F32)
        CK = ("consts",)

        SK = self.debug.get("skip", "")
        for _i in range(int(self.debug.get("pad") or 0)):
            self.memset("dve", self.ones_bf.ap, 1.0, w=[("c_onesbf",)])
        def iden(buf, key):
            self.memset("pool", buf.ap, 1.0, w=[key])
            if "sel" in SK:
                return
            p.add("pool", lambda e: e.affine_select(out=buf.ap, in_=buf.ap, pattern=[[1, 128]], compare_op=ALU.is_equal,
                                                     fill=0.0, base=0, channel_multiplier=-1), r=[key], w=[key])
        iden(self.ident_bf, ("c_identbf",))
        iden(self.ident_f, ("c_identf",))
        t2 = self.tri2.ap
        self.memset("pool", t2, 1.0, w=[("c_tri",)])
        if "sel" not in SK:
          p.add("pool", lambda e: e.affine_select(out=t2[:, 0:128], in_=t2[:, 0:128], pattern=[[1, 128]], compare_op=ALU.is_ge,
                                                 fill=0.0, base=0, channel_multiplier=-1), r=[("c_tri",)], w=[("c_tri",)])
        if "sel" not in SK:
          p.add("pool", lambda e: e.affine_select(out=t2[:, 128:256], in_=t2[:, 128:256], pattern=[[-1, 128]], compare_op=ALU.is_ge,
                                                 fill=0.0, base=0, channel_multiplier=1), r=[("c_tri",)], w=[("c_tri",)])
        us = self.ustrict.ap
        self.memset("pool", us, 1.0, w=[("c_us",)])
        if "sel" not in SK:
          p.add("pool", lambda e: e.affine_select(out=us, in_=us, pattern=[[1, 128]], compare_op=ALU.is_gt,
                                                 fill=0.0, base=0, channel_multiplier=-1), r=[("c_us",)], w=[("c_us",)])
        self.memset("pool", self.ones_f.ap, 1.0, w=[("c_ones",)])
        self.memset("pool", self.ones_bf.ap, 1.0, w=[("c_onesbf",)])
        self.memset("pool", self.neghalf.ap, -0.5, w=[("c_nh",)])
        self.memset("pool", self.negpi.ap, -PI, w=[("c_negpi",)])
        eb = self.ebase.ap
        if "iota" not in SK:
          p.add("pool", lambda e: e.iota(eb, pattern=[[CAP, NEXP]], base=0, channel_multiplier=0,
                                        allow_small_or_imprecise_dtypes=True), w=[("c_ebase",)])
        for j in range(16):
            self.memset("pool", self.inv_m.ap[:, j:j + 1], float(np.float32(10000.0) ** np.float32(-j * 2.0 / 32)), w=[("c_invm",)])
        for j in range(8):
            self.memset("pool", self.inv_d.ap[:, j:j + 1], float(np.float32(500000.0) ** np.float32(-j * 2.0 / 16)), w=[("c_invd",)])
        pv = [self.d_pos.rearrange("(t p) -> p t", p=128)]
        for d in (1, 4, 16):
            nb = NT // d
            pv.append(self.d_pos.rearrange("(j p r) -> p r j", p=128, r=d, j=nb))
        for gi in range(4):
            if "pos" in SK:
                break
            if gi == 0:
                dst = self.pos_i.ap[:, gi, :]
            else:
                d = (1, 4, 16)[gi - 1]
                dst = self.pos_i.ap[:, gi, :].rearrange("p (r j) -> p r j", r=d)
            self.dma("sp", dst, pv[gi], r=[], w=[("c_posi", gi)], sem="posload", allow_slow_non_contiguous=True)
        self.cp("dve", self.pos_f.ap, self.pos_i.ap, r=[("c_posi", g) for g in range(4)], w=[("c_posf",)])

        a = self.arena
        msc = a.mark()
        angb = a.alloc("ang", [NT * 16], F32)
        ab = a.alloc("ang_a", [NT * 16], F32)
        kib = a.alloc("ang_ki", [NT * 16], I32)
        kfb = a.alloc("ang_kf", [NT * 16], F32)
        mb = a.alloc("ang_m", [NT * 16], F32)

        def table(dst_cos, dst_sin, posf, inv, nf):
            n = NT * nf
            v3 = lambda b_: b_.ap[:, 0:n].rearrange("p (t j) -> p t j", t=NT)
            ang, aa, ki, kf, mm_ = v3(angb), v3(ab), v3(kib), v3(kfb), v3(mb)
            a0 = posf.unsqueeze(2).to_broadcast([128, NT, nf])
            a1 = inv.unsqueeze(1).to_broadcast([128, NT, nf])
            self.tt("dve", ang, a0, a1, ALU.mult, r=[("c_posf",), ("c_invm",), ("c_invd",)], w=[angb.k()])
            for dst, shift in ((dst_sin, 0.0), (dst_cos, 0.5 * PI)):
                self.ts("dve", aa, ang, shift, None, ALU.add, None, r=[angb.k()], w=[ab.k()])
                self.ts("dve", kf, aa, 1.0 / (2 * PI), None, ALU.mult, None, r=[ab.k()], w=[kfb.k()])
                self.cp("dve", ki, kf, r=[kfb.k()], w=[kib.k()])
                self.cp("dve", kf, ki, r=[kib.k()], w=[kfb.k()])
                self.stt(aa, kf, -2 * PI, aa, ALU.mult, ALU.add, r=[kfb.k(), ab.k()], w=[ab.k()])
                self.ts("dve", mm_, aa, PI, None, ALU.is_gt, None, r=[ab.k()], w=[mb.k()])
                self.stt(aa, mm_, -2 * PI, aa, ALU.mult, ALU.add, r=[mb.k(), ab.k()], w=[ab.k()])
                self.ts("dve", mm_, aa, -PI, None, ALU.is_lt, None, r=[ab.k()], w=[mb.k()])
                self.stt(aa, mm_, 2 * PI, aa, ALU.mult, ALU.add, r=[mb.k(), ab.k()], w=[ab.k()])
                self.ts("dve", aa, aa, 3.14159, -3.14159, ALU.min, ALU.max, r=[ab.k()], w=[ab.k()])
                self.act(dst, aa, AF.Copy if "nosin" in SK else AF.Sin, r=[ab.k()], w=[("c_tab",)])
        if "tab" not in SK:
            table(self.cos_m.ap, self.sin_m.ap, self.pos_f.ap[:, 0, :], self.inv_m.ap, 16)
        for g in range(3):
            if "tab" in SK:
                break
            table(self.cos_d.ap[:, g], self.sin_d.ap[:, g], self.pos_f.ap[:, g + 1, :], self.inv_d.ap, 8)
        a.release(msc)
        if "consts" in self.debug:
            self.dbg("cos_m", self.cos_m.ap, [128, NT, 16], F32, r=[("c_tab",)])
            self.dbg("sin_d", self.sin_d.ap, [128, 3, NT, 8], F32, r=[("c_tab",)])
            self.dbg("tri2", self.tri2.ap, [128, 256], BF16, r=[("c_tri",)])
            self.dbg("ident", self.ident_f.ap, [128, 128], F32, r=[("c_identf",)])
            self.dbg("ebase", self.ebase.ap, [128, 64], F32, r=[("c_ebase",)])
            self.dbg("ustrict", self.ustrict.ap, [128, 128], BF16, r=[("c_us",)])
        self.CONST_KEYS = [("c_identbf",), ("c_identf",), ("c_tri",), ("c_us",), ("c_ones",), ("c_onesbf",), ("c_nh",),
                           ("c_ebase",), ("c_tab",)]

    def x_rstd(self, tag):
        a = self.arena
        ss = a.alloc("ss" + tag, [NT], F32)
        rstd = a.alloc("rstd" + tag, [NT], F32)
        mj = a.mark()
        junk = a.alloc("junk" + tag, [D], BF16)
        for t in range(NT):
            self.act(junk.ap, self.x_tok[:, t, :], AF.Square, r=[("x", t)], w=[ss.k(t), junk.k()], accum_out=ss.ap[:, t:t + 1])
        a.release(mj)
        self.ts("dve", ss.ap, ss.ap, 1.0 / D, EPS, ALU.mult, ALU.add, r=ss.ks(range(NT)), w=[ss.k("ms")])
        self.act(ss.ap, ss.ap, AF.Sqrt, r=[ss.k("ms")], w=[ss.k("ms")])
        p_ = self.p
        p_.add("dve", lambda e: e.reciprocal(out=rstd.ap, in_=ss.ap), r=[ss.k("ms")], w=[rstd.k()])
        return rstd

    def gain_T(self, tag, d_g, li, nchunk, mode="nat"):
        gT = self.arena.alloc("gT" + tag, [nchunk], F32)
        if mode == "nat":
            src = d_g[li].rearrange("(c p) -> p c", p=128)
        else:
            src = d_g[li].rearrange("(p c) -> p c", p=128)
        self.dma("sp", gT.ap, src, r=[], w=[gT.k()], sem="gT" + tag, allow_slow_non_contiguous=True)
        return gT

    def norm_T_tile(self, t, rstd, gT, htok, dstT, dst_cols, psb, dst_keys):
        self.act(htok.ap, self.x_tok[:, t, :], AF.Copy, r=[("x", t), rstd.k()], w=[htok.k()], scale=rstd.ap[:, t:t + 1])
        pb = self.bank_bf(psb)
        for c in range(8):
            self.tr(pb[:, c * 128:(c + 1) * 128], htok.ap[:, c * 128:(c + 1) * 128], self.ident_bf.ap,
                    r=[htok.k(), ("c_identbf",)], w=[("ps", psb)])
        self.tt("dve", dstT[:, :, dst_cols], pb.rearrange("p (c t) -> p c t", c=8),
                gT.ap.unsqueeze(2).to_broadcast([128, 8, 128]), ALU.mult, r=[("ps", psb), gT.k()], w=dst_keys)

    def layer(self, li):
        L = self.first_layer + li
        a = self.arena
        m_layer = a.mark()
        self.oaT = a.alloc("oaT", [4 * S], F32)
        self.oaT_ap = self.oaT.ap.bitcast(BF16).rearrange("p (c s) -> p c s", c=8)
        self.acc_ap = self.oaT.ap.rearrange("p (h s) -> p h s", h=4)
        self.obT = a.alloc("obT", [2, S], BF16)
        self.mixer(li)
        a.release(m_layer)
        if self.debug.get("stop") in ("p1", "dil", "lat", "mla", "mixer"):
            return
        m = a.mark()
        self.moe(li)
        a.release(m)
        if self.debug.get("stop") == "moe":
            return
        m = a.mark()
        self.ple(li)
        a.release(m)

    def mixer(self, li):
        p = self.p
        a = self.arena
        rstd = self.x_rstd("mix")
        gT = self.gain_T("mix", self.d_g_mix, li, 8)
        self.rstd_mix = rstd
        self.gT_mix = gT
        m_lat = a.mark_hi()
        m1 = a.mark()
        hT = a.alloc("hT", [8, S], BF16)
        htok = [a.alloc(f"htok{i}", [D], BF16) for i in range(2)]
        for t in range(NT):
            self.norm_T_tile(t, rstd, gT, htok[t % 2], hT.ap, slice(t * 128, (t + 1) * 128), 6 + t % 2, [hT.k(t)])
        if "hT" in self.debug:
            self.dbg("hT", hT.ap, [128, 8, S], BF16, r=hT.ks(range(NT)))
        if self.debug.get("stop") == "p1":
            a.release(m1)
            return
        self.dilated(li, hT)
        if self.debug.get("stop") == "dil":
            for ch in self.dil_norm_chunks:
                ch()
            if "obT" in self.debug:
                self.dbg("obT", self.obT.ap, [128, 2, S], BF16, r=self.obT.ks([(pp, tc) for pp in range(2) for tc in range(4)]))
            a.release(m1)
            a.release_hi(m_lat)
            return
        hqT = a.alloc("hqT", [4, S], BF16, top=True)
        hkvT = a.alloc("hkvT", [2, S], BF16, top=True)
        krot = a.alloc("krot", [NT, 32], BF16, top=True)
        self.latents(li, hT, hqT, hkvT, krot)
        if "obT" in self.debug:
            self.dbg("obT", self.obT.ap, [128, 2, S], BF16, r=self.obT.ks([(pp, tc) for pp in range(2) for tc in range(4)]))
        a.release(m1)
        if self.debug.get("stop") == "lat":
            a.release_hi(m_lat)
            return
        self.p.alias(self.oaT.ks([(c, q, i) for c in range(8) for q in range(4) for i in range(2)]), self.oaT.ks([("acc", hh) for hh in range(4)]))
        self.mla(li, hqT, hkvT, krot)
        a.release_hi(m_lat)
        if self.debug.get("stop") == "mla":
            return
        self.merge(li)

    def dilated(self, li, hT):
        p = self.p
        a = self.arena
        m0 = a.mark()
        acc_ap = self.acc_ap
        akey = lambda hh: self.oaT.k(("acc", hh))
        wd = a.alloc("wdil", [8, 768], BF16)
        qkT = a.alloc("dqkT", [4, S], BF16)
        dv = a.alloc("dv", [NT, 4, 128], BF16)
        qk_tok = [a.alloc(f"dqk_tok{i}", [8, 64], BF16) for i in range(2)]
        tmp = [a.alloc(f"dtmp{i}", [8, 8], F32) for i in range(4)]
        pT = [a.alloc(f"dpT{i}", [256], BF16) for i in range(3)]
        w_in = self.d_w_in[li]
        self.memset("pool", dv.ap[:, :, :, 64:128], 1.0, w=[dv.k("ones")])
        for g, d in enumerate((1, 4, 16)):
            if "dil_g1" in self.debug.get("skip", "") and g > 0:
                break
            nb = NT // d
            c0 = 800 + g * 768
            self.dma("pool", wd.ap, w_in[:, c0:c0 + 768].rearrange("(c p) w -> p c w", p=128), r=[], w=[wd.k()], sem="wdil")
            cosg = self.cos_d.ap[:, g]
            sing = self.sin_d.ap[:, g]
            def dj_mm(u):
                r_, j_ = divmod(u, nb)
                st = r_ + d * 128 * j_
                tok = slice(st, st + d * 127 + 1, d)
                pa, pb_ = 0 + (u % 2) * 3, 1 + (u % 2) * 3
                for c in range(8):
                    self.mm(self.bank(pa), hT.ap[:, c, tok], wd.ap[:, c, 0:512], c == 0, c == 7,
                            r=hT.ks(range(NT)) + [wd.k()], w=[("ps", pa)])
                for c in range(8):
                    self.mm(self.bank(pb_)[:, 0:256], hT.ap[:, c, tok], wd.ap[:, c, 512:768], c == 0, c == 7,
                            r=hT.ks(range(NT)) + [wd.k()], w=[("ps", pb_)])

            def dj_ew(u):
                pa, pb_ = 0 + (u % 2) * 3, 1 + (u % 2) * 3
                qt = qk_tok[u % 2]
                pav = self.bank(pa).rearrange("p (h e) -> p h e", h=8)
                cb = cosg[:, u, :].unsqueeze(1).to_broadcast([128, 8, 8])
                sb = sing[:, u, :].unsqueeze(1).to_broadcast([128, 8, 8])
                x1 = pav[:, :, 0:8]
                x2 = pav[:, :, 8:16]
                t0, t1, t2, t3 = [x.ap for x in tmp]
                ck = [("c_tab",)]
                self.cp("dve", qt.ap[:, :, 16:64], pav[:, :, 16:64], r=[("ps", pa)], w=[qt.k("rest")])
                self.tt("dve", t0, x1, cb, ALU.mult, r=[("ps", pa)] + ck, w=[tmp[0].k()])
                self.tt("dve", t1, x2, sb, ALU.mult, r=[("ps", pa)] + ck, w=[tmp[1].k()])
                self.tt("dve", t2, x1, sb, ALU.mult, r=[("ps", pa)] + ck, w=[tmp[2].k()])
                self.tt("dve", t3, x2, cb, ALU.mult, r=[("ps", pa)] + ck, w=[tmp[3].k()])
                self.tt("pool", qt.ap[:, :, 0:8], t0, t1, ALU.subtract, r=[tmp[0].k(), tmp[1].k()], w=[qt.k("r1")])
                self.tt("pool", qt.ap[:, :, 8:16], t2, t3, ALU.add, r=[tmp[2].k(), tmp[3].k()], w=[qt.k("r2")])
                self.cp("act", dv.ap[:, u, :, 0:64], self.bank(pb_)[:, 0:256].rearrange("p (h e) -> p h e", h=4),
                        r=[("ps", pb_)], w=[dv.k(u)])

            def dj_tr(u):
                pt = 2 + (u % 2) * 3
                qt = qk_tok[u % 2]
                ptb = self.bank_bf(pt)
                qflat = qt.ap.rearrange("p h e -> p (h e)")
                for i4 in range(4):
                    self.tr(ptb[:, i4 * 128:(i4 + 1) * 128], qflat[:, i4 * 128:(i4 + 1) * 128], self.ident_bf.ap,
                            r=[qt.k("rest"), qt.k("r1"), qt.k("r2"), ("c_identbf",)], w=[("ps", pt)])
                self.cp("act", qkT.ap[:, :, u * 128:(u + 1) * 128], ptb[:, 0:512].rearrange("p (c t) -> p c t", c=4),
                        r=[("ps", pt)], w=[qkT.k(u)])

            for u in range(NT + 2):
                if u < NT:
                    dj_mm(u)
                if 0 <= u - 1 < NT:
                    dj_ew(u - 1)
                if 0 <= u - 2 < NT:
                    dj_tr(u - 2)
            for hh in range(4):
                if "dil_noattn" in self.debug.get("skip", ""):
                    break
                pp, pb0 = hh // 2, (hh % 2) * 64

                def a_qk(u):
                    j_ = u % nb
                    ncols = 256 if j_ + 1 < nb else 128
                    sb_ = 6 + (u % 2)
                    self.mm(self.bank(sb_)[:, 0:ncols], qkT.ap[pb0:pb0 + 64, 2 + pp, u * 128:(u + 1) * 128],
                            qkT.ap[pb0:pb0 + 64, pp, u * 128:u * 128 + ncols], True, True,
                            r=[qkT.k(u), qkT.k(u + 1)] if ncols == 256 else [qkT.k(u)], w=[("ps", sb_)])

                def a_exp(u):
                    j_ = u % nb
                    ncols = 256 if j_ + 1 < nb else 128
                    sb_ = 6 + (u % 2)
                    P = pT[u % 3]
                    self.act(P.ap[:, 0:ncols], self.bank(sb_)[:, 0:ncols], AF.Exp, r=[("ps", sb_)], w=[P.k()], scale=0.125)
                    self.tt("pool", P.ap[:, 0:ncols], P.ap[:, 0:ncols], self.tri2.ap[:, 0:ncols], ALU.mult,
                            r=[P.k(), ("c_tri",)], w=[P.k()])

                def a_pv(u):
                    j_ = u % nb
                    P = pT[u % 3]
                    ob = 4 + (u // 4) % 2
                    oc = (u % 4) * 128
                    oap = self.bank(ob)[:, oc:oc + 128]
                    if j_ > 0:
                        prevP = pT[(u - 1) % 3]
                        self.mm(oap, dv.ap[:, u - 1, hh, :], prevP.ap[:, 128:256], True, False,
                                r=[dv.k(u - 1), dv.k("ones"), prevP.k()], w=[("ps", ob)])
                    self.mm(oap, dv.ap[:, u, hh, :], P.ap[:, 0:128], j_ == 0, True,
                            r=[dv.k(u), dv.k("ones"), P.k()], w=[("ps", ob)])
                    if u % 4 == 3:
                        u0 = u - 3
                        src = self.bank(ob)
                        if d == 1:
                            dst = acc_ap[:, hh, u0 * 128:(u0 + 4) * 128]
                        elif d == 4:
                            r_ = u0 // nb
                            dst = acc_ap[:, hh, r_:r_ + 4 * 511 + 1:4]
                        else:
                            dst = acc_ap[:, hh, :].rearrange("p (i r) -> p r i", r=16)[:, u0:u0 + 4, :]
                            src = src.rearrange("p (r i) -> p r i", r=4)
                        if g == 0:
                            self.cp("dve", dst, src, r=[("ps", ob)], w=[akey(hh)])
                        else:
                            self.tt("dve", dst, src, dst, ALU.add, r=[("ps", ob), akey(hh)], w=[akey(hh)])

                for u in range(NT):
                    a_qk(u)
                    if u >= 1:
                        a_pv(u - 1)
                    a_exp(u)
                a_pv(NT - 1)
        rdn = [a.alloc("drdn0", [512], F32, top=True)] * 2
        chunks = []
        for hh in range(4):
            pp, pb0 = hh // 2, (hh % 2) * 64
            for tc in range(4):
                def chunk(hh=hh, tc=tc, pp=pp, pb0=pb0):
                    rd = rdn[(hh * 4 + tc) % 2]
                    cs = slice(tc * 512, (tc + 1) * 512)
                    p.add("dve", (lambda e: e.reciprocal(out=rd.ap[0:64, :], in_=acc_ap[64:128, hh, cs])), r=[akey(hh)], w=[rd.k()])
                    self.tt("dve", self.obT.ap[pb0:pb0 + 64, pp, cs], acc_ap[0:64, hh, cs], rd.ap[0:64, :], ALU.mult,
                            r=[akey(hh), rd.k()], w=[self.obT.k((pp, tc))])
                chunks.append(chunk)
        self.dil_norm_chunks = chunks
        a.release(m0)

    def latents(self, li, hT, hqT, hkvT, krot):
        p = self.p
        a = self.arena
        m0 = a.mark()
        wl = a.alloc("wlat", [8, 800], BF16)
        gq = self.gain_T("q", self.d_g_q, li, 4)
        gkv = self.gain_T("kv", self.d_g_kv, li, 2)
        st = a.alloc("lstat", [NT, 4], F32)
        junk = a.alloc("ljunk", [512], BF16)
        hq_tok = [a.alloc(f"hq_tok{i}", [768], BF16) for i in range(2)]
        tmp = [a.alloc(f"ltmp{i}", [16], F32) for i in range(4)]
        self.dma("pool", wl.ap, self.d_w_in[li][:, 0:800].rearrange("(c p) w -> p c w", p=128), r=[], w=[wl.k()], sem="wlat")
        def l_mm(t):
            pa, pb_ = 0 + (t % 2) * 3, 1 + (t % 2) * 3
            tok = slice(t * 128, (t + 1) * 128)
            for c in range(8):
                self.mm(self.bank(pa), hT.ap[:, c, tok], wl.ap[:, c, 0:512], c == 0, c == 7, r=[hT.k(t), wl.k()], w=[("ps", pa)])
            for c in range(8):
                self.mm(self.bank(pb_)[:, 0:288], hT.ap[:, c, tok], wl.ap[:, c, 512:800], c == 0, c == 7, r=[hT.k(t), wl.k()], w=[("ps", pb_)])

        def l_ew(t):
            pa, pb_ = 0 + (t % 2) * 3, 1 + (t % 2) * 3
            self.act(junk.ap, self.bank(pa), AF.Square, r=[("ps", pa)], w=[st.k((t, 0))], accum_out=st.ap[:, t, 0:1])
            self.act(junk.ap[:, 0:256], self.bank(pb_)[:, 0:256], AF.Square, r=[("ps", pb_)], w=[st.k((t, 1))], accum_out=st.ap[:, t, 1:2])
            self.ts("dve", st.ap[:, t, 0:1], st.ap[:, t, 0:1], 1.0 / 512, EPS, ALU.mult, ALU.add, r=[st.k((t, 0))], w=[st.k((t, 0))])
            self.ts("dve", st.ap[:, t, 1:2], st.ap[:, t, 1:2], 1.0 / 256, EPS, ALU.mult, ALU.add, r=[st.k((t, 1))], w=[st.k((t, 1))])
            self.act(st.ap[:, t, 0:2], st.ap[:, t, 0:2], AF.Sqrt, r=[st.k((t, 0)), st.k((t, 1))], w=[st.k((t, 0)), st.k((t, 1))])
            p.add("dve", (lambda e, t=t: e.reciprocal(out=st.ap[:, t, 2:4], in_=st.ap[:, t, 0:2])), r=[st.k((t, 0)), st.k((t, 1))], w=[st.k((t, 2))])
            hq = hq_tok[t % 2]
            self.ts("dve", hq.ap[:, 0:512], self.bank(pa), st.ap[:, t, 2:3], None, ALU.mult, None, r=[("ps", pa), st.k((t, 2))], w=[hq.k(0)])
            self.ts("dve", hq.ap[:, 512:768], self.bank(pb_)[:, 0:256], st.ap[:, t, 3:4], None, ALU.mult, None, r=[("ps", pb_), st.k((t, 2))], w=[hq.k(1)])
            x1 = self.bank(pb_)[:, 256:272]
            x2 = self.bank(pb_)[:, 272:288]
            cb = self.cos_m.ap[:, t, :]
            sb = self.sin_m.ap[:, t, :]
            ck = [("c_tab",), ("ps", pb_)]
            self.tt("dve", tmp[0].ap, x1, cb, ALU.mult, r=ck, w=[tmp[0].k()])
            self.tt("dve", tmp[1].ap, x2, sb, ALU.mult, r=ck, w=[tmp[1].k()])
            self.tt("dve", tmp[2].ap, x1, sb, ALU.mult, r=ck, w=[tmp[2].k()])
            self.tt("dve", tmp[3].ap, x2, cb, ALU.mult, r=ck, w=[tmp[3].k()])
            self.tt("pool", krot.ap[:, t, 0:16], tmp[0].ap, tmp[1].ap, ALU.subtract, r=[tmp[0].k(), tmp[1].k()], w=[krot.k((t, 0))])
            self.tt("pool", krot.ap[:, t, 16:32], tmp[2].ap, tmp[3].ap, ALU.add, r=[tmp[2].k(), tmp[3].k()], w=[krot.k((t, 1))])

        def l_tr(t):
            pt = 2 + (t % 2) * 3
            tok = slice(t * 128, (t + 1) * 128)
            hq = hq_tok[t % 2]
            ptb = self.bank_bf(pt)
            for c in range(6):
                self.tr(ptb[:, c * 128:(c + 1) * 128], hq.ap[:, c * 128:(c + 1) * 128], self.ident_bf.ap,
                        r=[hq.k(0), hq.k(1), ("c_identbf",)], w=[("ps", pt)])
            self.tt("dve", hqT.ap[:, :, tok], ptb[:, 0:512].rearrange("p (c t) -> p c t", c=4),
                    gq.ap.unsqueeze(2).to_broadcast([128, 4, 128]), ALU.mult, r=[("ps", pt), gq.k()], w=[hqT.k(t)])
            self.tt("dve", hkvT.ap[:, :, tok], ptb[:, 512:768].rearrange("p (c t) -> p c t", c=2),
                    gkv.ap.unsqueeze(2).to_broadcast([128, 2, 128]), ALU.mult, r=[("ps", pt), gkv.k()], w=[hkvT.k(t)])

        for t in range(NT + 2):
            if t < NT:
                l_mm(t)
            if 0 <= t - 1 < NT:
                l_ew(t - 1)
            if 0 <= t - 2 < NT:
                l_tr(t - 2)
            if t < NT:
                self.dil_norm_chunks[t]()
        if "hqT" in self.debug:
            self.dbg("hqT", hqT.ap, [128, 4, S], BF16, r=hqT.ks(range(NT)))
            self.dbg("hkvT", hkvT.ap, [128, 2, S], BF16, r=hkvT.ks(range(NT)))
            self.dbg("krot", krot.ap, [128, NT, 32], BF16, r=krot.ks([(t, i) for t in range(NT) for i in range(2)]))
        a.release(m0)

    def mla(self, li, hqT, hkvT, krot):
        p = self.p
        a = self.arena
        m0 = a.mark()
        G = 4
        qkT = a.alloc("qkT", [2 * G, S], BF16)
        v = a.alloc("v", [NT, G, 128], BF16)
        wq = a.alloc("wq", [4, G * 96], BF16)
        wkv = a.alloc("wkv", [2, G * 128], BF16)
        q_tok = [a.alloc(f"q_tok{i}", [2 * G, 96], BF16) for i in range(2)]
        tmp = [a.alloc(f"mtmp{i}", [G, 16], F32) for i in range(4)]
        pT = [a.alloc(f"pT{i}", [1024], BF16) for i in range(3)]
        rden = [a.alloc("rden0", [512], F32)] * 2
        scale = 96.0 ** -0.5
        krot_keys = krot.ks([(t, i) for t in range(NT) for i in range(2)])
        cnt = 0
        for gg in range(16 // G):
            self.dma("pool", wq.ap, self.d_w_q[li][:, gg * G * 96:(gg + 1) * G * 96].rearrange("(c p) w -> p c w", p=128), r=[], w=[wq.k()], sem="wq")
            self.dma("pool", wkv.ap, self.d_w_kv[li][:, gg * G * 128:(gg + 1) * G * 128].rearrange("(c p) w -> p c w", p=128), r=[], w=[wkv.k()], sem="wkv")
            self.memset("pool", v.ap[:, :, :, 64:128], 1.0, w=[v.k("ones")])
            def pj_mm(t):
                pa, pb_ = 0 + (t % 2) * 3, 1 + (t % 2) * 3
                tok = slice(t * 128, (t + 1) * 128)
                for c in range(4):
                    self.mm(self.bank(pa)[:, 0:G * 96], hqT.ap[:, c, tok], wq.ap[:, c, :], c == 0, c == 3, r=[hqT.k(t), wq.k()], w=[("ps", pa)])
                for c in range(2):
                    self.mm(self.bank(pb_), hkvT.ap[:, c, tok], wkv.ap[:, c, :], c == 0, c == 1, r=[hkvT.k(t), wkv.k()], w=[("ps", pb_)])

            def pj_ew(t):
                pa, pb_ = 0 + (t % 2) * 3, 1 + (t % 2) * 3
                qt = q_tok[t % 2]
                qv = self.bank(pa)[:, 0:G * 96].rearrange("p (h e) -> p h e", h=G)
                kvv = self.bank(pb_).rearrange("p (h e) -> p h e", h=G)
                self.cp("act", qt.ap[:, G:2 * G, 0:64], kvv[:, :, 0:64], r=[("ps", pb_)], w=[qt.k("kn")])
                self.cp("act", v.ap[:, t, :, 0:64], kvv[:, :, 64:128], r=[("ps", pb_)], w=[v.k(t)])
                self.cp("pool", qt.ap[:, G:2 * G, 64:96], krot.ap[:, t, :].unsqueeze(1).to_broadcast([128, G, 32]), r=krot_keys, w=[qt.k("kr")])
                cb = self.cos_m.ap[:, t, :].unsqueeze(1).to_broadcast([128, G, 16])
                sb = self.sin_m.ap[:, t, :].unsqueeze(1).to_broadcast([128, G, 16])
                x1 = qv[:, :, 64:80]
                x2 = qv[:, :, 80:96]
                ck = [("c_tab",), ("ps", pa)]
                self.cp("dve", qt.ap[:, 0:G, 0:64], qv[:, :, 0:64], r=[("ps", pa)], w=[qt.k("qn")])
                self.tt("dve", tmp[0].ap, x1, cb, ALU.mult, r=ck, w=[tmp[0].k()])
                self.tt("dve", tmp[1].ap, x2, sb, ALU.mult, r=ck, w=[tmp[1].k()])
                self.tt("dve", tmp[2].ap, x1, sb, ALU.mult, r=ck, w=[tmp[2].k()])
                self.tt("dve", tmp[3].ap, x2, cb, ALU.mult, r=ck, w=[tmp[3].k()])
                self.tt("pool", qt.ap[:, 0:G, 64:80], tmp[0].ap, tmp[1].ap, ALU.subtract, r=[tmp[0].k(), tmp[1].k()], w=[qt.k("r1")])
                self.tt("pool", qt.ap[:, 0:G, 80:96], tmp[2].ap, tmp[3].ap, ALU.add, r=[tmp[2].k(), tmp[3].k()], w=[qt.k("r2")])

            def pj_tr(t):
                pt = 2 + (t % 2) * 3
                tok = slice(t * 128, (t + 1) * 128)
                qt = q_tok[t % 2]
                ptb = self.bank_bf(pt)
                for hx in range(2 * G):
                    self.tr(ptb[0:96, hx * 128:(hx + 1) * 128], qt.ap[:, hx, :], self.ident_bf.ap,
                            r=[qt.k("qn"), qt.k("kn"), qt.k("kr"), qt.k("r1"), qt.k("r2"), ("c_identbf",)], w=[("ps", pt)])
                self.cp("act", qkT.ap[0:96, :, tok], ptb[0:96, :].rearrange("p (c t) -> p c t", c=2 * G), r=[("ps", pt)], w=[qkT.k(t)])

            for t in range(NT + 2):
                if t < NT:
                    pj_mm(t)
                if 0 <= t - 1 < NT:
                    pj_ew(t - 1)
                if 0 <= t - 2 < NT:
                    pj_tr(t - 2)
            if "dqkT" in self.debug:
                pass
            if "qkT" in self.debug and gg == 0:
                self.dbg("qkT", qkT.ap, [128, 2 * G, S], BF16, r=qkT.ks(range(NT)))
                self.dbg("v", v.ap, [128, NT, G, 128], BF16, r=v.ks(list(range(NT)) + ["ones"]))
            steps = [(hl, qc, kp) for hl in range(G) for qc in range(4) for kp in range((4 * qc + 4) // 2)]

            def qk(i):
                hl, qc, kp = steps[i]
                sbk = 2 * (i % 3)
                qkeys = qkT.ks(range(4 * qc, 4 * qc + 4))
                for half in range(2):
                    kt = 2 * kp + half
                    r_ = max(0, kt - 4 * qc)
                    self.mm(self.bank(sbk + half)[:, 128 * r_:512], qkT.ap[0:96, G + hl, kt * 128:(kt + 1) * 128],
                            qkT.ap[0:96, hl, qc * 512 + 128 * r_:(qc + 1) * 512], True, True,
                            r=qkeys + [qkT.k(kt)], w=[("ps", sbk + half)])

            def expm(i):
                hl, qc, kp = steps[i]
                sbk = 2 * (i % 3)
                P = pT[i % 3]
                self.act(P.ap, self.bank(sbk, 2), AF.Exp, r=[("ps", sbk), ("ps", sbk + 1)], w=[P.k()], scale=scale)
                for half in range(2):
                    kt = 2 * kp + half
                    r_ = kt - 4 * qc
                    if r_ >= 0:
                        dg = P.ap[:, half * 512 + 128 * r_: half * 512 + 128 * r_ + 128]
                        self.tt("pool", dg, dg, self.tri2.ap[:, 0:128], ALU.mult, r=[P.k(), ("c_tri",)], w=[P.k()])

            def pv(i):
                hl, qc, kp = steps[i]
                h = gg * G + hl
                cc, pb0 = h // 2, (h % 2) * 64
                nk = 4 * qc + 4
                hq = hl * 4 + qc
                ob = 6 + hq % 2
                o_ps = self.bank(ob)
                P = pT[i % 3]
                for half in range(2):
                    kt = 2 * kp + half
                    r_ = max(0, kt - 4 * qc)
                    self.mm(o_ps[:, 128 * r_:512], v.ap[:, kt, hl, :], P.ap[:, half * 512 + 128 * r_:(half + 1) * 512],
                            kt == 0, kt == nk - 1, r=[v.k(kt), v.k("ones"), P.k()], w=[("ps", ob)])
                if kp == nk // 2 - 1:
                    rd = rden[hq % 2]
                    qcols = slice(qc * 512, (qc + 1) * 512)
                    p.add("dve", (lambda e, rd=rd, o_ps=o_ps: e.reciprocal(out=rd.ap[0:64, :], in_=o_ps[64:128, :])), r=[("ps", ob)], w=[rd.k(0), rd.k(1)])
                    self.tt("dve", self.oaT_ap[pb0:pb0 + 64, cc, qcols], o_ps[0:64, :], rd.ap[0:64, :], ALU.mult,
                            r=[("ps", ob), rd.k(0), rd.k(1)], w=[self.oaT.k((cc, qc, h % 2))])

            ns_ = len(steps)
            qk(0)
            qk(1)
            expm(0)
            for i in range(ns_):
                if i + 2 < ns_:
                    qk(i + 2)
                pv(i)
                if i + 1 < ns_:
                    expm(i + 1)
        if "oaT" in self.debug:
            self.dbg("oaT", self.oaT_ap, [128, 8, S], BF16, r=self.oaT.ks([(c, q, i) for c in range(8) for q in range(4) for i in range(2)]))
        a.release(m0)

    def merge(self, li):
        p = self.p
        a = self.arena
        m0 = a.mark()
        wba = a.alloc("wba", [8, D], BF16)
        wbb = a.alloc("wbb", [2, D], BF16)
        wg = a.alloc("wg", [8, 2 * D], BF16)
        wo = a.alloc("wo", [8, D], BF16)
        hTc = a.alloc("hTc", [8, 512], BF16)
        mT = a.alloc("mT", [8, 512], BF16)
        htok = [a.alloc("mhtok", [D], BF16)] * 2
        sa = a.alloc("sa", [512], BF16)
        sb = a.alloc("sb", [512], BF16)
        m1 = a.alloc("m1", [512], F32)
        m2 = a.alloc("m2", [512], F32)
        self.dma("pool", wba.ap, self.d_w_ba[li].rearrange("(c p) w -> p c w", p=128), r=[], w=[wba.k()], sem="wba")
        self.dma("pool", wbb.ap, self.d_w_bb[li].rearrange("(c p) w -> p c w", p=128), r=[], w=[wbb.k()], sem="wbb")
        for hf in range(2):
            self.dma("pool", wg.ap[:, :, hf * D:(hf + 1) * D], self.d_w_in[li][:, 3104 + hf * D:3104 + (hf + 1) * D].rearrange("(c p) w -> p c w", p=128),
                     r=[], w=[wg.k(hf)], sem=f"wg{hf}")
        self.dma("pool", wo.ap, self.d_w_out[li].rearrange("(c p) w -> p c w", p=128), r=[], w=[wo.k()], sem="wo")
        oa_keys = lambda tc: self.oaT.ks([(c, tc, i) for c in range(8) for i in range(2)])
        ob_keys = lambda tc: self.obT.ks([(pp, tc) for pp in range(2)])
        for tc in range(4):
            cs = slice(tc * 512, (tc + 1) * 512)
            for tl in range(4):
                t = tc * 4 + tl
                self.norm_T_tile(t, self.rstd_mix, self.gT_mix, htok[t % 2], hTc.ap, slice(tl * 128, (tl + 1) * 128), 7, [hTc.k(tl)])
            for f in range(8):
                fs = slice(f * 128, (f + 1) * 128)
                for c in range(8):
                    self.mm(self.bank(0), wba.ap[:, c, fs], self.oaT_ap[:, c, cs], c == 0, c == 7, r=[wba.k()] + oa_keys(tc), w=[("ps", 0)])
                for c in range(2):
                    self.mm(self.bank(1), wbb.ap[:, c, fs], self.obT.ap[:, c, cs], c == 0, c == 1, r=[wbb.k()] + ob_keys(tc), w=[("ps", 1)])
                for c in range(8):
                    self.mm(self.bank(2), wg.ap[:, c, fs], hTc.ap[:, c, :], c == 0, c == 7, r=[wg.k(0)] + hTc.ks(range(4)), w=[("ps", 2)])
                for c in range(8):
                    self.mm(self.bank(3), wg.ap[:, c, D + f * 128:D + (f + 1) * 128], hTc.ap[:, c, :], c == 0, c == 7, r=[wg.k(1)] + hTc.ks(range(4)), w=[("ps", 3)])
                self.act(sa.ap, self.bank(2), AF.Sigmoid, r=[("ps", 2)], w=[sa.k()])
                self.act(sb.ap, self.bank(3), AF.Sigmoid, r=[("ps", 3)], w=[sb.k()])
                self.tt("dve", m1.ap, self.bank(0), sa.ap, ALU.mult, r=[("ps", 0), sa.k()], w=[m1.k()])
                self.tt("dve", m2.ap, self.bank(1), sb.ap, ALU.mult, r=[("ps", 1), sb.k()], w=[m2.k()])
                self.tt("pool", mT.ap[:, f, :], m1.ap, m2.ap, ALU.add, r=[m1.k(), m2.k()], w=[mT.k(f)])
            if "mT" in self.debug and tc == 0:
                self.dbg("mT", mT.ap, [128, 8, 512], BF16, r=mT.ks(range(8)))
            for tl in range(4):
                t = tc * 4 + tl
                for hf in range(2):
                    ob = 4 + (2 * tl + hf) % 2
                    for f in range(8):
                        self.mm(self.bank(ob), mT.ap[:, f, tl * 128:(tl + 1) * 128], wo.ap[:, f, hf * 512:(hf + 1) * 512], f == 0, f == 7,
                                r=mT.ks(range(8)) + [wo.k()], w=[("ps", ob)])
                    xs_ = self.x_tok[:, t, hf * 512:(hf + 1) * 512]
                    self.tt("dve", xs_, xs_, self.bank(ob), ALU.add, r=[("x", t), ("ps", ob)], w=[("x", t)])
        a.release(m0)

    def moe(self, li):
        p = self.p
        a = self.arena
        m0 = a.mark()
        rstd = self.x_rstd("ffn")
        gT = self.gain_T("ffn", self.d_g_ffn, li, 8)
        gT8 = self.gain_T("ffn8", self.d_g_ffn, li, 8, mode="p8")
        xh = a.alloc("xh", [NT, D], BF16)
        gw1 = a.alloc("gw1", [NT], F32)
        gw2 = a.alloc("gw2", [NT], F32)
        di1 = a.alloc("di1", [NT], I32)
        di2 = a.alloc("di2", [NT], I32)
        m_route = a.mark()
        wr = a.alloc("wr", [8, 72], F32)
        brb = a.alloc("brb", [72], F32)
        lt = a.alloc("lt", [NT, 72], F32)
        x32 = [a.alloc(f"x32_{i}", [D], F32) for i in range(2)]
        h2T = [a.alloc(f"h2T{i}", [8, 128], F32) for i in range(2)]
        self.dma("sp", wr.ap[:, :, 0:8], self.d_w_rg[li].rearrange("(c p) w -> p c w", p=128), r=[], w=[wr.k(0)], sem="wr0", allow_slow_non_contiguous=True)
        self.dma("sp", wr.ap[:, :, 8:72], self.d_w_re[li].rearrange("(c p) w -> p c w", p=128), r=[], w=[wr.k(1)], sem="wr1", allow_slow_non_contiguous=True)
        self.dma("sp", brb.ap[:, 0:8], self.d_b_rg[li:li + 1, :].to_broadcast([128, 8]), r=[], w=[brb.k(0)], sem="brb0", allow_slow_non_contiguous=True)
        self.dma("sp", brb.ap[:, 8:72], self.d_b_re[li:li + 1, :].to_broadcast([128, 64]), r=[], w=[brb.k(1)], sem="brb1", allow_slow_non_contiguous=True)
        for t in range(NT):
            self.act(xh.ap[:, t, :], self.x_tok[:, t, :], AF.Copy, r=[("x", t), rstd.k()], w=[xh.k(t)], scale=rstd.ap[:, t:t + 1])
            x3 = x32[t % 2]
            self.ts("dve", x3.ap, self.x_tok[:, t, :], rstd.ap[:, t:t + 1], None, ALU.mult, None, r=[("x", t), rstd.k()], w=[x3.k()])
            pa = 0 + 2 * (t % 2)
            for c in range(8):
                self.tr(self.bank(pa, 2)[:, c * 128:(c + 1) * 128], x3.ap[:, c * 128:(c + 1) * 128], self.ident_f.ap,
                        r=[x3.k(), ("c_identf",)], w=[("ps", pa), ("ps", pa + 1)])
            hT_ = h2T[t % 2]
            self.tt("dve", hT_.ap, self.bank(pa, 2).rearrange("p (c t) -> p c t", c=8), gT.ap.unsqueeze(2).to_broadcast([128, 8, 128]), ALU.mult,
                    r=[("ps", pa), ("ps", pa + 1), gT.k()], w=[hT_.k()])
            pr = 4 + (t % 2)
            for c in range(8):
                self.mm(self.bank(pr)[:, 0:72], hT_.ap[:, c, :], wr.ap[:, c, :], c == 0, c == 7, r=[hT_.k(), wr.k(0), wr.k(1)], w=[("ps", pr)])
            self.tt("dve", lt.ap[:, t, :], self.bank(pr)[:, 0:72], brb.ap, ALU.add, r=[("ps", pr), brb.k(0), brb.k(1)], w=[lt.k(t)])
        ltk = lt.ks(range(NT))
        al = lambda name, shape, dt=F32: a.alloc(name, shape, dt)
        ngmax = al("ngmax", [NT])
        oh_g = al("oh_g", [NT, 8])
        eg = al("eg", [NT, 8])
        sume = al("sume", [NT])
        pg = al("pg", [NT])
        tmp64 = al("tmp64", [NT, 8, 8])
        e_in = al("e_in", [NT, 8])
        mx1 = al("mx1", [NT])
        mk1 = al("mk1", [NT, 8])
        e2 = al("e2", [NT, 8])
        mx2 = al("mx2", [NT])
        mk2 = al("mk2", [NT, 8])
        dd = al("dd", [NT])
        sel1 = al("sel1", [NT, 8, 8])
        sel2 = al("sel2", [NT, 8, 8])
        selb = al("selb", [NT, 64], BF16)
        rank = al("rank", [NT, 64])
        rs1 = al("rs1", [NT])
        rs2 = al("rs2", [NT])
        d1 = al("d1", [NT])
        d2 = al("d2", [NT])
        lg = lt.ap[:, :, 0:8]
        le = lt.ap[:, :, 8:72].rearrange("p t (g e) -> p t g e", g=8)
        B3 = lambda ap_: ap_.unsqueeze(2).to_broadcast([128, NT, 8])
        V = "dve"
        p.add(V, lambda e: e.tensor_reduce(out=ngmax.ap, in_=lg, axis=AX.X, op=ALU.max, negate=True), r=ltk, w=[ngmax.k()])
        self.tt(V, eg.ap, lg, B3(ngmax.ap), ALU.add, r=ltk + [ngmax.k()], w=[eg.k()])
        self.ts(V, oh_g.ap, eg.ap, 0.0, None, ALU.is_equal, None, r=[eg.k()], w=[oh_g.k()])
        self.act(eg.ap, eg.ap, AF.Exp, r=[eg.k()], w=[eg.k()])
        p.add(V, lambda e: e.tensor_reduce(out=sume.ap, in_=eg.ap, axis=AX.X, op=ALU.add), r=[eg.k()], w=[sume.k()])
        p.add(V, lambda e: e.reciprocal(out=pg.ap, in_=sume.ap), r=[sume.k()], w=[pg.k()])
        self.tt(V, tmp64.ap, le, oh_g.ap.unsqueeze(3).to_broadcast([128, NT, 8, 8]), ALU.mult, r=ltk + [oh_g.k()], w=[tmp64.k()])
        p.add(V, lambda e: e.tensor_reduce(out=e_in.ap, in_=tmp64.ap.rearrange("p t g e -> p t e g"), axis=AX.X, op=ALU.add), r=[tmp64.k()], w=[e_in.k()])
        p.add(V, lambda e: e.tensor_reduce(out=mx1.ap, in_=e_in.ap, axis=AX.X, op=ALU.max), r=[e_in.k()], w=[mx1.k()])
        self.tt(V, mk1.ap, e_in.ap, B3(mx1.ap), ALU.is_equal, r=[e_in.k(), mx1.k()], w=[mk1.k()])
        self.stt(e2.ap, mk1.ap, -1e30, e_in.ap, ALU.mult, ALU.add, r=[mk1.k(), e_in.k()], w=[e2.k()])
        p.add(V, lambda e: e.tensor_reduce(out=mx2.ap, in_=e2.ap, axis=AX.X, op=ALU.max), r=[e2.k()], w=[mx2.k()])
        self.tt(V, mk2.ap, e2.ap, B3(mx2.ap), ALU.is_equal, r=[e2.k(), mx2.k()], w=[mk2.k()])
        self.tt(V, dd.ap, mx2.ap, mx1.ap, ALU.subtract, r=[mx1.k(), mx2.k()], w=[dd.k()])
        self.act(dd.ap, dd.ap, AF.Exp, r=[dd.k()], w=[dd.k()])
        self.ts(V, gw1.ap, dd.ap, 1.0, None, ALU.add, None, r=[dd.k()], w=[gw1.k()])
        p.add(V, lambda e: e.reciprocal(out=gw1.ap, in_=gw1.ap), r=[gw1.k()], w=[gw1.k()])
        self.tt(V, gw1.ap, gw1.ap, pg.ap, ALU.mult, r=[gw1.k(), pg.k()], w=[gw1.k()])
        self.tt(V, gw2.ap, gw1.ap, dd.ap, ALU.mult, r=[gw1.k(), dd.k()], w=[gw2.k()])
        ohb = oh_g.ap.unsqueeze(3).to_broadcast([128, NT, 8, 8])
        self.tt(V, sel1.ap, ohb, mk1.ap.unsqueeze(2).to_broadcast([128, NT, 8, 8]), ALU.mult, r=[oh_g.k(), mk1.k()], w=[sel1.k()])
        self.tt(V, sel2.ap, ohb, mk2.ap.unsqueeze(2).to_broadcast([128, NT, 8, 8]), ALU.mult, r=[oh_g.k(), mk2.k()], w=[sel2.k()])
        s1f = sel1.ap.rearrange("p t g e -> p t (g e)")
        s2f = sel2.ap.rearrange("p t g e -> p t (g e)")
        self.tt(V, selb.ap, s1f, s2f, ALU.add, r=[sel1.k(), sel2.k()], w=[selb.k()])
        for t in range(NT):
            rb = 6 + t // 8
            out = self.bank(rb)[:, (t % 8) * 64:(t % 8) * 64 + 64]
            self.mm(out, self.ustrict.ap, selb.ap[:, t, :], True, t == 0, r=[selb.k(), ("c_us",)], w=[("ps", rb)])
            for t2 in range(t):
                self.mm(out, self.ones_bf.ap, selb.ap[:, t2, :], False, t2 == t - 1, r=[selb.k(), ("c_onesbf",)], w=[("ps", rb)])
        self.cp(V, rank.ap, self.bank(6, 2).rearrange("p (t e) -> p t e", t=NT), r=[("ps", 6), ("ps", 7)], w=[rank.k()])
        ebb = self.ebase.ap.unsqueeze(1).to_broadcast([128, NT, 64])
        tf = tmp64.ap.rearrange("p t g e -> p t (g e)")
        for (sf, sk, rs, dst, dsti, gw) in ((s1f, sel1, rs1, d1, di1, gw1), (s2f, sel2, rs2, d2, di2, gw2)):
            self.tt(V, tf, sf, rank.ap, ALU.mult, r=[sk.k(), rank.k()], w=[tmp64.k()])
            p.add(V, (lambda e, rs=rs: e.tensor_reduce(out=rs.ap, in_=tf, axis=AX.X, op=ALU.add)), r=[tmp64.k()], w=[rs.k()])
            self.tt(V, tf, sf, ebb, ALU.mult, r=[sk.k(), ("c_ebase",)], w=[tmp64.k()])
            p.add(V, (lambda e, dst=dst: e.tensor_reduce(out=dst.ap, in_=tf, axis=AX.X, op=ALU.add)), r=[tmp64.k()], w=[dst.k()])
            self.tt(V, dst.ap, dst.ap, rs.ap, ALU.add, r=[dst.k(), rs.k()], w=[dst.k()])
            self.ts(V, rs.ap, rs.ap, float(CAP), None, ALU.is_ge, None, r=[rs.k()], w=[rs.k()])
            self.stt(dst.ap, rs.ap, 1.0e6, dst.ap, ALU.mult, ALU.add, r=[rs.k(), dst.k()], w=[dst.k()])
            self.cp(V, dsti.ap, dst.ap, r=[dst.k()], w=[dsti.k()])
        if "route" in self.debug:
            self.dbg("lt", lt.ap, [128, NT, 72], F32, r=ltk)
            self.dbg("d1", d1.ap, [128, NT], F32, r=[d1.k()])
            self.dbg("d2", d2.ap, [128, NT], F32, r=[d2.k()])
            self.dbg("gw1", gw1.ap, [128, NT], F32, r=[gw1.k()])
            self.dbg("gw2", gw2.ap, [128, NT], F32, r=[gw2.k()])
        xs_d = self.d_xs[li]
        ys_d = self.d_ys[li]
        for t in range(NT):
            for (dsti, nm) in ((di1, "a"), (di2, "b")):
                p.add("pool", (lambda e, t=t, dsti=dsti: e.indirect_dma_start(
                    out=xs_d, out_offset=bass.IndirectOffsetOnAxis(ap=dsti.ap[:, t:t + 1], axis=0),
                    in_=xh.ap[:, t, :], in_offset=None, bounds_check=self.bc_reg(e), oob_is_err=False)),
                    r=[xh.k(t), dsti.k()], w=[("xs", li, t, nm)], dma="scat")
        xs_keys = [("xs", li, t, nm) for t in range(NT) for nm in "ab"]
        a.release(m_route)
        m_exp = a.mark()
        NW, NW2, NX = 4, 6, 3
        w1 = [a.alloc(f"w1_{i}", [8, 256], BF16) for i in range(NW)]
        w3 = [a.alloc(f"w3_{i}", [8, 256], BF16) for i in range(NW)]
        w2 = [a.alloc(f"w2_{i}", [2, D], BF16) for i in range(NW2)]
        xse = [a.alloc(f"xse{i}", [D], BF16) for i in range(NX)]
        xsT = [a.alloc(f"xsT{i}", [8, 128], BF16) for i in range(2)]
        sl = [a.alloc(f"sl{i}", [256], F32) for i in range(2)]
        gg_ = [a.alloc(f"g{i}", [256], BF16) for i in range(2)]
        gT_ = [a.alloc(f"gT{i}", [2, 128], BF16) for i in range(2)]
        ye = [a.alloc(f"ye{i}", [D], F32) for i in range(2)]

        def st_load(ex):
            self.dma("pool", w1[ex % NW].ap, self.d_w1[li, ex].rearrange("(p c) f -> p c f", p=128), r=[], w=[w1[ex % NW].k()], sem=f"w1_{ex % NW}")
            self.dma("pool", w3[ex % NW].ap, self.d_w3[li, ex].rearrange("(p c) f -> p c f", p=128), r=[], w=[w3[ex % NW].k()], sem=f"w3_{ex % NW}")
            self.dma("pool", w2[ex % NW2].ap, self.d_w2[li, ex].rearrange("(p c) f -> p c f", p=128), r=[], w=[w2[ex % NW2].k()], sem=f"w2_{ex % NW2}")
            self.dma("sp", xse[ex % NX].ap, xs_d[ex * CAP:(ex + 1) * CAP, :], r=xs_keys, w=[xse[ex % NX].k()], sem=f"xse{ex % NX}")

        def st_a(ex):
            pt = ex % 2
            ptb = self.bank_bf(pt)
            xb = xse[ex % NX]
            for c in range(8):
                self.tr(ptb[:, c * 128:(c + 1) * 128], xb.ap[:, c:D:8], self.ident_bf.ap, r=[xb.k(), ("c_identbf",)], w=[("ps", pt)])
            self.tt("dve", xsT[ex % 2].ap, ptb.rearrange("p (c t) -> p c t", c=8), gT8.ap.unsqueeze(2).to_broadcast([128, 8, 128]), ALU.mult,
                    r=[("ps", pt), gT8.k()], w=[xsT[ex % 2].k()])

        def st_b(ex):
            s2 = ex % 2
            ph = 2 + s2
            for c in range(8):
                self.mm(self.bank(ph)[:, 0:256], xsT[s2].ap[:, c, :], w1[ex % NW].ap[:, c, :], c == 0, c == 7, r=[xsT[s2].k(), w1[ex % NW].k()], w=[("ps", ph)])
            for c in range(8):
                self.mm(self.bank(ph)[:, 256:512], xsT[s2].ap[:, c, :], w3[ex % NW].ap[:, c, :], c == 0, c == 7, r=[xsT[s2].k(), w3[ex % NW].k()], w=[("ps", ph)])
            self.act(sl[s2].ap, self.bank(ph)[:, 0:256], AF.Silu, r=[("ps", ph)], w=[sl[s2].k()])
            self.tt("dve", gg_[s2].ap, sl[s2].ap, self.bank(ph)[:, 256:512], ALU.mult, r=[sl[s2].k(), ("ps", ph)], w=[gg_[s2].k()])

        def st_c(ex):
            s2 = ex % 2
            pt = (ex + 1) % 2
            ptb = self.bank_bf(pt)
            for c in range(2):
                self.tr(ptb[:, c * 128:(c + 1) * 128], gg_[s2].ap[:, c:256:2], self.ident_bf.ap, r=[gg_[s2].k(), ("c_identbf",)], w=[("ps", pt)])
            self.cp("act", gT_[s2].ap, ptb[:, 0:256].rearrange("p (c t) -> p c t", c=2), r=[("ps", pt)], w=[gT_[s2].k()])

        def st_d(ex):
            s2 = ex % 2
            py = 4 + 2 * s2
            for hf in range(2):
                for c in range(2):
                    self.mm(self.bank(py + hf), gT_[s2].ap[:, c, :], w2[ex % NW2].ap[:, c, hf * 512:(hf + 1) * 512], c == 0, c == 1,
                            r=[gT_[s2].k(), w2[ex % NW2].k()], w=[("ps", py + hf)])
            self.cp("act", ye[s2].ap[:, 0:512], self.bank(py), r=[("ps", py)], w=[ye[s2].k(0)])
            self.cp("dve", ye[s2].ap[:, 512:1024], self.bank(py + 1), r=[("ps", py + 1)], w=[ye[s2].k(1)])
            self.dma("sp", ys_d[ex * CAP:(ex + 1) * CAP, :], ye[s2].ap, r=[ye[s2].k(0), ye[s2].k(1)], w=[("ys", li, ex)], sem=f"yst{s2}")

        for ex in range(min(NX - 1, NEXP)):
            st_load(ex)
        for i in range(NEXP + 4):
            if 0 <= i - 1 < NEXP:
                st_a(i - 1)
            if 0 <= i - 2 < NEXP:
                st_b(i - 2)
            if 0 <= i - 3 < NEXP:
                st_c(i - 3)
            if 0 <= i - 4 < NEXP:
                st_d(i - 4)
            if i + NX - 1 < NEXP:
                st_load(i + NX - 1)
        ys_keys = [("ys", li, ex) for ex in range(NEXP)]
        a.release(m_exp)
        y1 = [a.alloc(f"y1_{i}", [D], F32) for i in range(2)]
        y2 = [a.alloc(f"y2_{i}", [D], F32) for i in range(2)]
        for t in range(NT):
            for (yb, dsti, gw, nm) in ((y1[t % 2], di1, gw1, "a"), (y2[t % 2], di2, gw2, "b")):
                self.memset("pool", yb.ap, 0.0, w=[yb.k()])
                p.add("pool", (lambda e, t=t, yb=yb, dsti=dsti: e.indirect_dma_start(
                    out=yb.ap, out_offset=None, in_=ys_d, in_offset=bass.IndirectOffsetOnAxis(ap=dsti.ap[:, t:t + 1], axis=0),
                    bounds_check=self.bc_reg(e), oob_is_err=False)), r=ys_keys + [dsti.k(), yb.k()], w=[yb.k()], dma=f"gath{nm}{t % 2}")
                self.stt(self.x_tok[:, t, :], yb.ap, gw.ap[:, t:t + 1], self.x_tok[:, t, :], ALU.mult, ALU.add,
                         r=[yb.k(), gw.k(), ("x", t)], w=[("x", t)])
        a.release(m0)

    def ple(self, li):
        p = self.p
        a = self.arena
        m0 = a.mark()
        rstd = self.x_rstd("ple")
        gT = self.gain_T("ple", self.d_g_ple, li, 8)
        wpg = a.alloc("wpg", [8, D], BF16)
        wpp = a.alloc("wpp", [2, D], BF16)
        ptok = [a.alloc(f"ptok{i}", [256], BF16) for i in range(2)]
        pTt = [a.alloc(f"pT{i}", [2, 128], BF16) for i in range(2)]
        htok = [a.alloc(f"phtok{i}", [D], BF16) for i in range(2)]
        h3T = [a.alloc(f"h3T{i}", [8, 128], BF16) for i in range(2)]
        sg = [a.alloc(f"sg{i}", [512], F32) for i in range(2)]
        ge = [a.alloc(f"ge{i}", [512], F32) for i in range(2)]
        self.dma("pool", wpg.ap, self.d_w_pg[li].rearrange("(c p) w -> p c w", p=128), r=[], w=[wpg.k()], sem="wpg")
        self.dma("pool", wpp.ap, self.d_w_pp[li].rearrange("(c p) w -> p c w", p=128), r=[], w=[wpp.k()], sem="wpp")
        pv = self.d_p[li].rearrange("(t p) d -> p t d", p=128)
        for t in range(NT):
            s2 = t % 2
            self.dma("pool", ptok[s2].ap, pv[:, t, :], r=[], w=[ptok[s2].k()], sem=f"ptok{s2}")
            self.norm_T_tile(t, rstd, gT, htok[s2], h3T[s2].ap, slice(0, 128), 6, [h3T[s2].k()])
            ptb = self.bank_bf(7)
            for c in range(2):
                self.tr(ptb[:, c * 128:(c + 1) * 128], ptok[s2].ap[:, c * 128:(c + 1) * 128], self.ident_bf.ap, r=[ptok[s2].k(), ("c_identbf",)], w=[("ps", 7)])
            self.cp("act", pTt[s2].ap, ptb[:, 0:256].rearrange("p (c t) -> p c t", c=2), r=[("ps", 7)], w=[pTt[s2].k()])
            for hf in range(2):
                pg_, pe_ = 0 + 2 * hf, 1 + 2 * hf
                cs = slice(hf * 512, (hf + 1) * 512)
                for c in range(8):
                    self.mm(self.bank(pg_), h3T[s2].ap[:, c, :], wpg.ap[:, c, cs], c == 0, c == 7, r=[h3T[s2].k(), wpg.k()], w=[("ps", pg_)])
                for c in range(2):
                    self.mm(self.bank(pe_), pTt[s2].ap[:, c, :], wpp.ap[:, c, cs], c == 0, c == 1, r=[pTt[s2].k(), wpp.k()], w=[("ps", pe_)])
                self.act(sg[hf].ap, self.bank(pg_), AF.Sigmoid, r=[("ps", pg_)], w=[sg[hf].k()])
                self.tt("dve", ge[hf].ap, sg[hf].ap, self.bank(pe_), ALU.mult, r=[sg[hf].k(), ("ps", pe_)], w=[ge[hf].k()])
                xs_ = self.x_tok[:, t, cs]
                self.tt("pool", xs_, xs_, ge[hf].ap, ALU.add, r=[("x", t), ge[hf].k()], w=[("x", t)])
        a.release(m0)

    def final(self):
        p = self.p
        a = self.arena
        m0 = a.mark()
        rstd = self.x_rstd("fin")
        gb = a.alloc("gfin", [D], F32)
        yb = [a.alloc(f"fy{i}", [D], F32) for i in range(2)]
        self.dma("sp", gb.ap, self.d_g_fin.rearrange("(o d) -> o d", o=1).to_broadcast([128, D]), r=[], w=[gb.k()], sem="gfin", allow_slow_non_contiguous=True)
        ov = self.d_out.rearrange("(t p) d -> p t d", p=128)
        for t in range(NT):
            y = yb[t % 2]
            self.stt(y.ap, self.x_tok[:, t, :], rstd.ap[:, t:t + 1], gb.ap, ALU.mult, ALU.mult, r=[("x", t), rstd.k(), gb.k()], w=[y.k()])
            self.dma("sp", ov[:, t, :], y.ap, r=[y.k()], w=[("out", t)], sem=f"ost{t % 2}")
            self.out_keys.append(("out", t))
        a.release(m0)


_W_NAMES = ["g_mix", "w_in", "g_q_lat", "w_q_up", "g_kv_lat", "w_kv_up", "w_branch_a", "w_branch_b", "w_out", "g_ffn",
            "w_router_grp", "b_router_grp", "w_router_exp", "b_router_exp", "w_exp_gate", "w_exp_up", "w_exp_down",
            "g_ple", "w_ple_gate", "w_ple_proj"]

LAYERS_PER_LAUNCH = 4


def _run(x, inputs, layer_lo, layer_hi, final_norm):
    nl = layer_hi - layer_lo
    b = Builder(nl, first_layer=layer_lo, final_norm=final_norm)
    nc = b.build()
    shared = {}
    for k in _W_NAMES:
        shared[k] = np.ascontiguousarray(np.asarray(inputs[k])[layer_lo:layer_hi])
    shared["b_router_exp"] = shared["b_router_exp"].reshape(nl, 64)
    shared["g_final"] = np.ascontiguousarray(np.asarray(inputs["g_final"]))
    p_all = np.asarray(inputs["p"])
    pos = np.asarray(inputs["positions"]).astype(np.int32)
    in_maps = []
    for c in range(NCORES):
        m = dict(shared)
        m["x"] = np.ascontiguousarray(x[c])
        m["p"] = np.ascontiguousarray(p_all[layer_lo:layer_hi, c])
        m["positions"] = np.ascontiguousarray(pos[c])
        in_maps.append(m)
    res = run_bass_kernel_spmd(nc, in_maps, core_ids=list(range(NCORES)))
    return np.stack([np.asarray(r["out"]) for r in res.results], axis=0)


def kernel(**inputs):
    x = np.asarray(inputs["x"]).astype(np.float32, copy=False)
    lo = 0
    while lo < DEPTH:
        hi = min(DEPTH, lo + LAYERS_PER_LAUNCH)
        x = _run(x, inputs, lo, hi, final_norm=(hi == DEPTH))
        lo = hi
    return x.astype(np.float32, copy=False)
```

```python
import math
import bisect
from contextlib import ExitStack

import numpy as np
import concourse.bass as bass
import concourse.mybir as mybir
from concourse.bass_utils import run_bass_kernel_spmd

F32 = mybir.dt.float32
BF16 = mybir.dt.bfloat16
I32 = mybir.dt.int32
AF = mybir.ActivationFunctionType
ALU = mybir.AluOpType
AX = mybir.AxisListType

S = 2048
D = 1024
NT = 16
DEPTH = 4
NCORES = 8
EPS = 1e-6
CAP = 128
NEXP = 64
XS_ROWS = NEXP * CAP
PI = math.pi

EPOCH = 30000


class Prog:
    ENGS = ("pe", "act", "dve", "pool", "sp")

    def __init__(self):
        self.ops = []
        self.lastw = {}
        self.rd = {}

    def add(self, eng, fn, r=(), w=(), dma=None, noinc=False):
        i = len(self.ops)
        raw = set()
        oth = set()
        psr = [k for k in r if isinstance(k, tuple) and k and k[0] == "ps"]
        if psr:
            r = [k for k in r if not (isinstance(k, tuple) and k and k[0] == "ps")]
            w = list(w) + [k for k in psr if k not in w]
        for k in r:
            lw = self.lastw.get(k)
            if lw is not None:
                raw.add(lw)
        for k in w:
            lw = self.lastw.get(k)
            if lw is not None:
                oth.add(lw)
            oth.update(self.rd.get(k, ()))
        raw.discard(i)
        oth.discard(i)
        for k in r:
            self.rd.setdefault(k, []).append(i)
        for k in w:
            self.lastw[k] = i
            self.rd[k] = []
        self.ops.append(dict(eng=eng, fn=fn, raw=raw, oth=oth - raw, dma=dma, noinc=noinc))
        return i

    def alias(self, new_keys, old_keys):
        s = set()
        for k in old_keys:
            lw = self.lastw.get(k)
            if lw is not None:
                s.add(lw)
            s.update(self.rd.get(k, ()))
        for k in new_keys:
            cur = self.rd.get(k, [])
            self.rd[k] = list(set(cur) | s)

    def emit(self, nc, stack, trunc=None):
        ops = self.ops
        cnt = {}
        dma_hist = {}
        for i, op in enumerate(ops):
            op["skip"] = trunc is not None and trunc[0] <= i < trunc[1]
            if op["skip"]:
                op["stream"] = None
                continue
            if op["noinc"]:
                op["stream"] = None
                continue
            if op["dma"] is not None:
                base = "d_" + op["dma"]
                c = cnt.get(base, 0)
                cnt[base] = c + 1
                per = EPOCH // 16
                op["stream"] = f"{base}_{c // per}"
                op["val"] = (c % per + 1) * 16
                dma_hist.setdefault(op["stream"], []).append((i, op["val"]))
            else:
                base = "c_" + op["eng"]
                c = cnt.get(base, 0)
                cnt[base] = c + 1
                op["stream"] = f"{base}_{c // EPOCH}"
                op["val"] = c % EPOCH + 1
        streams = sorted({op["stream"] for op in ops if op["stream"] is not None})
        sems = {s: stack.enter_context(nc.semaphore(s)) for s in streams}
        self.n_sems = len(sems)
        queues = {E: [] for E in self.ENGS}
        for i, op in enumerate(ops):
            queues[op["eng"]].append(i)
        waited = {E: {} for E in self.ENGS}
        stats = {"waits": 0}

        def run(e, E):
            for i in queues[E]:
                op = ops[i]
                if op["skip"]:
                    continue
                need = {}
                is_c = op["dma"] is None
                implied = set()
                for kind in ("raw", "oth"):
                    for d in op[kind]:
                        implied |= ops[d]["raw"]
                        implied |= ops[d]["oth"]
                for kind in ("raw", "oth"):
                    for d in op[kind]:
                        if d in implied:
                            continue
                        dop = ops[d]
                        if dop["skip"]:
                            continue
                        if is_c and dop["dma"] is None and dop["eng"] == E:
                            if E == "pe" or kind == "oth":
                                continue
                        s, v = dop["stream"], dop["val"]
                        if need.get(s, 0) < v:
                            need[s] = v
                for s, v in need.items():
                    if waited[E].get(s, 0) >= v:
                        continue
                    if s.startswith("d_"):
                        hist = dma_hist[s]
                        j = bisect.bisect_left(hist, (i, 0)) - 1
                        if not (j >= 0 and hist[j][1] == v):
                            kd = v // 16 - 1
                            d_idx, d1_idx = hist[kd][0], hist[kd + 1][0]
                            seen, stack_, found = set(), [d1_idx], False
                            while stack_ and not found and len(seen) < 400000:
                                n_ = stack_.pop()
                                for dd in (ops[n_]["raw"] | ops[n_]["oth"]):
                                    if dd == d_idx:
                                        found = True
                                        break
                                    if dd > d_idx and dd not in seen:
                                        seen.add(dd)
                                        stack_.append(dd)
                            assert found, f"partial DMA wait on {s}: want {v} issued {hist[j][1]} op {i}"
                    e.wait_ge(sems[s], v)
                    waited[E][s] = v
                    stats["waits"] += 1
                ins = op["fn"](e)
                if op["stream"] is not None:
                    ins.then_inc(sems[op["stream"]], 16 if op["dma"] is not None else 1)

        with nc.Block() as block:
            @block.tensor
            def _(e):
                run(e, "pe")

            @block.scalar
            def _(e):
                run(e, "act")

            @block.vector
            def _(e):
                run(e, "dve")

            @block.gpsimd
            def _(e):
                run(e, "pool")

            @block.sync
            def _(e):
                run(e, "sp")
        self.stats = stats


class Buf:
    def __init__(self, prog, name, gen, ap, start, end, old_keys):
        self.p = prog
        self.name = name
        self.gen = gen
        self.ap = ap
        self.start = start
        self.end = end
        self.old_keys = old_keys
        self.keys = {}

    def k(self, i=0):
        key = self.keys.get(i)
        if key is None:
            key = (self.name, self.gen, i)
            self.keys[i] = key
            if self.old_keys:
                self.p.alias([key], self.old_keys)
        return key

    def ks(self, it):
        return [self.k(i) for i in it]


class Arena:
    def __init__(self, prog, big, nwords):
        self.p = prog
        self.big = big
        self.n = nwords
        self.top = 0
        self.hi = nwords
        self.dead = []
        self.live = []
        self.live_hi = []
        self.gen = 0
        self.peak = 0

    def mark(self):
        return (self.top, len(self.live))

    def release(self, m):
        top, nl = m
        while len(self.live) > nl:
            self.dead.append(self.live.pop())
        self.top = top

    def mark_hi(self):
        return (self.hi, len(self.live_hi))

    def release_hi(self, m):
        hi, nl = m
        while len(self.live_hi) > nl:
            self.dead.append(self.live_hi.pop())
        self.hi = hi

    def alloc(self, name, shape, dtype, top=False):
        nel = 1
        for s_ in shape:
            nel *= s_
        esz = 4 if dtype in (F32, I32) else 2
        nw = (nel * esz + 3) // 4
        nw = (nw + 7) // 8 * 8
        if top:
            end = self.hi
            start = end - nw
            assert start >= self.top, f"SBUF arena overflow allocating {name} (top)"
            self.hi = start
        else:
            start = self.top
            end = start + nw
            assert end <= self.hi, f"SBUF arena overflow allocating {name}: {end} > {self.hi}"
            self.top = end
        self.peak = max(self.peak, self.top + (self.n - self.hi))
        old_keys = []
        keep = []
        for b in self.dead:
            if b.start < end and start < b.end:
                old_keys.extend(b.keys.values())
                old_keys.extend(b.old_keys)
                if not (start <= b.start and b.end <= end):
                    keep.append(b)
            else:
                keep.append(b)
        self.dead = keep
        self.gen += 1
        ap = self.big[:, start:end]
        if esz == 2:
            ap = ap.bitcast(dtype)[:, 0:nel]
        elif dtype == I32:
            ap = ap.bitcast(I32)[:, 0:nel]
        else:
            ap = ap[:, 0:nel]
        if len(shape) == 2:
            ap = ap.rearrange("p (a b) -> p a b", a=shape[0])
        elif len(shape) == 3:
            ap = ap.rearrange("p (a b c) -> p a b c", a=shape[0], b=shape[1])
        b = Buf(self.p, name, self.gen, ap, start, end, list(dict.fromkeys(old_keys)))
        (self.live_hi if top else self.live).append(b)
        return b


class Builder:
    def __init__(self, n_layers, first_layer=0, final_norm=True, debug=None):
        self.n_layers = n_layers
        self.first_layer = first_layer
        self.final_norm = final_norm
        self.debug = debug or {}
        self.dbg_outs = []

    def mm(self, out, lhsT, rhs, start, stop, r, w):
        self.p.add("pe", lambda e: e.matmul(out, lhsT=lhsT, rhs=rhs, start=start, stop=stop), r=r, w=w)

    def tr(self, out, in_, ident, r, w):
        self.p.add("pe", lambda e: e.transpose(out, in_, ident), r=r, w=w)

    def act(self, out, in_, func, r, w, bias=None, scale=None, accum_out=None, eng="act"):
        kw = {}
        if bias is not None:
            kw["bias"] = bias
        if scale is not None:
            kw["scale"] = scale
        if accum_out is not None:
            kw["accum_out"] = accum_out
        self.p.add(eng, lambda e: e.activation(out=out, in_=in_, func=func, **kw), r=r, w=w)

    def tt(self, eng, out, in0, in1, op, r, w):
        if eng == "pool" and "nopool" in self.debug.get("skip", ""):
            eng = "dve"
        self.p.add(eng, lambda e: e.tensor_tensor(out=out, in0=in0, in1=in1, op=op), r=r, w=w)

    def ts(self, eng, out, in0, s1, s2, op0, op1, r, w):
        if op1 is None:
            self.p.add(eng, lambda e: e.tensor_scalar(out=out, in0=in0, scalar1=s1, scalar2=None, op0=op0), r=r, w=w)
        else:
            self.p.add(eng, lambda e: e.tensor_scalar(out=out, in0=in0, scalar1=s1, scalar2=s2, op0=op0, op1=op1), r=r, w=w)

    def stt(self, out, in0, scalar, in1, op0, op1, r, w):
        self.p.add("dve", lambda e: e.scalar_tensor_tensor(out=out, in0=in0, scalar=scalar, in1=in1, op0=op0, op1=op1), r=r, w=w)

    def cp(self, eng, out, in_, r, w):
        if eng == "act":
            self.p.add("act", lambda e: e.activation(out=out, in_=in_, func=AF.Copy), r=r, w=w)
        else:
            self.p.add(eng, lambda e: e.tensor_copy(out=out, in_=in_), r=r, w=w)

    def memset(self, eng, ap, val, w):
        if eng == "pool" and "nopoolms" in self.debug.get("skip", ""):
            eng = "dve"
        self.p.add(eng, lambda e: e.memset(ap, val), w=w)

    def dma(self, eng, out, in_, r, w, sem, **kw):
        self.p.add(eng, lambda e: e.dma_start(out=out, in_=in_, **kw), r=r, w=w, dma=sem)

    def bc_reg(self, e):
        if getattr(self, "_bc_reg", None) is None:
            self._bc_reg = e.to_reg(XS_ROWS - 1)
        return self._bc_reg

    def dbg(self, name, ap, shape, dtype, r):
        t = self.nc.dram_tensor("dbg_" + name, list(shape), dtype, kind="ExternalOutput").ap()
        self.dma("sp", t, ap, r=r, w=[("dbgout", name)], sem="dbg_" + name)
        self.dbg_outs.append(name)
        self.out_keys.append(("dbgout", name))

    def build(self):
        nc = bass.Bass("TRN2", target_bir_lowering=False)
        self.nc = nc
        p = Prog()
        self.p = p
        nl = self.n_layers
        self.out_keys = []

        def din(name, shape, dt=F32):
            return nc.dram_tensor(name, list(shape), dt, kind="ExternalInput").ap()

        self.d_x = din("x", [S, D])
        self.d_p = din("p", [nl, S, 256])
        self.d_pos = din("positions", [S], I32)
        self.d_g_mix = din("g_mix", [nl, D])
        self.d_w_in = din("w_in", [nl, D, 5152])
        self.d_g_q = din("g_q_lat", [nl, 512])
        self.d_w_q = din("w_q_up", [nl, 512, 1536])
        self.d_g_kv = din("g_kv_lat", [nl, 256])
        self.d_w_kv = din("w_kv_up", [nl, 256, 2048])
        self.d_w_ba = din("w_branch_a", [nl, D, D])
        self.d_w_bb = din("w_branch_b", [nl, 256, D])
        self.d_w_out = din("w_out", [nl, D, D])
        self.d_g_ffn = din("g_ffn", [nl, D])
        self.d_w_rg = din("w_router_grp", [nl, D, 8])
        self.d_b_rg = din("b_router_grp", [nl, 8])
        self.d_w_re = din("w_router_exp", [nl, D, 64])
        self.d_b_re = din("b_router_exp", [nl, 64])
        self.d_w1 = din("w_exp_gate", [nl, NEXP, D, 256])
        self.d_w3 = din("w_exp_up", [nl, NEXP, D, 256])
        self.d_w2 = din("w_exp_down", [nl, NEXP, 256, D])
        self.d_g_ple = din("g_ple", [nl, D])
        self.d_w_pg = din("w_ple_gate", [nl, D, D])
        self.d_w_pp = din("w_ple_proj", [nl, 256, D])
        self.d_g_fin = din("g_final", [D])
        self.d_out = nc.dram_tensor("out", [S, D], F32, kind="ExternalOutput").ap()
        self.d_xs = [nc.dram_tensor(f"xs{l}", [XS_ROWS, D], BF16, kind="Internal").ap() for l in range(nl)]
        self.d_ys = [nc.dram_tensor(f"ys{l}", [XS_ROWS, D], F32, kind="Internal").ap() for l in range(nl)]

        with ExitStack() as stack:
            XW = NT * D
            CW = 2560
            total_words = 212800 // 4
            AW = total_words - XW - CW
            big = stack.enter_context(nc.sbuf_tensor("big", [128, total_words], F32))
            self.ps = stack.enter_context(nc.psum_tensor("ps", [128, 4096], F32))
            self.x_tok = big[:, 0:XW].rearrange("p (t d) -> p t d", t=NT)
            self.carena = Arena(p, big[:, XW:XW + CW], CW)
            self.arena = Arena(p, big[:, XW + CW:XW + CW + AW], AW)
            self.setup_consts()
            xv = self.d_x.rearrange("(t p) d -> p t d", p=128)
            for t in range(NT):
                self.dma("sp", self.x_tok[:, t, :], xv[:, t, :], r=[], w=[("x", t)], sem=f"xload{t}")
            for li in range(nl):
                if self.debug.get("stop") == "consts":
                    break
                self.layer(li)
            n_body = len(p.ops)
            if self.final_norm:
                self.final()
            else:
                ov = self.d_out.rearrange("(t p) d -> p t d", p=128)
                for t in range(NT):
                    self.dma("sp", ov[:, t, :], self.x_tok[:, t, :], r=[("x", t)], w=[("out", t)], sem=f"ost{t % 4}")
                    self.out_keys.append(("out", t))
            p.add("sp", lambda e: e.nop(), r=list(self.out_keys), w=[], noinc=True)
            mo = self.debug.get("maxops")
            p.emit(nc, stack, trunc=(int(mo), n_body) if mo else None)
        self.arena_peak = self.arena.peak
        return nc

    def bank(self, b, n=1):
        return self.ps[:, b * 512:(b + n) * 512]

    def bank_bf(self, b):
        return self.ps[:, b * 512:(b + 1) * 512].bitcast(BF16)

    def setup_consts(self):
        p = self.p
        ca = self.carena
        self.ident_bf = ca.alloc("ident_bf", [128], BF16)
        self.ident_f = ca.alloc("ident_f", [128], F32)
        self.tri2 = ca.alloc("tri2", [256], BF16)
        self.ones_f = ca.alloc("ones_f", [128], F32)
        self.ones_bf = ca.alloc("ones_bf", [128], BF16)
        self.ustrict = ca.alloc("ustrict", [128], BF16)
        self.pos_i = ca.alloc("pos_i", [4, NT], I32)
        self.pos_f = ca.alloc("pos_f", [4, NT], F32)
        self.inv_m = ca.alloc("inv_m", [16], F32)
        self.inv_d = ca.alloc("inv_d", [8], F32)
        self.cos_m = ca.alloc("cos_m", [NT, 16], F32)
        self.sin_m = ca.alloc("sin_m", [NT, 16], F32)
        self.cos_d = ca.alloc("cos_d", [3, NT, 8], F32)
        self.sin_d = ca.alloc("sin_d", [3, NT, 8], F32)
        self.neghalf = ca.alloc("neghalf", [NT], F32)
        self.ebase = ca.alloc("ebase", [NEXP], F32)
        self.negpi = ca.alloc("negpi", [1], F32)
        CK = ("consts",)

        SK = self.debug.get("skip", "")
        for _i in range(int(self.debug.get("pad") or 0)):
            self.memset("dve", self.ones_bf.ap, 1.0, w=[("c_onesbf",)])
        def iden(buf, key):
            self.memset("pool", buf.ap, 1.0, w=[key])
            if "sel" in SK:
                return
            p.add("pool", lambda e: e.affine_select(out=buf.ap, in_=buf.ap, pattern=[[1, 128]], compare_op=ALU.is_equal,
                                                     fill=0.0, base=0, channel_multiplier=-1), r=[key], w=[key])
        iden(self.ident_bf, ("c_identbf",))
        iden(self.ident_f, ("c_identf",))
        t2 = self.tri2.ap
        self.memset("pool", t2, 1.0, w=[("c_tri",)])
        if "sel" not in SK:
          p.add("pool", lambda e: e.affine_select(out=t2[:, 0:128], in_=t2[:, 0:128], pattern=[[1, 128]], compare_op=ALU.is_ge,
                                                 fill=0.0, base=0, channel_multiplier=-1), r=[("c_tri",)], w=[("c_tri",)])
        if "sel" not in SK:
          p.add("pool", lambda e: e.affine_select(out=t2[:, 128:256], in_=t2[:, 128:256], pattern=[[-1, 128]], compare_op=ALU.is_ge,
                                                 fill=0.0, base=0, channel_multiplier=1), r=[("c_tri",)], w=[("c_tri",)])
        us = self.ustrict.ap
        self.memset("pool", us, 1.0, w=[("c_us",)])
        if "sel" not in SK:
          p.add("pool", lambda e: e.affine_select(out=us, in_=us, pattern=[[1, 128]], compare_op=ALU.is_gt,
                                                 fill=0.0, base=0, channel_multiplier=-1), r=[("c_us",)], w=[("c_us",)])
        self.memset("pool", self.ones_f.ap, 1.0, w=[("c_ones",)])
        self.memset("pool", self.ones_bf.ap, 1.0, w=[("c_onesbf",)])
        self.memset("pool", self.neghalf.ap, -0.5, w=[("c_nh",)])
        self.memset("pool", self.negpi.ap, -PI, w=[("c_negpi",)])
        eb = self.ebase.ap
        if "iota" not in SK:
          p.add("pool", lambda e: e.iota(eb, pattern=[[CAP, NEXP]], base=0, channel_multiplier=0,
                                        allow_small_or_imprecise_dtypes=True), w=[("c_ebase",)])
        for j in range(16):
            self.memset("pool", self.inv_m.ap[:, j:j + 1], float(np.float32(10000.0) ** np.float32(-j * 2.0 / 32)), w=[("c_invm",)])
        for j in range(8):
            self.memset("pool", self.inv_d.ap[:, j:j + 1], float(np.float32(500000.0) ** np.float32(-j * 2.0 / 16)), w=[("c_invd",)])
        pv = [self.d_pos.rearrange("(t p) -> p t", p=128)]
        for d in (1, 4, 16):
            nb = NT // d
            pv.append(self.d_pos.rearrange("(j p r) -> p r j", p=128, r=d, j=nb))
        for gi in range(4):
            if "pos" in SK:
                break
            if gi == 0:
                dst = self.pos_i.ap[:, gi, :]
            else:
                d = (1, 4, 16)[gi - 1]
                dst = self.pos_i.ap[:, gi, :].rearrange("p (r j) -> p r j", r=d)
            self.dma("sp", dst, pv[gi], r=[], w=[("c_posi", gi)], sem="posload", allow_slow_non_contiguous=True)
        self.cp("dve", self.pos_f.ap, self.pos_i.ap, r=[("c_posi", g) for g in range(4)], w=[("c_posf",)])

        a = self.arena
        msc = a.mark()
        angb = a.alloc("ang", [NT * 16], F32)
        ab = a.alloc("ang_a", [NT * 16], F32)
        kib = a.alloc("ang_ki", [NT * 16], I32)
        kfb = a.alloc("ang_kf", [NT * 16], F32)
        mb = a.alloc("ang_m", [NT * 16], F32)

        def table(dst_cos, dst_sin, posf, inv, nf):
            n = NT * nf
            v3 = lambda b_: b_.ap[:, 0:n].rearrange("p (t j) -> p t j", t=NT)
            ang, aa, ki, kf, mm_ = v3(angb), v3(ab), v3(kib), v3(kfb), v3(mb)
            a0 = posf.unsqueeze(2).to_broadcast([128, NT, nf])
            a1 = inv.unsqueeze(1).to_broadcast([128, NT, nf])
            self.tt("dve", ang, a0, a1, ALU.mult, r=[("c_posf",), ("c_invm",), ("c_invd",)], w=[angb.k()])
            for dst, shift in ((dst_sin, 0.0), (dst_cos, 0.5 * PI)):
                self.ts("dve", aa, ang, shift, None, ALU.add, None, r=[angb.k()], w=[ab.k()])
                self.ts("dve", kf, aa, 1.0 / (2 * PI), None, ALU.mult, None, r=[ab.k()], w=[kfb.k()])
                self.cp("dve", ki, kf, r=[kfb.k()], w=[kib.k()])
                self.cp("dve", kf, ki, r=[kib.k()], w=[kfb.k()])
                self.stt(aa, kf, -2 * PI, aa, ALU.mult, ALU.add, r=[kfb.k(), ab.k()], w=[ab.k()])
                self.ts("dve", mm_, aa, PI, None, ALU.is_gt, None, r=[ab.k()], w=[mb.k()])
                self.stt(aa, mm_, -2 * PI, aa, ALU.mult, ALU.add, r=[mb.k(), ab.k()], w=[ab.k()])
                self.ts("dve", mm_, aa, -PI, None, ALU.is_lt, None, r=[ab.k()], w=[mb.k()])
                self.stt(aa, mm_, 2 * PI, aa, ALU.mult, ALU.add, r=[mb.k(), ab.k()], w=[ab.k()])
                self.ts("dve", aa, aa, 3.14159, -3.14159, ALU.min, ALU.max, r=[ab.k()], w=[ab.k()])
                self.act(dst, aa, AF.Copy if "nosin" in SK else AF.Sin, r=[ab.k()], w=[("c_tab",)])
        if "tab" not in SK:
            table(self.cos_m.ap, self.sin_m.ap, self.pos_f.ap[:, 0, :], self.inv_m.ap, 16)
        for g in range(3):
            if "tab" in SK:
                break
            table(self.cos_d.ap[:, g], self.sin_d.ap[:, g], self.pos_f.ap[:, g + 1, :], self.inv_d.ap, 8)
        a.release(msc)
        if "consts" in self.debug:
            self.dbg("cos_m", self.cos_m.ap, [128, NT, 16], F32, r=[("c_tab",)])
            self.dbg("sin_d", self.sin_d.ap, [128, 3, NT, 8], F32, r=[("c_tab",)])
            self.dbg("tri2", self.tri2.ap, [128, 256], BF16, r=[("c_tri",)])
            self.dbg("ident", self.ident_f.ap, [128, 128], F32, r=[("c_identf",)])
            self.dbg("ebase", self.ebase.ap, [128, 64], F32, r=[("c_ebase",)])
            self.dbg("ustrict", self.ustrict.ap, [128, 128], BF16, r=[("c_us",)])
        self.CONST_KEYS = [("c_identbf",), ("c_identf",), ("c_tri",), ("c_us",), ("c_ones",), ("c_onesbf",), ("c_nh",),
                           ("c_ebase",), ("c_tab",)]

    def x_rstd(self, tag):
        a = self.arena
        ss = a.alloc("ss" + tag, [NT], F32)
        rstd = a.alloc("rstd" + tag, [NT], F32)
        mj = a.mark()
        junk = a.alloc("junk" + tag, [D], BF16)
        for t in range(NT):
            self.act(junk.ap, self.x_tok[:, t, :], AF.Square, r=[("x", t)], w=[ss.k(t), junk.k()], accum_out=ss.ap[:, t:t + 1])
        a.release(mj)
        self.ts("dve", ss.ap, ss.ap, 1.0 / D, EPS, ALU.mult, ALU.add, r=ss.ks(range(NT)), w=[ss.k("ms")])
        self.act(ss.ap, ss.ap, AF.Sqrt, r=[ss.k("ms")], w=[ss.k("ms")])
        p_ = self.p
        p_.add("dve", lambda e: e.reciprocal(out=rstd.ap, in_=ss.ap), r=[ss.k("ms")], w=[rstd.k()])
        return rstd

    def gain_T(self, tag, d_g, li, nchunk, mode="nat"):
        gT = self.arena.alloc("gT" + tag, [nchunk], F32)
        if mode == "nat":
            src = d_g[li].rearrange("(c p) -> p c", p=128)
        else:
            src = d_g[li].rearrange("(p c) -> p c", p=128)
        self.dma("sp", gT.ap, src, r=[], w=[gT.k()], sem="gT" + tag, allow_slow_non_contiguous=True)
        return gT

    def norm_T_tile(self, t, rstd, gT, htok, dstT, dst_cols, psb, dst_keys):
        self.act(htok.ap, self.x_tok[:, t, :], AF.Copy, r=[("x", t), rstd.k()], w=[htok.k()], scale=rstd.ap[:, t:t + 1])
        pb = self.bank_bf(psb)
        for c in range(8):
            self.tr(pb[:, c * 128:(c + 1) * 128], htok.ap[:, c * 128:(c + 1) * 128], self.ident_bf.ap,
                    r=[htok.k(), ("c_identbf",)], w=[("ps", psb)])
        self.tt("dve", dstT[:, :, dst_cols], pb.rearrange("p (c t) -> p c t", c=8),
                gT.ap.unsqueeze(2).to_broadcast([128, 8, 128]), ALU.mult, r=[("ps", psb), gT.k()], w=dst_keys)

    def layer(self, li):
        L = self.first_layer + li
        a = self.arena
        m_layer = a.mark()
        self.oaT = a.alloc("oaT", [4 * S], F32)
        self.oaT_ap = self.oaT.ap.bitcast(BF16).rearrange("p (c s) -> p c s", c=8)
        self.acc_ap = self.oaT.ap.rearrange("p (h s) -> p h s", h=4)
        self.obT = a.alloc("obT", [2, S], BF16)
        self.mixer(li)
        a.release(m_layer)
        if self.debug.get("stop") in ("p1", "dil", "lat", "mla", "mixer"):
            return
        m = a.mark()
        self.moe(li)
        self.ple(li, do_ple=self.debug.get("stop") != "moe")
        a.release(m)

    def mixer(self, li):
        p = self.p
        a = self.arena
        rstd = self.x_rstd("mix")
        gT = self.gain_T("mix", self.d_g_mix, li, 8)
        self.rstd_mix = rstd
        self.gT_mix = gT
        m_lat = a.mark_hi()
        m1 = a.mark()
        hT = a.alloc("hT", [8, S], BF16)
        htok = [a.alloc(f"htok{i}", [D], BF16) for i in range(2)]
        for t in range(NT):
            self.norm_T_tile(t, rstd, gT, htok[t % 2], hT.ap, slice(t * 128, (t + 1) * 128), 6 + t % 2, [hT.k(t)])
        if "hT" in self.debug:
            self.dbg("hT", hT.ap, [128, 8, S], BF16, r=hT.ks(range(NT)))
        if self.debug.get("stop") == "p1":
            a.release(m1)
            return
        self.dilated(li, hT)
        if self.debug.get("stop") == "dil":
            for ch in self.dil_norm_chunks:
                ch()
            if "obT" in self.debug:
                self.dbg("obT", self.obT.ap, [128, 2, S], BF16, r=self.obT.ks([(pp, tc) for pp in range(2) for tc in range(4)]))
            a.release(m1)
            a.release_hi(m_lat)
            return
        hqT = a.alloc("hqT", [4, S], BF16, top=True)
        hkvT = a.alloc("hkvT", [2, S], BF16, top=True)
        krot = a.alloc("krot", [NT, 32], BF16, top=True)
        self.latents(li, hT, hqT, hkvT, krot)
        if "obT" in self.debug:
            self.dbg("obT", self.obT.ap, [128, 2, S], BF16, r=self.obT.ks([(pp, tc) for pp in range(2) for tc in range(4)]))
        a.release(m1)
        if self.debug.get("stop") == "lat":
            a.release_hi(m_lat)
            return
        self.p.alias(self.oaT.ks([(c, q, i) for c in range(8) for q in range(4) for i in range(2)]), self.oaT.ks([("acc", hh) for hh in range(4)]))
        self.mla(li, hqT, hkvT, krot)
        a.release_hi(m_lat)
        if self.debug.get("stop") == "mla":
            return
        self.merge(li)

    def dilated(self, li, hT):
        p = self.p
        a = self.arena
        m0 = a.mark()
        acc_ap = self.acc_ap
        akey = lambda hh: self.oaT.k(("acc", hh))
        wd = a.alloc("wdil", [8, 768], BF16)
        qkT = a.alloc("dqkT", [4, S], BF16)
        dv = a.alloc("dv", [NT, 4, 128], BF16)
        qk_tok = [a.alloc(f"dqk_tok{i}", [8, 64], BF16) for i in range(2)]
        tmp = [a.alloc(f"dtmp{i}", [8, 8], F32) for i in range(4)]
        pT = [a.alloc(f"dpT{i}", [256], BF16) for i in range(3)]
        w_in = self.d_w_in[li]
        self.memset("pool", dv.ap[:, :, :, 64:128], 1.0, w=[dv.k("ones")])
        for g, d in enumerate((1, 4, 16)):
            if "dil_g1" in self.debug.get("skip", "") and g > 0:
                break
            nb = NT // d
            c0 = 800 + g * 768
            self.dma("pool", wd.ap, w_in[:, c0:c0 + 768].rearrange("(c p) w -> p c w", p=128), r=[], w=[wd.k()], sem="wdil")
            cosg = self.cos_d.ap[:, g]
            sing = self.sin_d.ap[:, g]
            def dj_mm(u):
                r_, j_ = divmod(u, nb)
                st = r_ + d * 128 * j_
                tok = slice(st, st + d * 127 + 1, d)
                pa, pb_ = 0 + (u % 2) * 3, 1 + (u % 2) * 3
                for c in range(8):
                    self.mm(self.bank(pa), hT.ap[:, c, tok], wd.ap[:, c, 0:512], c == 0, c == 7,
                            r=hT.ks(range(NT)) + [wd.k()], w=[("ps", pa)])
                for c in range(8):
                    self.mm(self.bank(pb_)[:, 0:256], hT.ap[:, c, tok], wd.ap[:, c, 512:768], c == 0, c == 7,
                            r=hT.ks(range(NT)) + [wd.k()], w=[("ps", pb_)])

            def dj_ew(u):
                pa, pb_ = 0 + (u % 2) * 3, 1 + (u % 2) * 3
                qt = qk_tok[u % 2]
                pav = self.bank(pa).rearrange("p (h e) -> p h e", h=8)
                cb = cosg[:, u, :].unsqueeze(1).to_broadcast([128, 8, 8])
                sb = sing[:, u, :].unsqueeze(1).to_broadcast([128, 8, 8])
                x1 = pav[:, :, 0:8]
                x2 = pav[:, :, 8:16]
                t0, t1, t2, t3 = [x.ap for x in tmp]
                ck = [("c_tab",)]
                self.cp("dve", qt.ap[:, :, 16:64], pav[:, :, 16:64], r=[("ps", pa)], w=[qt.k("rest")])
                self.tt("dve", t0, x1, cb, ALU.mult, r=[("ps", pa)] + ck, w=[tmp[0].k()])
                self.tt("dve", t1, x2, sb, ALU.mult, r=[("ps", pa)] + ck, w=[tmp[1].k()])
                self.tt("dve", t2, x1, sb, ALU.mult, r=[("ps", pa)] + ck, w=[tmp[2].k()])
                self.tt("dve", t3, x2, cb, ALU.mult, r=[("ps", pa)] + ck, w=[tmp[3].k()])
                self.tt("pool", qt.ap[:, :, 0:8], t0, t1, ALU.subtract, r=[tmp[0].k(), tmp[1].k()], w=[qt.k("r1")])
                self.tt("pool", qt.ap[:, :, 8:16], t2, t3, ALU.add, r=[tmp[2].k(), tmp[3].k()], w=[qt.k("r2")])
                self.cp("act", dv.ap[:, u, :, 0:64], self.bank(pb_)[:, 0:256].rearrange("p (h e) -> p h e", h=4),
                        r=[("ps", pb_)], w=[dv.k(u)])

            def dj_tr(u):
                pt = 2 + (u % 2) * 3
                qt = qk_tok[u % 2]
                ptb = self.bank_bf(pt)
                qflat = qt.ap.rearrange("p h e -> p (h e)")
                for i4 in range(4):
                    self.tr(ptb[:, i4 * 128:(i4 + 1) * 128], qflat[:, i4 * 128:(i4 + 1) * 128], self.ident_bf.ap,
                            r=[qt.k("rest"), qt.k("r1"), qt.k("r2"), ("c_identbf",)], w=[("ps", pt)])
                self.cp("act", qkT.ap[:, :, u * 128:(u + 1) * 128], ptb[:, 0:512].rearrange("p (c t) -> p c t", c=4),
                        r=[("ps", pt)], w=[qkT.k(u)])

            for u in range(NT + 2):
                if u < NT:
                    dj_mm(u)
                if 0 <= u - 1 < NT:
                    dj_ew(u - 1)
                if 0 <= u - 2 < NT:
                    dj_tr(u - 2)
            for hh in range(4):
                if "dil_noattn" in self.debug.get("skip", ""):
                    break
                pp, pb0 = hh // 2, (hh % 2) * 64

                def a_qk(u):
                    j_ = u % nb
                    ncols = 256 if j_ + 1 < nb else 128
                    sb_ = 6 + (u % 2)
                    self.mm(self.bank(sb_)[:, 0:ncols], qkT.ap[pb0:pb0 + 64, 2 + pp, u * 128:(u + 1) * 128],
                            qkT.ap[pb0:pb0 + 64, pp, u * 128:u * 128 + ncols], True, True,
                            r=[qkT.k(u), qkT.k(u + 1)] if ncols == 256 else [qkT.k(u)], w=[("ps", sb_)])

                def a_exp(u):
                    j_ = u % nb
                    ncols = 256 if j_ + 1 < nb else 128
                    sb_ = 6 + (u % 2)
                    P = pT[u % 3]
                    self.act(P.ap[:, 0:ncols], self.bank(sb_)[:, 0:ncols], AF.Exp, r=[("ps", sb_)], w=[P.k()], scale=0.125)
                    self.tt("pool", P.ap[:, 0:ncols], P.ap[:, 0:ncols], self.tri2.ap[:, 0:ncols], ALU.mult,
                            r=[P.k(), ("c_tri",)], w=[P.k()])

                def a_pv(u):
                    j_ = u % nb
                    P = pT[u % 3]
                    ob = 4 + (u // 4) % 2
                    oc = (u % 4) * 128
                    oap = self.bank(ob)[:, oc:oc + 128]
                    if j_ > 0:
                        prevP = pT[(u - 1) % 3]
                        self.mm(oap, dv.ap[:, u - 1, hh, :], prevP.ap[:, 128:256], True, False,
                                r=[dv.k(u - 1), dv.k("ones"), prevP.k()], w=[("ps", ob)])
                    self.mm(oap, dv.ap[:, u, hh, :], P.ap[:, 0:128], j_ == 0, True,
                            r=[dv.k(u), dv.k("ones"), P.k()], w=[("ps", ob)])
                    if u % 4 == 3:
                        u0 = u - 3
                        src = self.bank(ob)
                        if d == 1:
                            dst = acc_ap[:, hh, u0 * 128:(u0 + 4) * 128]
                        elif d == 4:
                            r_ = u0 // nb
                            dst = acc_ap[:, hh, r_:r_ + 4 * 511 + 1:4]
                        else:
                            dst = acc_ap[:, hh, :].rearrange("p (i r) -> p r i", r=16)[:, u0:u0 + 4, :]
                            src = src.rearrange("p (r i) -> p r i", r=4)
                        if g == 0:
                            self.cp("dve", dst, src, r=[("ps", ob)], w=[akey(hh)])
                        else:
                            self.tt("dve", dst, src, dst, ALU.add, r=[("ps", ob), akey(hh)], w=[akey(hh)])

                for u in range(NT):
                    a_qk(u)
                    if u >= 1:
                        a_pv(u - 1)
                    a_exp(u)
                a_pv(NT - 1)
        rdn = [a.alloc("drdn0", [512], F32, top=True)] * 2
        chunks = []
        for hh in range(4):
            pp, pb0 = hh // 2, (hh % 2) * 64
            for tc in range(4):
                def chunk(hh=hh, tc=tc, pp=pp, pb0=pb0):
                    rd = rdn[(hh * 4 + tc) % 2]
                    cs = slice(tc * 512, (tc + 1) * 512)
                    p.add("dve", (lambda e: e.reciprocal(out=rd.ap[0:64, :], in_=acc_ap[64:128, hh, cs])), r=[akey(hh)], w=[rd.k()])
                    self.tt("dve", self.obT.ap[pb0:pb0 + 64, pp, cs], acc_ap[0:64, hh, cs], rd.ap[0:64, :], ALU.mult,
                            r=[akey(hh), rd.k()], w=[self.obT.k((pp, tc))])
                chunks.append(chunk)
        self.dil_norm_chunks = chunks
        a.release(m0)

    def latents(self, li, hT, hqT, hkvT, krot):
        p = self.p
        a = self.arena
        m0 = a.mark()
        wl = a.alloc("wlat", [8, 800], BF16)
        gq = self.gain_T("q", self.d_g_q, li, 4)
        gkv = self.gain_T("kv", self.d_g_kv, li, 2)
        st = a.alloc("lstat", [NT, 4], F32)
        junk = a.alloc("ljunk", [512], BF16)
        hq_tok = [a.alloc(f"hq_tok{i}", [768], BF16) for i in range(2)]
        tmp = [a.alloc(f"ltmp{i}", [16], F32) for i in range(4)]
        self.dma("pool", wl.ap, self.d_w_in[li][:, 0:800].rearrange("(c p) w -> p c w", p=128), r=[], w=[wl.k()], sem="wlat")
        def l_mm(t):
            pa, pb_ = 0 + (t % 2) * 3, 1 + (t % 2) * 3
            tok = slice(t * 128, (t + 1) * 128)
            for c in range(8):
                self.mm(self.bank(pa), hT.ap[:, c, tok], wl.ap[:, c, 0:512], c == 0, c == 7, r=[hT.k(t), wl.k()], w=[("ps", pa)])
            for c in range(8):
                self.mm(self.bank(pb_)[:, 0:288], hT.ap[:, c, tok], wl.ap[:, c, 512:800], c == 0, c == 7, r=[hT.k(t), wl.k()], w=[("ps", pb_)])

        def l_ew(t):
            pa, pb_ = 0 + (t % 2) * 3, 1 + (t % 2) * 3
            self.act(junk.ap, self.bank(pa), AF.Square, r=[("ps", pa)], w=[st.k((t, 0))], accum_out=st.ap[:, t, 0:1])
            self.act(junk.ap[:, 0:256], self.bank(pb_)[:, 0:256], AF.Square, r=[("ps", pb_)], w=[st.k((t, 1))], accum_out=st.ap[:, t, 1:2])
            self.ts("dve", st.ap[:, t, 0:1], st.ap[:, t, 0:1], 1.0 / 512, EPS, ALU.mult, ALU.add, r=[st.k((t, 0))], w=[st.k((t, 0))])
            self.ts("dve", st.ap[:, t, 1:2], st.ap[:, t, 1:2], 1.0 / 256, EPS, ALU.mult, ALU.add, r=[st.k((t, 1))], w=[st.k((t, 1))])
            self.act(st.ap[:, t, 0:2], st.ap[:, t, 0:2], AF.Sqrt, r=[st.k((t, 0)), st.k((t, 1))], w=[st.k((t, 0)), st.k((t, 1))])
            p.add("dve", (lambda e, t=t: e.reciprocal(out=st.ap[:, t, 2:4], in_=st.ap[:, t, 0:2])), r=[st.k((t, 0)), st.k((t, 1))], w=[st.k((t, 2))])
            hq = hq_tok[t % 2]
            self.ts("dve", hq.ap[:, 0:512], self.bank(pa), st.ap[:, t, 2:3], None, ALU.mult, None, r=[("ps", pa), st.k((t, 2))], w=[hq.k(0)])
            self.ts("dve", hq.ap[:, 512:768], self.bank(pb_)[:, 0:256], st.ap[:, t, 3:4], None, ALU.mult, None, r=[("ps", pb_), st.k((t, 2))], w=[hq.k(1)])
            x1 = self.bank(pb_)[:, 256:272]
            x2 = self.bank(pb_)[:, 272:288]
            cb = self.cos_m.ap[:, t, :]
            sb = self.sin_m.ap[:, t, :]
            ck = [("c_tab",), ("ps", pb_)]
            self.tt("dve", tmp[0].ap, x1, cb, ALU.mult, r=ck, w=[tmp[0].k()])
            self.tt("dve", tmp[1].ap, x2, sb, ALU.mult, r=ck, w=[tmp[1].k()])
            self.tt("dve", tmp[2].ap, x1, sb, ALU.mult, r=ck, w=[tmp[2].k()])
            self.tt("dve", tmp[3].ap, x2, cb, ALU.mult, r=ck, w=[tmp[3].k()])
            self.tt("pool", krot.ap[:, t, 0:16], tmp[0].ap, tmp[1].ap, ALU.subtract, r=[tmp[0].k(), tmp[1].k()], w=[krot.k((t, 0))])
            self.tt("pool", krot.ap[:, t, 16:32], tmp[2].ap, tmp[3].ap, ALU.add, r=[tmp[2].k(), tmp[3].k()], w=[krot.k((t, 1))])

        def l_tr(t):
            pt = 2 + (t % 2) * 3
            tok = slice(t * 128, (t + 1) * 128)
            hq = hq_tok[t % 2]
            ptb = self.bank_bf(pt)
            for c in range(6):
                self.tr(ptb[:, c * 128:(c + 1) * 128], hq.ap[:, c * 128:(c + 1) * 128], self.ident_bf.ap,
                        r=[hq.k(0), hq.k(1), ("c_identbf",)], w=[("ps", pt)])
            self.tt("dve", hqT.ap[:, :, tok], ptb[:, 0:512].rearrange("p (c t) -> p c t", c=4),
                    gq.ap.unsqueeze(2).to_broadcast([128, 4, 128]), ALU.mult, r=[("ps", pt), gq.k()], w=[hqT.k(t)])
            self.tt("dve", hkvT.ap[:, :, tok], ptb[:, 512:768].rearrange("p (c t) -> p c t", c=2),
                    gkv.ap.unsqueeze(2).to_broadcast([128, 2, 128]), ALU.mult, r=[("ps", pt), gkv.k()], w=[hkvT.k(t)])

        for t in range(NT + 2):
            if t < NT:
                l_mm(t)
            if 0 <= t - 1 < NT:
                l_ew(t - 1)
            if 0 <= t - 2 < NT:
                l_tr(t - 2)
            if t < NT:
                self.dil_norm_chunks[t]()
        if "hqT" in self.debug:
            self.dbg("hqT", hqT.ap, [128, 4, S], BF16, r=hqT.ks(range(NT)))
            self.dbg("hkvT", hkvT.ap, [128, 2, S], BF16, r=hkvT.ks(range(NT)))
            self.dbg("krot", krot.ap, [128, NT, 32], BF16, r=krot.ks([(t, i) for t in range(NT) for i in range(2)]))
        a.release(m0)

    def mla(self, li, hqT, hkvT, krot):
        p = self.p
        a = self.arena
        m0 = a.mark()
        G = 4
        qkT = a.alloc("qkT", [2 * G, S], BF16)
        v = a.alloc("v", [NT, G, 128], BF16)
        wq = a.alloc("wq", [4, G * 96], BF16)
        wkv = a.alloc("wkv", [2, G * 128], BF16)
        q_tok = [a.alloc(f"q_tok{i}", [2 * G, 96], BF16) for i in range(2)]
        tmp = [a.alloc(f"mtmp{i}", [G, 16], F32) for i in range(4)]
        pT = [a.alloc(f"pT{i}", [1024], BF16) for i in range(3)]
        rden = [a.alloc("rden0", [512], F32)] * 2
        scale = 96.0 ** -0.5
        krot_keys = krot.ks([(t, i) for t in range(NT) for i in range(2)])
        cnt = 0
        for gg in range(16 // G):
            self.dma("pool", wq.ap, self.d_w_q[li][:, gg * G * 96:(gg + 1) * G * 96].rearrange("(c p) w -> p c w", p=128), r=[], w=[wq.k()], sem="wq")
            self.dma("pool", wkv.ap, self.d_w_kv[li][:, gg * G * 128:(gg + 1) * G * 128].rearrange("(c p) w -> p c w", p=128), r=[], w=[wkv.k()], sem="wkv")
            self.memset("pool", v.ap[:, :, :, 64:128], 1.0, w=[v.k("ones")])
            def pj_mm(t):
                pa, pb_ = 0 + (t % 2) * 3, 1 + (t % 2) * 3
                tok = slice(t * 128, (t + 1) * 128)
                for c in range(4):
                    self.mm(self.bank(pa)[:, 0:G * 96], hqT.ap[:, c, tok], wq.ap[:, c, :], c == 0, c == 3, r=[hqT.k(t), wq.k()], w=[("ps", pa)])
                for c in range(2):
                    self.mm(self.bank(pb_), hkvT.ap[:, c, tok], wkv.ap[:, c, :], c == 0, c == 1, r=[hkvT.k(t), wkv.k()], w=[("ps", pb_)])

            def pj_ew(t):
                pa, pb_ = 0 + (t % 2) * 3, 1 + (t % 2) * 3
                qt = q_tok[t % 2]
                qv = self.bank(pa)[:, 0:G * 96].rearrange("p (h e) -> p h e", h=G)
                kvv = self.bank(pb_).rearrange("p (h e) -> p h e", h=G)
                self.cp("act", qt.ap[:, G:2 * G, 0:64], kvv[:, :, 0:64], r=[("ps", pb_)], w=[qt.k("kn")])
                self.cp("act", v.ap[:, t, :, 0:64], kvv[:, :, 64:128], r=[("ps", pb_)], w=[v.k(t)])
                self.cp("pool", qt.ap[:, G:2 * G, 64:96], krot.ap[:, t, :].unsqueeze(1).to_broadcast([128, G, 32]), r=krot_keys, w=[qt.k("kr")])
                cb = self.cos_m.ap[:, t, :].unsqueeze(1).to_broadcast([128, G, 16])
                sb = self.sin_m.ap[:, t, :].unsqueeze(1).to_broadcast([128, G, 16])
                x1 = qv[:, :, 64:80]
                x2 = qv[:, :, 80:96]
                ck = [("c_tab",), ("ps", pa)]
                self.cp("dve", qt.ap[:, 0:G, 0:64], qv[:, :, 0:64], r=[("ps", pa)], w=[qt.k("qn")])
                self.tt("dve", tmp[0].ap, x1, cb, ALU.mult, r=ck, w=[tmp[0].k()])
                self.tt("dve", tmp[1].ap, x2, sb, ALU.mult, r=ck, w=[tmp[1].k()])
                self.tt("dve", tmp[2].ap, x1, sb, ALU.mult, r=ck, w=[tmp[2].k()])
                self.tt("dve", tmp[3].ap, x2, cb, ALU.mult, r=ck, w=[tmp[3].k()])
                self.tt("pool", qt.ap[:, 0:G, 64:80], tmp[0].ap, tmp[1].ap, ALU.subtract, r=[tmp[0].k(), tmp[1].k()], w=[qt.k("r1")])
                self.tt("pool", qt.ap[:, 0:G, 80:96], tmp[2].ap, tmp[3].ap, ALU.add, r=[tmp[2].k(), tmp[3].k()], w=[qt.k("r2")])

            def pj_tr(t):
                pt = 2 + (t % 2) * 3
                tok = slice(t * 128, (t + 1) * 128)
                qt = q_tok[t % 2]
                ptb = self.bank_bf(pt)
                for hx in range(2 * G):
                    self.tr(ptb[0:96, hx * 128:(hx + 1) * 128], qt.ap[:, hx, :], self.ident_bf.ap,
                            r=[qt.k("qn"), qt.k("kn"), qt.k("kr"), qt.k("r1"), qt.k("r2"), ("c_identbf",)], w=[("ps", pt)])
                self.cp("act", qkT.ap[0:96, :, tok], ptb[0:96, :].rearrange("p (c t) -> p c t", c=2 * G), r=[("ps", pt)], w=[qkT.k(t)])

            for t in range(NT + 2):
                if t < NT:
                    pj_mm(t)
                if 0 <= t - 1 < NT:
                    pj_ew(t - 1)
                if 0 <= t - 2 < NT:
                    pj_tr(t - 2)
            if "dqkT" in self.debug:
                pass
            if "qkT" in self.debug and gg == 0:
                self.dbg("qkT", qkT.ap, [128, 2 * G, S], BF16, r=qkT.ks(range(NT)))
                self.dbg("v", v.ap, [128, NT, G, 128], BF16, r=v.ks(list(range(NT)) + ["ones"]))
            steps = [(hl, qc, kp) for hl in range(G) for qc in range(4) for kp in range((4 * qc + 4) // 2)]

            def qk(i):
                hl, qc, kp = steps[i]
                sbk = 2 * (i % 3)
                qkeys = qkT.ks(range(4 * qc, 4 * qc + 4))
                for half in range(2):
                    kt = 2 * kp + half
                    r_ = max(0, kt - 4 * qc)
                    self.mm(self.bank(sbk + half)[:, 128 * r_:512], qkT.ap[0:96, G + hl, kt * 128:(kt + 1) * 128],
                            qkT.ap[0:96, hl, qc * 512 + 128 * r_:(qc + 1) * 512], True, True,
                            r=qkeys + [qkT.k(kt)], w=[("ps", sbk + half)])

            def expm(i):
                hl, qc, kp = steps[i]
                sbk = 2 * (i % 3)
                P = pT[i % 3]
                self.act(P.ap, self.bank(sbk, 2), AF.Exp, r=[("ps", sbk), ("ps", sbk + 1)], w=[P.k()], scale=scale)
                for half in range(2):
                    kt = 2 * kp + half
                    r_ = kt - 4 * qc
                    if r_ >= 0:
                        dg = P.ap[:, half * 512 + 128 * r_: half * 512 + 128 * r_ + 128]
                        self.tt("pool", dg, dg, self.tri2.ap[:, 0:128], ALU.mult, r=[P.k(), ("c_tri",)], w=[P.k()])

            def pv(i):
                hl, qc, kp = steps[i]
                h = gg * G + hl
                cc, pb0 = h // 2, (h % 2) * 64
                nk = 4 * qc + 4
                hq = hl * 4 + qc
                ob = 6 + hq % 2
                o_ps = self.bank(ob)
                P = pT[i % 3]
                for half in range(2):
                    kt = 2 * kp + half
                    r_ = max(0, kt - 4 * qc)
                    self.mm(o_ps[:, 128 * r_:512], v.ap[:, kt, hl, :], P.ap[:, half * 512 + 128 * r_:(half + 1) * 512],
                            kt == 0, kt == nk - 1, r=[v.k(kt), v.k("ones"), P.k()], w=[("ps", ob)])
                if kp == nk // 2 - 1:
                    rd = rden[hq % 2]
                    qcols = slice(qc * 512, (qc + 1) * 512)
                    p.add("dve", (lambda e, rd=rd, o_ps=o_ps: e.reciprocal(out=rd.ap[0:64, :], in_=o_ps[64:128, :])), r=[("ps", ob)], w=[rd.k(0), rd.k(1)])
                    self.tt("dve", self.oaT_ap[pb0:pb0 + 64, cc, qcols], o_ps[0:64, :], rd.ap[0:64, :], ALU.mult,
                            r=[("ps", ob), rd.k(0), rd.k(1)], w=[self.oaT.k((cc, qc, h % 2))])

            ns_ = len(steps)
            qk(0)
            qk(1)
            expm(0)
            for i in range(ns_):
                if i + 2 < ns_:
                    qk(i + 2)
                pv(i)
                if i + 1 < ns_:
                    expm(i + 1)
        if "oaT" in self.debug:
            self.dbg("oaT", self.oaT_ap, [128, 8, S], BF16, r=self.oaT.ks([(c, q, i) for c in range(8) for q in range(4) for i in range(2)]))
        a.release(m0)

    def merge(self, li):
        p = self.p
        a = self.arena
        m0 = a.mark()
        wba = a.alloc("wba", [8, D], BF16)
        wbb = a.alloc("wbb", [2, D], BF16)
        wg = a.alloc("wg", [8, 2 * D], BF16)
        wo = a.alloc("wo", [8, D], BF16)
        hTc = a.alloc("hTc", [8, 512], BF16)
        mT = a.alloc("mT", [8, 512], BF16)
        htok = [a.alloc("mhtok", [D], BF16)] * 2
        sa = a.alloc("sa", [512], BF16)
        sb = a.alloc("sb", [512], BF16)
        m1 = a.alloc("m1", [512], F32)
        m2 = a.alloc("m2", [512], F32)
        self.dma("pool", wba.ap, self.d_w_ba[li].rearrange("(c p) w -> p c w", p=128), r=[], w=[wba.k()], sem="wba")
        self.dma("pool", wbb.ap, self.d_w_bb[li].rearrange("(c p) w -> p c w", p=128), r=[], w=[wbb.k()], sem="wbb")
        for hf in range(2):
            self.dma("pool", wg.ap[:, :, hf * D:(hf + 1) * D], self.d_w_in[li][:, 3104 + hf * D:3104 + (hf + 1) * D].rearrange("(c p) w -> p c w", p=128),
                     r=[], w=[wg.k(hf)], sem=f"wg{hf}")
        self.dma("pool", wo.ap, self.d_w_out[li].rearrange("(c p) w -> p c w", p=128), r=[], w=[wo.k()], sem="wo")
        oa_keys = lambda tc: self.oaT.ks([(c, tc, i) for c in range(8) for i in range(2)])
        ob_keys = lambda tc: self.obT.ks([(pp, tc) for pp in range(2)])
        for tc in range(4):
            cs = slice(tc * 512, (tc + 1) * 512)
            for tl in range(4):
                t = tc * 4 + tl
                self.norm_T_tile(t, self.rstd_mix, self.gT_mix, htok[t % 2], hTc.ap, slice(tl * 128, (tl + 1) * 128), 7, [hTc.k(tl)])
            for f in range(8):
                fs = slice(f * 128, (f + 1) * 128)
                for c in range(8):
                    self.mm(self.bank(0), wba.ap[:, c, fs], self.oaT_ap[:, c, cs], c == 0, c == 7, r=[wba.k()] + oa_keys(tc), w=[("ps", 0)])
                for c in range(2):
                    self.mm(self.bank(1), wbb.ap[:, c, fs], self.obT.ap[:, c, cs], c == 0, c == 1, r=[wbb.k()] + ob_keys(tc), w=[("ps", 1)])
                for c in range(8):
                    self.mm(self.bank(2), wg.ap[:, c, fs], hTc.ap[:, c, :], c == 0, c == 7, r=[wg.k(0)] + hTc.ks(range(4)), w=[("ps", 2)])
                for c in range(8):
                    self.mm(self.bank(3), wg.ap[:, c, D + f * 128:D + (f + 1) * 128], hTc.ap[:, c, :], c == 0, c == 7, r=[wg.k(1)] + hTc.ks(range(4)), w=[("ps", 3)])
                self.act(sa.ap, self.bank(2), AF.Sigmoid, r=[("ps", 2)], w=[sa.k()])
                self.act(sb.ap, self.bank(3), AF.Sigmoid, r=[("ps", 3)], w=[sb.k()])
                self.tt("dve", m1.ap, self.bank(0), sa.ap, ALU.mult, r=[("ps", 0), sa.k()], w=[m1.k()])
                self.tt("dve", m2.ap, self.bank(1), sb.ap, ALU.mult, r=[("ps", 1), sb.k()], w=[m2.k()])
                self.tt("pool", mT.ap[:, f, :], m1.ap, m2.ap, ALU.add, r=[m1.k(), m2.k()], w=[mT.k(f)])
            if "mT" in self.debug and tc == 0:
                self.dbg("mT", mT.ap, [128, 8, 512], BF16, r=mT.ks(range(8)))
            for tl in range(4):
                t = tc * 4 + tl
                for hf in range(2):
                    ob = 4 + (2 * tl + hf) % 2
                    for f in range(8):
                        self.mm(self.bank(ob), mT.ap[:, f, tl * 128:(tl + 1) * 128], wo.ap[:, f, hf * 512:(hf + 1) * 512], f == 0, f == 7,
                                r=mT.ks(range(8)) + [wo.k()], w=[("ps", ob)])
                    xs_ = self.x_tok[:, t, hf * 512:(hf + 1) * 512]
                    self.tt("dve", xs_, xs_, self.bank(ob), ALU.add, r=[("x", t), ("ps", ob)], w=[("x", t)])
        a.release(m0)

    def moe(self, li):
        p = self.p
        a = self.arena
        m0 = a.mark()
        rstd = self.x_rstd("ffn")
        gT = self.gain_T("ffn", self.d_g_ffn, li, 8)
        gT8 = self.gain_T("ffn8", self.d_g_ffn, li, 8, mode="p8")
        xh = a.alloc("xh", [NT, D], BF16)
        gw1 = a.alloc("gw1", [NT], F32)
        gw2 = a.alloc("gw2", [NT], F32)
        di1 = a.alloc("di1", [NT], I32)
        di2 = a.alloc("di2", [NT], I32)
        m_route = a.mark()
        wr = a.alloc("wr", [8, 72], F32)
        brb = a.alloc("brb", [72], F32)
        lt = a.alloc("lt", [NT, 72], F32)
        x32 = [a.alloc(f"x32_{i}", [D], F32) for i in range(2)]
        h2T = [a.alloc(f"h2T{i}", [8, 128], F32) for i in range(2)]
        self.dma("sp", wr.ap[:, :, 0:8], self.d_w_rg[li].rearrange("(c p) w -> p c w", p=128), r=[], w=[wr.k(0)], sem="wr0", allow_slow_non_contiguous=True)
        self.dma("sp", wr.ap[:, :, 8:72], self.d_w_re[li].rearrange("(c p) w -> p c w", p=128), r=[], w=[wr.k(1)], sem="wr1", allow_slow_non_contiguous=True)
        self.dma("sp", brb.ap[:, 0:8], self.d_b_rg[li:li + 1, :].to_broadcast([128, 8]), r=[], w=[brb.k(0)], sem="brb0", allow_slow_non_contiguous=True)
        self.dma("sp", brb.ap[:, 8:72], self.d_b_re[li:li + 1, :].to_broadcast([128, 64]), r=[], w=[brb.k(1)], sem="brb1", allow_slow_non_contiguous=True)
        def r_a(t):
            self.act(xh.ap[:, t, :], self.x_tok[:, t, :], AF.Copy, r=[("x", t), rstd.k()], w=[xh.k(t)], scale=rstd.ap[:, t:t + 1])
            x3 = x32[t % 2]
            self.ts("dve", x3.ap, self.x_tok[:, t, :], rstd.ap[:, t:t + 1], None, ALU.mult, None, r=[("x", t), rstd.k()], w=[x3.k()])

        def r_b(t):
            x3 = x32[t % 2]
            pa = 0 + 2 * (t % 2)
            for c in range(8):
                self.tr(self.bank(pa, 2)[:, c * 128:(c + 1) * 128], x3.ap[:, c * 128:(c + 1) * 128], self.ident_f.ap,
                        r=[x3.k(), ("c_identf",)], w=[("ps", pa), ("ps", pa + 1)])
            hT_ = h2T[t % 2]
            self.tt("dve", hT_.ap, self.bank(pa, 2).rearrange("p (c t) -> p c t", c=8), gT.ap.unsqueeze(2).to_broadcast([128, 8, 128]), ALU.mult,
                    r=[("ps", pa), ("ps", pa + 1), gT.k()], w=[hT_.k()])

        def r_c(t):
            hT_ = h2T[t % 2]
            pr = 4 + (t % 2)
            for c in range(8):
                self.mm(self.bank(pr)[:, 0:72], hT_.ap[:, c, :], wr.ap[:, c, :], c == 0, c == 7, r=[hT_.k(), wr.k(0), wr.k(1)], w=[("ps", pr)])
            self.tt("dve", lt.ap[:, t, :], self.bank(pr)[:, 0:72], brb.ap, ALU.add, r=[("ps", pr), brb.k(0), brb.k(1)], w=[lt.k(t)])

        for t in range(NT + 2):
            if t < NT:
                r_a(t)
            if 0 <= t - 1 < NT:
                r_b(t - 1)
            if 0 <= t - 2 < NT:
                r_c(t - 2)
        ltk = lt.ks(range(NT))
        al = lambda name, shape, dt=F32: a.alloc(name, shape, dt)
        ngmax = al("ngmax", [NT])
        oh_g = al("oh_g", [NT, 8])
        eg = al("eg", [NT, 8])
        sume = al("sume", [NT])
        pg = al("pg", [NT])
        tmp64 = al("tmp64", [NT, 8, 8])
        e_in = al("e_in", [NT, 8])
        mx1 = al("mx1", [NT])
        mk1 = al("mk1", [NT, 8])
        e2 = al("e2", [NT, 8])
        mx2 = al("mx2", [NT])
        mk2 = al("mk2", [NT, 8])
        dd = al("dd", [NT])
        sel1 = al("sel1", [NT, 8, 8])
        sel2 = al("sel2", [NT, 8, 8])
        selb = al("selb", [NT, 64], BF16)
        rank = al("rank", [NT, 64])
        rs1 = al("rs1", [NT])
        rs2 = al("rs2", [NT])
        d1 = al("d1", [NT])
        d2 = al("d2", [NT])
        lg = lt.ap[:, :, 0:8]
        le = lt.ap[:, :, 8:72].rearrange("p t (g e) -> p t g e", g=8)
        B3 = lambda ap_: ap_.unsqueeze(2).to_broadcast([128, NT, 8])
        V = "dve"
        p.add(V, lambda e: e.tensor_reduce(out=ngmax.ap, in_=lg, axis=AX.X, op=ALU.max, negate=True), r=ltk, w=[ngmax.k()])
        self.tt(V, eg.ap, lg, B3(ngmax.ap), ALU.add, r=ltk + [ngmax.k()], w=[eg.k()])
        self.ts(V, oh_g.ap, eg.ap, 0.0, None, ALU.is_equal, None, r=[eg.k()], w=[oh_g.k()])
        self.act(eg.ap, eg.ap, AF.Exp, r=[eg.k()], w=[eg.k()])
        p.add(V, lambda e: e.tensor_reduce(out=sume.ap, in_=eg.ap, axis=AX.X, op=ALU.add), r=[eg.k()], w=[sume.k()])
        p.add(V, lambda e: e.reciprocal(out=pg.ap, in_=sume.ap), r=[sume.k()], w=[pg.k()])
        self.tt(V, tmp64.ap, le, oh_g.ap.unsqueeze(3).to_broadcast([128, NT, 8, 8]), ALU.mult, r=ltk + [oh_g.k()], w=[tmp64.k()])
        p.add(V, lambda e: e.tensor_reduce(out=e_in.ap, in_=tmp64.ap.rearrange("p t g e -> p t e g"), axis=AX.X, op=ALU.add), r=[tmp64.k()], w=[e_in.k()])
        p.add(V, lambda e: e.tensor_reduce(out=mx1.ap, in_=e_in.ap, axis=AX.X, op=ALU.max), r=[e_in.k()], w=[mx1.k()])
        self.tt(V, mk1.ap, e_in.ap, B3(mx1.ap), ALU.is_equal, r=[e_in.k(), mx1.k()], w=[mk1.k()])
        self.stt(e2.ap, mk1.ap, -1e30, e_in.ap, ALU.mult, ALU.add, r=[mk1.k(), e_in.k()], w=[e2.k()])
        p.add(V, lambda e: e.tensor_reduce(out=mx2.ap, in_=e2.ap, axis=AX.X, op=ALU.max), r=[e2.k()], w=[mx2.k()])
        self.tt(V, mk2.ap, e2.ap, B3(mx2.ap), ALU.is_equal, r=[e2.k(), mx2.k()], w=[mk2.k()])
        self.tt(V, dd.ap, mx2.ap, mx1.ap, ALU.subtract, r=[mx1.k(), mx2.k()], w=[dd.k()])
        self.act(dd.ap, dd.ap, AF.Exp, r=[dd.k()], w=[dd.k()])
        self.ts(V, gw1.ap, dd.ap, 1.0, None, ALU.add, None, r=[dd.k()], w=[gw1.k()])
        p.add(V, lambda e: e.reciprocal(out=gw1.ap, in_=gw1.ap), r=[gw1.k()], w=[gw1.k()])
        self.tt(V, gw1.ap, gw1.ap, pg.ap, ALU.mult, r=[gw1.k(), pg.k()], w=[gw1.k()])
        self.tt(V, gw2.ap, gw1.ap, dd.ap, ALU.mult, r=[gw1.k(), dd.k()], w=[gw2.k()])
        ohb = oh_g.ap.unsqueeze(3).to_broadcast([128, NT, 8, 8])
        self.tt(V, sel1.ap, ohb, mk1.ap.unsqueeze(2).to_broadcast([128, NT, 8, 8]), ALU.mult, r=[oh_g.k(), mk1.k()], w=[sel1.k()])
        self.tt(V, sel2.ap, ohb, mk2.ap.unsqueeze(2).to_broadcast([128, NT, 8, 8]), ALU.mult, r=[oh_g.k(), mk2.k()], w=[sel2.k()])
        s1f = sel1.ap.rearrange("p t g e -> p t (g e)")
        s2f = sel2.ap.rearrange("p t g e -> p t (g e)")
        self.tt(V, selb.ap, s1f, s2f, ALU.add, r=[sel1.k(), sel2.k()], w=[selb.k()])
        for t in range(NT):
            rb = 6 + t // 8
            out = self.bank(rb)[:, (t % 8) * 64:(t % 8) * 64 + 64]
            self.mm(out, self.ustrict.ap, selb.ap[:, t, :], True, t == 0, r=[selb.k(), ("c_us",)], w=[("ps", rb)])
            for t2 in range(t):
                self.mm(out, self.ones_bf.ap, selb.ap[:, t2, :], False, t2 == t - 1, r=[selb.k(), ("c_onesbf",)], w=[("ps", rb)])
        self.cp(V, rank.ap, self.bank(6, 2).rearrange("p (t e) -> p t e", t=NT), r=[("ps", 6), ("ps", 7)], w=[rank.k()])
        ebb = self.ebase.ap.unsqueeze(1).to_broadcast([128, NT, 64])
        tf = tmp64.ap.rearrange("p t g e -> p t (g e)")
        for (sf, sk, rs, dst, dsti, gw) in ((s1f, sel1, rs1, d1, di1, gw1), (s2f, sel2, rs2, d2, di2, gw2)):
            self.tt(V, tf, sf, rank.ap, ALU.mult, r=[sk.k(), rank.k()], w=[tmp64.k()])
            p.add(V, (lambda e, rs=rs: e.tensor_reduce(out=rs.ap, in_=tf, axis=AX.X, op=ALU.add)), r=[tmp64.k()], w=[rs.k()])
            self.tt(V, tf, sf, ebb, ALU.mult, r=[sk.k(), ("c_ebase",)], w=[tmp64.k()])
            p.add(V, (lambda e, dst=dst: e.tensor_reduce(out=dst.ap, in_=tf, axis=AX.X, op=ALU.add)), r=[tmp64.k()], w=[dst.k()])
            self.tt(V, dst.ap, dst.ap, rs.ap, ALU.add, r=[dst.k(), rs.k()], w=[dst.k()])
            self.ts(V, rs.ap, rs.ap, float(CAP), None, ALU.is_ge, None, r=[rs.k()], w=[rs.k()])
            self.stt(dst.ap, rs.ap, 1.0e6, dst.ap, ALU.mult, ALU.add, r=[rs.k(), dst.k()], w=[dst.k()])
            self.cp(V, dsti.ap, dst.ap, r=[dst.k()], w=[dsti.k()])
        if "route" in self.debug:
            self.dbg("lt", lt.ap, [128, NT, 72], F32, r=ltk)
            self.dbg("d1", d1.ap, [128, NT], F32, r=[d1.k()])
            self.dbg("d2", d2.ap, [128, NT], F32, r=[d2.k()])
            self.dbg("gw1", gw1.ap, [128, NT], F32, r=[gw1.k()])
            self.dbg("gw2", gw2.ap, [128, NT], F32, r=[gw2.k()])
        xs_d = self.d_xs[li]
        ys_d = self.d_ys[li]
        for t in range(NT):
            for (dsti, nm) in ((di1, "a"), (di2, "b")):
                p.add("pool", (lambda e, t=t, dsti=dsti: e.indirect_dma_start(
                    out=xs_d, out_offset=bass.IndirectOffsetOnAxis(ap=dsti.ap[:, t:t + 1], axis=0),
                    in_=xh.ap[:, t, :], in_offset=None, bounds_check=self.bc_reg(e), oob_is_err=False)),
                    r=[xh.k(t), dsti.k()], w=[("xs", li, t, nm)], dma="scat")
        xs_keys = [("xs", li, t, nm) for t in range(NT) for nm in "ab"]
        a.release(m_route)
        m_exp = a.mark()
        NW, NW2, NX = 4, 6, 3
        w1 = [a.alloc(f"w1_{i}", [8, 256], BF16) for i in range(NW)]
        w3 = [a.alloc(f"w3_{i}", [8, 256], BF16) for i in range(NW)]
        w2 = [a.alloc(f"w2_{i}", [2, D], BF16) for i in range(NW2)]
        xse = [a.alloc(f"xse{i}", [D], BF16) for i in range(NX)]
        xsT = [a.alloc(f"xsT{i}", [8, 128], BF16) for i in range(2)]
        sl = [a.alloc(f"sl{i}", [256], F32) for i in range(2)]
        gg_ = [a.alloc(f"g{i}", [256], BF16) for i in range(2)]
        gT_ = [a.alloc(f"gT{i}", [2, 128], BF16) for i in range(2)]
        ye = [a.alloc(f"ye{i}", [D], F32) for i in range(2)]

        def st_load(ex):
            self.dma("pool", w1[ex % NW].ap, self.d_w1[li, ex].rearrange("(p c) f -> p c f", p=128), r=[], w=[w1[ex % NW].k()], sem=f"w1_{ex % NW}")
            self.dma("pool", w3[ex % NW].ap, self.d_w3[li, ex].rearrange("(p c) f -> p c f", p=128), r=[], w=[w3[ex % NW].k()], sem=f"w3_{ex % NW}")
            self.dma("pool", w2[ex % NW2].ap, self.d_w2[li, ex].rearrange("(p c) f -> p c f", p=128), r=[], w=[w2[ex % NW2].k()], sem=f"w2_{ex % NW2}")
            self.dma("sp", xse[ex % NX].ap, xs_d[ex * CAP:(ex + 1) * CAP, :], r=xs_keys, w=[xse[ex % NX].k()], sem=f"xse{ex % NX}")

        def st_a(ex):
            pt = ex % 2
            ptb = self.bank_bf(pt)
            xb = xse[ex % NX]
            for c in range(8):
                self.tr(ptb[:, c * 128:(c + 1) * 128], xb.ap[:, c:D:8], self.ident_bf.ap, r=[xb.k(), ("c_identbf",)], w=[("ps", pt)])
            self.tt("dve", xsT[ex % 2].ap, ptb.rearrange("p (c t) -> p c t", c=8), gT8.ap.unsqueeze(2).to_broadcast([128, 8, 128]), ALU.mult,
                    r=[("ps", pt), gT8.k()], w=[xsT[ex % 2].k()])

        def st_b(ex):
            s2 = ex % 2
            ph = 2 + s2
            for c in range(8):
                self.mm(self.bank(ph)[:, 0:256], xsT[s2].ap[:, c, :], w1[ex % NW].ap[:, c, :], c == 0, c == 7, r=[xsT[s2].k(), w1[ex % NW].k()], w=[("ps", ph)])
            for c in range(8):
                self.mm(self.bank(ph)[:, 256:512], xsT[s2].ap[:, c, :], w3[ex % NW].ap[:, c, :], c == 0, c == 7, r=[xsT[s2].k(), w3[ex % NW].k()], w=[("ps", ph)])
            self.act(sl[s2].ap, self.bank(ph)[:, 0:256], AF.Silu, r=[("ps", ph)], w=[sl[s2].k()])
            self.tt("dve", gg_[s2].ap, sl[s2].ap, self.bank(ph)[:, 256:512], ALU.mult, r=[sl[s2].k(), ("ps", ph)], w=[gg_[s2].k()])

        def st_c(ex):
            s2 = ex % 2
            pt = (ex + 1) % 2
            ptb = self.bank_bf(pt)
            for c in range(2):
                self.tr(ptb[:, c * 128:(c + 1) * 128], gg_[s2].ap[:, c:256:2], self.ident_bf.ap, r=[gg_[s2].k(), ("c_identbf",)], w=[("ps", pt)])
            self.cp("act", gT_[s2].ap, ptb[:, 0:256].rearrange("p (c t) -> p c t", c=2), r=[("ps", pt)], w=[gT_[s2].k()])

        def st_d(ex):
            s2 = ex % 2
            py = 4 + 2 * s2
            for hf in range(2):
                for c in range(2):
                    self.mm(self.bank(py + hf), gT_[s2].ap[:, c, :], w2[ex % NW2].ap[:, c, hf * 512:(hf + 1) * 512], c == 0, c == 1,
                            r=[gT_[s2].k(), w2[ex % NW2].k()], w=[("ps", py + hf)])
            self.cp("act", ye[s2].ap[:, 0:512], self.bank(py), r=[("ps", py)], w=[ye[s2].k(0)])
            self.cp("dve", ye[s2].ap[:, 512:1024], self.bank(py + 1), r=[("ps", py + 1)], w=[ye[s2].k(1)])
            self.dma("sp", ys_d[ex * CAP:(ex + 1) * CAP, :], ye[s2].ap, r=[ye[s2].k(0), ye[s2].k(1)], w=[("ys", li, ex)], sem=f"yst{s2}")

        for ex in range(min(NX - 1, NEXP)):
            st_load(ex)
        for i in range(NEXP + 4):
            if 0 <= i - 1 < NEXP:
                st_a(i - 1)
            if 0 <= i - 2 < NEXP:
                st_b(i - 2)
            if 0 <= i - 3 < NEXP:
                st_c(i - 3)
            if 0 <= i - 4 < NEXP:
                st_d(i - 4)
            if i + NX - 1 < NEXP:
                st_load(i + NX - 1)
        ys_keys = [("ys", li, ex) for ex in range(NEXP)]
        a.release(m_exp)
        self.moe_state = dict(di1=di1, di2=di2, gw1=gw1, gw2=gw2, ys_d=ys_d, ys_keys=ys_keys)

    def ple(self, li, do_ple=True):
        p = self.p
        a = self.arena
        m0 = a.mark()
        ms = self.moe_state
        di = (ms["di1"], ms["di2"])
        gw = (ms["gw1"], ms["gw2"])
        ys_d, ys_keys = ms["ys_d"], ms["ys_keys"]
        yb = [[a.alloc(f"y{k}_{i}", [D], F32) for i in range(2)] for k in range(2)]
        gT = self.gain_T("ple", self.d_g_ple, li, 8)
        st = a.alloc("pstat", [NT, 2], F32)
        wpg = a.alloc("wpg", [8, D], BF16)
        wpp = a.alloc("wpp", [2, D], BF16)
        ptok = [a.alloc(f"ptok{i}", [256], BF16) for i in range(3)]
        pTt = [a.alloc(f"pT{i}", [2, 128], BF16) for i in range(2)]
        htok = [a.alloc(f"phtok{i}", [D], BF16) for i in range(2)]
        h3T = [a.alloc(f"h3T{i}", [8, 128], BF16) for i in range(2)]
        sg = [a.alloc(f"sg{i}", [512], F32) for i in range(4)]
        ge = [a.alloc(f"ge{i}", [512], F32) for i in range(4)]
        junk = a.alloc("pjunk", [D], BF16)
        if do_ple:
            self.dma("pool", wpg.ap, self.d_w_pg[li].rearrange("(c p) w -> p c w", p=128), r=[], w=[wpg.k()], sem="wpg")
            self.dma("pool", wpp.ap, self.d_w_pp[li].rearrange("(c p) w -> p c w", p=128), r=[], w=[wpp.k()], sem="wpp")
        pv = self.d_p[li].rearrange("(t p) d -> p t d", p=128)

        def c_g(t):
            for k in range(2):
                y = yb[k][t % 2]
                self.memset("pool", y.ap, 0.0, w=[y.k()])
                p.add("pool", (lambda e, t=t, y=y, k=k: e.indirect_dma_start(
                    out=y.ap, out_offset=None, in_=ys_d, in_offset=bass.IndirectOffsetOnAxis(ap=di[k].ap[:, t:t + 1], axis=0),
                    bounds_check=self.bc_reg(e), oob_is_err=False)), r=ys_keys + [di[k].k(), y.k()], w=[y.k()], dma=f"gath{k}{t % 2}")
            if do_ple:
                self.dma("pool", ptok[t % 3].ap, pv[:, t, :], r=[], w=[ptok[t % 3].k()], sem=f"ptok{t % 3}")

        def c_x(t):
            for k in range(2):
                y = yb[k][t % 2]
                self.stt(self.x_tok[:, t, :], y.ap, gw[k].ap[:, t:t + 1], self.x_tok[:, t, :], ALU.mult, ALU.add,
                         r=[y.k(), gw[k].k(), ("x", t)], w=[("x", t)])
            if not do_ple:
                return
            self.act(junk.ap, self.x_tok[:, t, :], AF.Square, r=[("x", t)], w=[st.k((t, 0)), junk.k()], accum_out=st.ap[:, t, 0:1])
            self.ts("dve", st.ap[:, t, 0:1], st.ap[:, t, 0:1], 1.0 / D, EPS, ALU.mult, ALU.add, r=[st.k((t, 0))], w=[st.k((t, 0))])
            self.act(st.ap[:, t, 0:1], st.ap[:, t, 0:1], AF.Sqrt, r=[st.k((t, 0))], w=[st.k((t, 0))])
            p.add("dve", (lambda e, t=t: e.reciprocal(out=st.ap[:, t, 1:2], in_=st.ap[:, t, 0:1])), r=[st.k((t, 0))], w=[st.k((t, 1))])
            self.act(htok[t % 2].ap, self.x_tok[:, t, :], AF.Copy, r=[("x", t), st.k((t, 1))], w=[htok[t % 2].k()], scale=st.ap[:, t, 1:2])

        def p_b(t):
            s2 = t % 2
            pb = self.bank_bf(4 + s2)
            for c in range(8):
                self.tr(pb[:, c * 128:(c + 1) * 128], htok[s2].ap[:, c * 128:(c + 1) * 128], self.ident_bf.ap,
                        r=[htok[s2].k(), ("c_identbf",)], w=[("ps", 4 + s2)])
            self.tt("dve", h3T[s2].ap, pb.rearrange("p (c t) -> p c t", c=8), gT.ap.unsqueeze(2).to_broadcast([128, 8, 128]), ALU.mult,
                    r=[("ps", 4 + s2), gT.k()], w=[h3T[s2].k()])
            ptb = self.bank_bf(6 + s2)
            for c in range(2):
                self.tr(ptb[:, c * 128:(c + 1) * 128], ptok[t % 3].ap[:, c * 128:(c + 1) * 128], self.ident_bf.ap, r=[ptok[t % 3].k(), ("c_identbf",)], w=[("ps", 6 + s2)])
            self.cp("act", pTt[s2].ap, ptb[:, 0:256].rearrange("p (c t) -> p c t", c=2), r=[("ps", 6 + s2)], w=[pTt[s2].k()])

        def p_c(t):
            s2 = t % 2
            for hf in range(2):
                pg_, pe_ = 0 + 2 * hf, 1 + 2 * hf
                cs = slice(hf * 512, (hf + 1) * 512)
                ix = 2 * s2 + hf
                for c in range(8):
                    self.mm(self.bank(pg_), h3T[s2].ap[:, c, :], wpg.ap[:, c, cs], c == 0, c == 7, r=[h3T[s2].k(), wpg.k()], w=[("ps", pg_)])
                for c in range(2):
                    self.mm(self.bank(pe_), pTt[s2].ap[:, c, :], wpp.ap[:, c, cs], c == 0, c == 1, r=[pTt[s2].k(), wpp.k()], w=[("ps", pe_)])
                self.act(sg[ix].ap, self.bank(pg_), AF.Sigmoid, r=[("ps", pg_)], w=[sg[ix].k()])
                self.tt("dve", ge[ix].ap, sg[ix].ap, self.bank(pe_), ALU.mult, r=[sg[ix].k(), ("ps", pe_)], w=[ge[ix].k()])
                xs_ = self.x_tok[:, t, cs]
                self.tt("pool", xs_, xs_, ge[ix].ap, ALU.add, r=[("x", t), ge[ix].k()], w=[("x", t)])

        for i in range(NT + 3):
            if i < NT:
                c_g(i)
            if 0 <= i - 1 < NT:
                c_x(i - 1)
            if do_ple and 0 <= i - 2 < NT:
                p_b(i - 2)
            if do_ple and 0 <= i - 3 < NT:
                p_c(i - 3)
        a.release(m0)

    def final(self):
        p = self.p
        a = self.arena
        m0 = a.mark()
        rstd = self.x_rstd("fin")
        gb = a.alloc("gfin", [D], F32)
        yb = [a.alloc(f"fy{i}", [D], F32) for i in range(2)]
        self.dma("sp", gb.ap, self.d_g_fin.rearrange("(o d) -> o d", o=1).to_broadcast([128, D]), r=[], w=[gb.k()], sem="gfin", allow_slow_non_contiguous=True)
        ov = self.d_out.rearrange("(t p) d -> p t d", p=128)
        for t in range(NT):
            y = yb[t % 2]
            self.stt(y.ap, self.x_tok[:, t, :], rstd.ap[:, t:t + 1], gb.ap, ALU.mult, ALU.mult, r=[("x", t), rstd.k(), gb.k()], w=[y.k()])
            self.dma("sp", ov[:, t, :], y.ap, r=[y.k()], w=[("out", t)], sem=f"ost{t % 2}")
            self.out_keys.append(("out", t))
        a.release(m0)


_W_NAMES = ["g_mix", "w_in", "g_q_lat", "w_q_up", "g_kv_lat", "w_kv_up", "w_branch_a", "w_branch_b", "w_out", "g_ffn",
            "w_router_grp", "b_router_grp", "w_router_exp", "b_router_exp", "w_exp_gate", "w_exp_up", "w_exp_down",
            "g_ple", "w_ple_gate", "w_ple_proj"]

LAYERS_PER_LAUNCH = 4


def _run(x, inputs, layer_lo, layer_hi, final_norm):
    nl = layer_hi - layer_lo
    b = Builder(nl, first_layer=layer_lo, final_norm=final_norm)
    nc = b.build()
    shared = {}
    for k in _W_NAMES:
        shared[k] = np.ascontiguousarray(np.asarray(inputs[k])[layer_lo:layer_hi])
    shared["b_router_exp"] = shared["b_router_exp"].reshape(nl, 64)
    shared["g_final"] = np.ascontiguousarray(np.asarray(inputs["g_final"]))
    p_all = np.asarray(inputs["p"])
    pos = np.asarray(inputs["positions"]).astype(np.int32)
    in_maps = []
    for c in range(NCORES):
        m = dict(shared)
        m["x"] = np.ascontiguousarray(x[c])
        m["p"] = np.ascontiguousarray(p_all[layer_lo:layer_hi, c])
        m["positions"] = np.ascontiguousarray(pos[c])
        in_maps.append(m)
    res = run_bass_kernel_spmd(nc, in_maps, core_ids=list(range(NCORES)))
    return np.stack([np.asarray(r["out"]) for r in res.results], axis=0)


def kernel(**inputs):
    x = np.asarray(inputs["x"]).astype(np.float32, copy=False)
    lo = 0
    while lo < DEPTH:
        hi = min(DEPTH, lo + LAYERS_PER_LAUNCH)
        x = _run(x, inputs, lo, hi, final_norm=(hi == DEPTH))
        lo = hi
    return x.astype(np.float32, copy=False)
```
